# Optimizing a Trainium2 kernel written in Bass

```python
import math
import jax
import jax.numpy as jnp
from jax import lax
import numpy as np

D_MODEL = 1024
BATCH = 8
SEQ = 8192
DEPTH = 2

CTX_LEN = 256
GRID_W = 64
HEAD_DIM = 64
GQA_HEADS = 8
GQA_KV_HEADS = 2
GQA_GROUP = GQA_HEADS // GQA_KV_HEADS
DIFF_HEADS = 4
DIFF_DIM = HEAD_DIM
DIFF_VDIM = 2 * DIFF_DIM
GDN_HEADS = 4
GDN_DK = 128
GDN_DV = 128
GDN_CONV = 3
GDN_CHUNK = 64
N_BRANCH = 3
BRANCH_W = 512
N_EXPERTS = 16
EXPERT_FF = 1024
CAPACITY_FACTOR = 2
Q_BLOCK = 128
ROPE_THETA = 10000.0
EPS = 1e-6

A_Q = GQA_HEADS * HEAD_DIM
A_KV = GQA_KV_HEADS * HEAD_DIM
B_QK = DIFF_HEADS * 2 * DIFF_DIM
B_V = DIFF_HEADS * DIFF_VDIM
C_QK = GDN_HEADS * GDN_DK
C_V = GDN_HEADS * GDN_DV
C_GATE = 2 * GDN_HEADS
IN_WIDTHS = (A_Q, A_KV, A_KV, B_QK, B_QK, B_V, C_QK, C_QK, C_V, C_V, C_GATE, C_GATE)
IN_COLS = sum(IN_WIDTHS)
GDN_CONV_CH = 2 * C_QK + C_V

kernel_name = 'hybrid_dit_gqa_diffattn_gdn_ecmoe'


def rms_norm(x, w):
    xf = x.astype(jnp.float32)
    y = xf * lax.rsqrt(jnp.mean(xf * xf, axis=-1, keepdims=True) + EPS)
    return (y * w.astype(jnp.float32)).astype(x.dtype)


def l2_norm(x):
    xf = x.astype(jnp.float32)
    return xf * lax.rsqrt(jnp.sum(xf * xf, axis=-1, keepdims=True) + EPS)


def lambda_init(layer_idx):
    return 0.8 - 0.6 * math.exp(-0.3 * layer_idx)


def axial_rope(n, dim):
    rows = n // GRID_W
    row = jnp.repeat(jnp.arange(rows, dtype=jnp.float32), GRID_W)
    col = jnp.tile(jnp.arange(GRID_W, dtype=jnp.float32), rows)
    axis_dim = dim // 2
    inv_freq = ROPE_THETA ** (-jnp.arange(0, axis_dim, 2, dtype=jnp.float32) / axis_dim)
    ang = jnp.concatenate([row[:, None] * inv_freq, col[:, None] * inv_freq], axis=-1)
    return jnp.cos(ang), jnp.sin(ang)


def apply_rope(x, cos, sin):
    b, n, h, dim = x.shape
    xa = x.reshape(b, n, h, 2, 2, dim // 4)
    x1, x2 = xa[..., 0, :], xa[..., 1, :]
    c = cos.reshape(n, 1, 2, dim // 4).astype(x.dtype)
    s = sin.reshape(n, 1, 2, dim // 4).astype(x.dtype)
    out = jnp.stack([x1 * c - x2 * s, x2 * c + x1 * s], axis=-2)
    return out.reshape(b, n, h, dim)


def qk_heads(t, n_heads, dim, norm_w, rope):
    b, n = t.shape[:2]
    t = rms_norm(t.reshape(b, n, n_heads, dim), norm_w)
    if rope is not None:
        t = apply_rope(t, *rope)
    return t


def _blocks(q):
    b, n = q.shape[:2]
    q = q.reshape((b, n // Q_BLOCK, Q_BLOCK) + q.shape[2:])
    return jnp.moveaxis(q, 1, 0)


def _unblocks(o):
    o = jnp.moveaxis(o, 0, 1)
    return o.reshape((o.shape[0], o.shape[1] * o.shape[2]) + o.shape[3:])


def gqa_attend(q, k, v):
    scale = HEAD_DIM ** -0.5

    def one(qb):
        s = jnp.einsum('bqhgd,bkhd->bhgqk', qb, k, preferred_element_type=jnp.float32) * scale
        p = jax.nn.softmax(s, axis=-1).astype(v.dtype)
        return jnp.einsum('bhgqk,bkhd->bqhgd', p, v)

    return _unblocks(lax.map(one, _blocks(q)))


def diff_attend(q, k, v, lam):
    scale = DIFF_DIM ** -0.5

    def one(qb):
        s = jnp.einsum('bqhcd,bkhcd->bhcqk', qb, k, preferred_element_type=jnp.float32) * scale
        p = jax.nn.softmax(s, axis=-1)
        w = (p[:, :, 0] - lam * p[:, :, 1]).astype(v.dtype)
        return jnp.einsum('bhqk,bkhe->bqhe', w, v)

    return _unblocks(lax.map(one, _blocks(q)))


def gqa_branch(lat, ctx, qn, kn, rope, need_ctx):
    q, k, v = lat
    qc, kc, vc = ctx
    b, n = q.shape[:2]

    def grp(t):
        return t.reshape(t.shape[0], t.shape[1], GQA_KV_HEADS, GQA_GROUP, HEAD_DIM)

    def vh(t):
        return t.reshape(t.shape[0], t.shape[1], GQA_KV_HEADS, HEAD_DIM)

    k_ctx = qk_heads(kc, GQA_KV_HEADS, HEAD_DIM, kn, None)
    v_ctx = vh(vc)
    k_all = jnp.concatenate([qk_heads(k, GQA_KV_HEADS, HEAD_DIM, kn, rope), k_ctx], axis=1)
    v_all = jnp.concatenate([vh(v), v_ctx], axis=1)
    y = gqa_attend(grp(qk_heads(q, GQA_HEADS, HEAD_DIM, qn, rope)), k_all, v_all).reshape(b, n, A_Q)
    yc = None
    if need_ctx:
        yc = gqa_attend(grp(qk_heads(qc, GQA_HEADS, HEAD_DIM, qn, None)), k_ctx, v_ctx)
        yc = yc.reshape(qc.shape[0], qc.shape[1], A_Q)
    return y, yc


def diff_branch(lat, ctx, qn, kn, lam, lam_init, subln_w, rope, need_ctx):
    q, k, v = lat
    qc, kc, vc = ctx

    def heads(t, norm_w, rp):
        t = qk_heads(t, DIFF_HEADS * 2, DIFF_DIM, norm_w, rp)
        return t.reshape(t.shape[0], t.shape[1], DIFF_HEADS, 2, DIFF_DIM)

    def vh(t):
        return t.reshape(t.shape[0], t.shape[1], DIFF_HEADS, DIFF_VDIM)

    def finish(o):
        o = rms_norm(o, subln_w) * (1.0 - lam_init)
        return o.reshape(o.shape[0], o.shape[1], B_V)

    k_ctx = heads(kc, kn, None)
    v_ctx = vh(vc)
    k_all = jnp.concatenate([heads(k, kn, rope), k_ctx], axis=1)
    v_all = jnp.concatenate([vh(v), v_ctx], axis=1)
    y = finish(diff_attend(heads(q, qn, rope), k_all, v_all, lam))
    yc = finish(diff_attend(heads(qc, qn, None), k_ctx, v_ctx, lam)) if need_ctx else None
    return y, yc


def short_conv(x, w):
    ch = x.shape[-1]
    pad = GDN_CONV // 2
    return lax.conv_general_dilated(x, w[:, None, :].astype(x.dtype), window_strides=(1,),
                                    padding=[(pad, pad)], dimension_numbers=('NWC', 'WIO', 'NWC'),
                                    feature_group_count=ch)


def gdn_inputs(q, k, v, bt, a, conv_w, a_log, dt_bias):
    b, n = q.shape[:2]
    qkv = jax.nn.silu(short_conv(jnp.concatenate([q, k, v], axis=-1), conv_w))
    q, k, v = jnp.split(qkv, [C_QK, 2 * C_QK], axis=-1)
    q = l2_norm(q.reshape(b, n, GDN_HEADS, GDN_DK)) * (GDN_DK ** -0.5)
    k = l2_norm(k.reshape(b, n, GDN_HEADS, GDN_DK))
    v = v.reshape(b, n, GDN_HEADS, GDN_DV).astype(jnp.float32)
    beta = jax.nn.sigmoid(bt.astype(jnp.float32).reshape(b, n, 2, GDN_HEADS))
    g = -jnp.exp(a_log.astype(jnp.float32)) * jax.nn.softplus(
        a.astype(jnp.float32).reshape(b, n, 2, GDN_HEADS) + dt_bias.astype(jnp.float32))
    return q, k, v, g, beta


def gdn_chunked(q, k, v, g, beta, s0):
    b, t, h, dk = q.shape
    dv = v.shape[-1]
    nc, L = t // GDN_CHUNK, GDN_CHUNK

    def to_chunks(x):
        x = x.reshape((b, nc, L) + x.shape[2:])
        return jnp.moveaxis(x, 2, 3)

    q, k, v, g, beta = (to_chunks(z) for z in (q, k, v, g, beta))
    gc = jnp.cumsum(g, axis=-1)
    tril = jnp.tril(jnp.ones((L, L), dtype=bool))
    diff = gc[..., :, None] - gc[..., None, :]
    decay = jnp.where(tril, jnp.exp(jnp.where(tril, diff, 0.0)), 0.0)
    kb = k * beta[..., None]
    a_mat = jnp.tril(jnp.einsum('bnhid,bnhjd->bnhij', kb, k) * decay, -1)
    eye = jnp.eye(L, dtype=a_mat.dtype)
    rhs = jnp.concatenate([v * beta[..., None], kb * jnp.exp(gc)[..., None]], axis=-1)
    sol = lax.linalg.triangular_solve(a_mat + eye, rhs, left_side=True, lower=True, unit_diagonal=True)
    u, w = sol[..., :dv], sol[..., dv:]
    qk = jnp.einsum('bnhid,bnhjd->bnhij', q, k) * decay
    q_dec = q * jnp.exp(gc)[..., None]
    k_dec = k * jnp.exp(gc[..., -1:] - gc)[..., None]
    g_last = jnp.exp(gc[..., -1])

    def step(s, xs):
        qd, kd, ui, wi, qki, gl = xs
        v_new = ui - jnp.einsum('bhlk,bhkv->bhlv', wi, s)
        o = jnp.einsum('bhlk,bhkv->bhlv', qd, s) + jnp.einsum('bhij,bhjv->bhiv', qki, v_new)
        s = s * gl[..., None, None] + jnp.einsum('bhlk,bhlv->bhkv', kd, v_new)
        return s, o

    xs = tuple(jnp.moveaxis(z, 1, 0) for z in (q_dec, k_dec, u, w, qk, g_last))
    s_fin, o = lax.scan(step, s0, xs)
    o = jnp.transpose(o, (1, 0, 3, 2, 4)).reshape(b, t, h, dv)
    return o, s_fin


def gdn_branch(lat, ctx, conv_w, a_log, dt_bias, norm_w):
    q, k, v, z, bt, a = lat
    qc, kc, vc, zc, btc, ac = ctx
    b, n = q.shape[:2]
    li = gdn_inputs(q, k, v, bt, a, conv_w, a_log, dt_bias)
    ci = gdn_inputs(qc, kc, vc, btc, ac, conv_w, a_log, dt_bias)
    s0 = jnp.zeros((b, GDN_HEADS, GDN_DK, GDN_DV), jnp.float32)
    o_lat = jnp.zeros((b, n, GDN_HEADS, GDN_DV), jnp.float32)
    o_ctx = jnp.zeros((b, qc.shape[1], GDN_HEADS, GDN_DV), jnp.float32)
    for d in range(2):
        def orient(t):
            return jnp.flip(t, axis=1) if d == 1 else t

        def args(ins):
            qq, kk, vv, gg, bb = ins
            return orient(qq), orient(kk), orient(vv), orient(gg[:, :, d]), orient(bb[:, :, d])

        oc, s_ctx = gdn_chunked(*args(ci), s0)
        ol, _ = gdn_chunked(*args(li), s_ctx)
        o_lat = o_lat + orient(ol)
        o_ctx = o_ctx + orient(oc)

    def finish(o, zz):
        zz = zz.reshape(o.shape).astype(jnp.float32)
        return (rms_norm(o, norm_w) * jax.nn.silu(zz)).reshape(o.shape[0], o.shape[1], C_V).astype(q.dtype)

    return finish(o_lat, z), finish(o_ctx, zc)


def gated_merge(h, ys, w_mgate, w_branch, w_o):
    merged = jnp.zeros(h.shape, h.dtype)
    for i, y in enumerate(ys):
        gate = jax.nn.sigmoid(jnp.einsum('bnd,de->bne', h, w_mgate[i]))
        merged = merged + gate * jnp.einsum('bnw,wd->bnd', y, w_branch[i])
    return jnp.einsum('bnd,de->bne', merged, w_o)


def hybrid_mixer(h, hc, rope, w_in, gqa_qn, gqa_kn, diff_qn, diff_kn, lam, lam_init, diff_subln,
                 conv_w, a_log, dt_bias, gdn_norm_w, w_mgate, w_branch, w_o, need_ctx):
    splits = [int(s) for s in np.cumsum(IN_WIDTHS)[:-1]]
    p = jnp.split(jnp.einsum('bnd,dc->bnc', h, w_in), splits, axis=-1)
    pc = jnp.split(jnp.einsum('bnd,dc->bnc', hc, w_in), splits, axis=-1)
    ya, yac = gqa_branch(p[0:3], pc[0:3], gqa_qn, gqa_kn, rope, need_ctx)
    yb, ybc = diff_branch(p[3:6], pc[3:6], diff_qn, diff_kn, lam, lam_init, diff_subln, rope, need_ctx)
    yg, ygc = gdn_branch(p[6:12], pc[6:12], conv_w, a_log, dt_bias, gdn_norm_w)
    y = gated_merge(h, (ya, yb, yg), w_mgate, w_branch, w_o)
    yc = gated_merge(hc, (yac, ybc, ygc), w_mgate, w_branch, w_o) if need_ctx else None
    return y, yc


def expert_choice_ffn(h, w_router, w_gate, w_up, w_down):
    b, n, d = h.shape
    cap = CAPACITY_FACTOR * n // N_EXPERTS
    logits = jnp.einsum('bnd,de->ben', h, w_router, preferred_element_type=jnp.float32)
    aff = jax.nn.softmax(logits, axis=1)
    top_w, top_i = lax.top_k(aff, cap)
    xin = jax.vmap(lambda hb, ib: hb[ib])(h, top_i)
    hid = jax.nn.silu(jnp.einsum('becd,edf->becf', xin, w_gate)) * jnp.einsum('becd,edf->becf', xin, w_up)
    y = jnp.einsum('becf,efd->becd', hid, w_down) * top_w[..., None].astype(h.dtype)

    def scatter(ib, yb):
        return jnp.zeros((n, d), y.dtype).at[ib.reshape(-1)].add(yb.reshape(-1, d))

    return jax.vmap(scatter)(top_i, y)


def setup_inputs(seed: int = 0) -> dict:
    key = jax.random.key(seed)
    ks = jax.random.split(key, 32)
    D = D_MODEL
    f32 = jnp.float32

    def nrm(i, shape, scale):
        return jax.random.normal(ks[i], shape, f32) * scale

    def gain(i, shape):
        return 1.0 + 0.05 * jax.random.normal(ks[i], shape, f32)

    dt = jnp.exp(jax.random.uniform(ks[20], (DEPTH, 2, GDN_HEADS), f32, math.log(1e-3), math.log(1e-1)))
    return {
        'x': nrm(0, (BATCH, SEQ, D), 1.0),
        'c': nrm(1, (BATCH, D), 1.0),
        'ctx': nrm(2, (BATCH, CTX_LEN, D), 1.0),
        'c_ctx': nrm(3, (D,), 1.0),
        'w_mod': nrm(4, (DEPTH, D, 6 * D), 0.5 * D ** -0.5),
        'b_mod': nrm(5, (DEPTH, 6 * D), 0.01),
        'norm1_w': gain(6, (DEPTH, D)),
        'norm2_w': gain(7, (DEPTH, D)),
        'w_in': nrm(8, (DEPTH, D, IN_COLS), D ** -0.5),
        'gqa_q_norm': gain(9, (DEPTH, HEAD_DIM)),
        'gqa_k_norm': gain(10, (DEPTH, HEAD_DIM)),
        'diff_q_norm': gain(11, (DEPTH, DIFF_DIM)),
        'diff_k_norm': gain(12, (DEPTH, DIFF_DIM)),
        'diff_lambda_q1': nrm(13, (DEPTH, DIFF_DIM), 0.1),
        'diff_lambda_k1': nrm(14, (DEPTH, DIFF_DIM), 0.1),
        'diff_lambda_q2': nrm(15, (DEPTH, DIFF_DIM), 0.1),
        'diff_lambda_k2': nrm(16, (DEPTH, DIFF_DIM), 0.1),
        'diff_subln': gain(17, (DEPTH, DIFF_VDIM)),
        'gdn_conv_w': nrm(18, (DEPTH, GDN_CONV, GDN_CONV_CH), GDN_CONV ** -0.5),
        'gdn_a_log': jnp.log(jax.random.uniform(ks[19], (DEPTH, 2, GDN_HEADS), f32, 1.0, 16.0)),
        'gdn_dt_bias': dt + jnp.log(-jnp.expm1(-dt)),
        'gdn_norm_w': gain(21, (DEPTH, GDN_DV)),
        'w_merge_gate': nrm(22, (DEPTH, N_BRANCH, D, D), D ** -0.5),
        'w_branch': nrm(23, (DEPTH, N_BRANCH, BRANCH_W, D), BRANCH_W ** -0.5),
        'w_out': nrm(24, (DEPTH, D, D), D ** -0.5),
        'w_router': nrm(25, (DEPTH, D, N_EXPERTS), D ** -0.5),
        'w_exp_gate': nrm(26, (DEPTH, N_EXPERTS, D, EXPERT_FF), D ** -0.5),
        'w_exp_up': nrm(27, (DEPTH, N_EXPERTS, D, EXPERT_FF), D ** -0.5),
        'w_exp_down': nrm(28, (DEPTH, N_EXPERTS, EXPERT_FF, D), EXPERT_FF ** -0.5),
    }


def reference(x, c, ctx, c_ctx, w_mod, b_mod, norm1_w, norm2_w, w_in, gqa_q_norm, gqa_k_norm,
              diff_q_norm, diff_k_norm, diff_lambda_q1, diff_lambda_k1, diff_lambda_q2, diff_lambda_k2,
              diff_subln, gdn_conv_w, gdn_a_log, gdn_dt_bias, gdn_norm_w, w_merge_gate, w_branch, w_out,
              w_router, w_exp_gate, w_exp_up, w_exp_down):
    n = x.shape[1]
    rope = axial_rope(n, HEAD_DIM)
    c_act = jax.nn.silu(c)
    cc_act = jax.nn.silu(c_ctx)
    xc = ctx
    for li in range(DEPTH):
        last = li == DEPTH - 1
        mod = (c_act @ w_mod[li] + b_mod[li])[:, None, :]
        modc = cc_act @ w_mod[li] + b_mod[li]
        sh1, sc1, g1, sh2, sc2, g2 = jnp.split(mod, 6, axis=-1)
        sh1c, sc1c, g1c, sh2c, sc2c, g2c = jnp.split(modc, 6, axis=-1)
        lam_init = lambda_init(li)
        lam = (jnp.exp(jnp.sum(diff_lambda_q1[li] * diff_lambda_k1[li]).astype(jnp.float32))
               - jnp.exp(jnp.sum(diff_lambda_q2[li] * diff_lambda_k2[li]).astype(jnp.float32)) + lam_init)

        h = rms_norm(x, norm1_w[li]) * (1.0 + sc1) + sh1
        hc = rms_norm(xc, norm1_w[li]) * (1.0 + sc1c) + sh1c
        y, yc = hybrid_mixer(h, hc, rope, w_in[li], gqa_q_norm[li], gqa_k_norm[li], diff_q_norm[li],
                             diff_k_norm[li], lam, lam_init, diff_subln[li], gdn_conv_w[li], gdn_a_log[li],
                             gdn_dt_bias[li], gdn_norm_w[li], w_merge_gate[li], w_branch[li], w_out[li],
                             not last)
        x = x + g1 * y
        h2 = rms_norm(x, norm2_w[li]) * (1.0 + sc2) + sh2
        x = x + g2 * expert_choice_ffn(h2, w_router[li], w_exp_gate[li], w_exp_up[li], w_exp_down[li])
        if not last:
            xc = xc + g1c * yc
            h2c = rms_norm(xc, norm2_w[li]) * (1.0 + sc2c) + sh2c
            xc = xc + g2c * expert_choice_ffn(h2c, w_router[li], w_exp_gate[li], w_exp_up[li], w_exp_down[li])
    return x
```

```python
import math
from contextlib import ExitStack

import numpy as np
import concourse.bass as bass
import concourse.mybir as mybir
from concourse.bass_utils import run_bass_kernel_spmd

F32 = mybir.dt.float32
BF16 = mybir.dt.bfloat16
I32 = mybir.dt.int32
AF = mybir.ActivationFunctionType
ALU = mybir.AluOpType
AX = mybir.AxisListType

D = 1024
DEPTH = 2
NEXP = 16
IN_COLS = 4368
EPS = 1e-6

COMPUTE = ('pe', 'act', 'dve', 'pool')
QUEUES = ('sp', 'act', 'pool')
EPOCH = 30000
NS = 8


class Sched:
    def __init__(self):
        self.stream = {e: [] for e in ('pe', 'act', 'dve', 'pool', 'sp')}
        self.ncomp = {e: 0 for e in COMPUTE}
        self.ndma = {q: 0 for q in QUEUES}
        self.lastw = {}
        self.readers = {}
        self.waited = {}
        self.semkeys = set()

    def _need(self, eng, tok):
        semkey, val = tok
        if semkey[0] == 'c':
            if semkey[1] == 'pe' and eng == 'pe':
                return
            g = semkey[2] * EPOCH + val
            k = (eng, 'c', semkey[1])
            if self.waited.get(k, 0) >= g:
                return
            self.waited[k] = g
        else:
            k = (eng, semkey)
            if self.waited.get(k, 0) >= val:
                return
            self.waited[k] = val
        self.stream[eng].append(('wait', semkey, val))

    def _deps(self, eng, reads, writes):
        for r in reads:
            t = self.lastw.get(r)
            if t is not None:
                self._need(eng, t)
        for w in writes:
            t = self.lastw.get(w)
            if t is not None:
                self._need(eng, t)
            rd = self.readers.get(w)
            if rd:
                for sk, v in list(rd.items()):
                    self._need(eng, (sk, v))

    def _record(self, tok, reads, writes):
        for w in writes:
            self.lastw[w] = tok
            self.readers[w] = {}
        sk, v = tok
        for r in reads:
            if r in writes:
                continue
            d = self.readers.setdefault(r, {})
            if sk[0] == 'c':
                for old in [o for o in d if o[0] == 'c' and o[1] == sk[1]]:
                    del d[old]
                d[sk] = v
            else:
                d[sk] = max(d.get(sk, 0), v)

    def op(self, eng, fn, reads=(), writes=()):
        reads = tuple(reads)
        writes = tuple(writes)
        self._deps(eng, reads, writes)
        seq = self.ncomp[eng]
        self.ncomp[eng] += 1
        semkey = ('c', eng, seq // EPOCH)
        tok = (semkey, seq % EPOCH + 1)
        self.semkeys.add(semkey)
        self.stream[eng].append(('op', fn, semkey))
        self._record(tok, reads, writes)
        return tok

    def dma(self, q, fn, reads=(), writes=()):
        reads = tuple(reads)
        writes = tuple(writes)
        k = self.ndma[q]
        self.ndma[q] += 1
        slot = k % NS
        semkey = ('d', q, slot)
        self.semkeys.add(semkey)
        if k >= NS:
            self._need(q, (semkey, 16 * (k // NS)))
        self._deps(q, reads, writes)
        tok = (semkey, 16 * (k // NS + 1))
        self.stream[q].append(('dma', fn, semkey))
        self._record(tok, reads, writes)
        return tok

    def _all_tokens(self):
        toks = []
        for q in QUEUES:
            n = self.ndma[q]
            for k in range(max(0, n - NS), n):
                toks.append((('d', q, k % NS), 16 * (k // NS + 1)))
        for e in COMPUTE:
            n = self.ncomp[e]
            if n:
                toks.append((('c', e, (n - 1) // EPOCH), (n - 1) % EPOCH + 1))
        return toks

    def barrier(self):
        toks = self._all_tokens()
        for e in ('pe', 'act', 'dve', 'pool', 'sp'):
            for t in toks:
                if e == 'pe' and t[0][0] == 'c' and t[0][1] == 'pe':
                    continue
                self._need(e, t)
        self.lastw.clear()
        self.readers.clear()

    def finish(self):
        for t in self._all_tokens():
            self._need('sp', t)

    def emit(self, nc, stack):
        sems = {}
        for sk in sorted(self.semkeys):
            sems[sk] = stack.enter_context(nc.semaphore("s_" + "_".join(str(x) for x in sk)))
        block = stack.enter_context(nc.Block())
        streams = self.stream

        def run(eng, name):
            for item in streams[name]:
                if item[0] == 'wait':
                    eng.wait_ge(sems[item[1]], item[2])
                elif item[0] == 'op':
                    item[1](eng).then_inc(sems[item[2]], 1)
                else:
                    item[1](eng).then_inc(sems[item[2]], 16)

        @block.tensor
        def _(e):
            run(e, 'pe')

        @block.scalar
        def _(e):
            run(e, 'act')

        @block.vector
        def _(e):
            run(e, 'dve')

        @block.gpsimd
        def _(e):
            run(e, 'pool')

        @block.sync
        def _(e):
            run(e, 'sp')


class Buf:
    __slots__ = ('name', 'ap')

    def __init__(self, name, ap):
        self.name = name
        self.ap = ap

    def __getitem__(self, k):
        return self.ap[k]

    def __repr__(self):
        return self.name


_DTSIZE = {F32: 4, BF16: 2, I32: 4}


class Arena:
    def __init__(self, nc, stack, kib):
        self.words = kib * 256
        self.t = stack.enter_context(nc.sbuf_tensor("arena", [128, self.words], F32))
        self.off = 0
        self.n = 0

    def mark(self):
        return self.off

    def reset(self, m):
        self.off = m

    def alloc(self, name, shape, dt=F32):
        p = shape[0]
        free = int(np.prod(shape[1:]))
        words = (free * _DTSIZE[dt] + 3) // 4
        words = (words + 7) // 8 * 8
        assert self.off + words <= self.words, f"arena overflow at {name}: {self.off}+{words}>{self.words}"
        ap = self.t[0:p, self.off:self.off + words]
        self.off += words
        if dt != F32:
            ap = ap.bitcast(dt)
        ap = ap[:, 0:free]
        if len(shape) == 3:
            ap = ap.rearrange("p (a b) -> p a b", a=shape[1])
        elif len(shape) == 4:
            ap = ap.rearrange("p (a b c) -> p a b c", a=shape[1], b=shape[2])
        self.n += 1
        return Buf(f"{name}#{self.n}", ap)


SRC_RANGES = [(0, 512), (512, 640), (768, 1280), (1280, 1792),
              (640, 768), (1792, 2304),
              (3840, 4352),
              (4352, 4368),
              (2304, 3840)]
O_NK, O_V, O_Z, O_GA, O_C = 0, 1664, 2304, 2816, 2832
NSUB = 26


class K:
    pass


def build(N=8192, CTX=256, nlayers=DEPTH, stop_after=None, debug=(), nexp_decl=NEXP):
    T = CTX + N
    nc = bass.Bass("TRN2", target_bir_lowering=False)
    k = K()
    k.nc, k.N, k.CTX, k.T = nc, N, CTX, T
    k.S = S = Sched()
    global LASTS
    LASTS = S

    def din(name, shape, dt=F32):
        return nc.dram_tensor(name, list(shape), dt, kind="ExternalInput").ap()

    def dscr(name, shape, dt=F32):
        kind = "ExternalOutput" if name in debug else "Internal"
        return Buf(name, nc.dram_tensor(name, list(shape), dt, kind=kind).ap())

    I = {}
    I['x'] = din('x', [N, D])
    I['c'] = din('c', [D])
    I['ctx'] = din('ctx', [CTX, D])
    I['c_ctx'] = din('c_ctx', [D])
    I['w_mod'] = din('w_mod', [DEPTH, D, 6 * D])
    I['b_mod'] = din('b_mod', [DEPTH, 6 * D])
    I['norm1_w'] = din('norm1_w', [DEPTH, D])
    I['norm2_w'] = din('norm2_w', [DEPTH, D])
    I['w_in'] = din('w_in', [DEPTH, D, IN_COLS])
    for nm in ('gqa_q_norm', 'gqa_k_norm', 'diff_q_norm', 'diff_k_norm', 'diff_lambda_q1', 'diff_lambda_k1',
               'diff_lambda_q2', 'diff_lambda_k2'):
        I[nm] = din(nm, [DEPTH, 64])
    I['diff_subln'] = din('diff_subln', [DEPTH, 128])
    I['gdn_conv_w'] = din('gdn_conv_w', [DEPTH, 3, 1536])
    I['gdn_a_log'] = din('gdn_a_log', [DEPTH, 8])
    I['gdn_dt_bias'] = din('gdn_dt_bias', [DEPTH, 8])
    I['gdn_norm_w'] = din('gdn_norm_w', [DEPTH, 128])
    I['w_merge_gate'] = din('w_merge_gate', [DEPTH, 3, D, D])
    I['w_branch'] = din('w_branch', [DEPTH, 3, 512, D])
    I['w_out'] = din('w_out', [DEPTH, D, D])
    I['w_router'] = din('w_router', [DEPTH, D, NEXP])
    I['w_exp_gate'] = din('w_exp_gate', [DEPTH, nexp_decl, D, D])
    I['w_exp_up'] = din('w_exp_up', [DEPTH, nexp_decl, D, D])
    I['w_exp_down'] = din('w_exp_down', [DEPTH, nexp_decl, D, D])
    I['ident'] = din('ident', [128, 128])
    I['ropeC'] = din('ropeC', [N, 64])
    I['ropeS'] = din('ropeS', [N, 64])
    I['ustrict'] = din('ustrict', [128, 128])
    I['gdnc'] = din('gdnc', [2, 128, 5, 64])
    k.I = I
    out = nc.dram_tensor('out', [N, D], F32, kind="ExternalOutput").ap()
    k.out = Buf('out', out)

    k.xs = [dscr(f'xs{i}', [T, D]) for i in range(2)]
    k.hT_d = dscr('hT_d', [128, 8, T], BF16)
    k.qkT_d = dscr('qkT_d', [NSUB, 64, T], BF16)
    k.v_d = dscr('v_d', [T, 640], BF16)
    k.z_d = dscr('z_d', [T, 512], BF16)
    k.gb_d = dscr('gb_d', [T, 16])
    k.cT_d = dscr('cT_d', [12, 128, T])
    k.yT_d = dscr('yT_d', [3, 512, T], BF16)
    k.cP_d = dscr('cP_d', [12, 128, T])
    k.of_d = dscr('of_d', [T, 512])
    k.xn_d = dscr('xn_d', [T, D], BF16)
    SL = N // 8 + CTX // 8
    k.xin_d = [dscr(f'xin_d{e_}', [SL, D], BF16) for e_ in range(NEXP)]
    k.yexp_d = [dscr(f'yexp_d{e_}', [SL, D]) for e_ in range(NEXP)]

    with ExitStack() as st:
        k.st = st
        k.ar = Arena(nc, st, 200)
        k.banks = [Buf(f'bank{i}', st.enter_context(nc.psum_tensor(f'bank{i}', [128, 512], F32))[:]) for i in range(8)]
        _program(k, nlayers, stop_after)
        S.finish()
        S.emit(nc, st)
    return nc


def bank_view(b, dt, shape):
    ap = b.ap
    if dt != F32:
        ap = ap.bitcast(dt)
    p = shape[0]
    free = int(np.prod(shape[1:]))
    ap = ap[0:p, 0:free]
    if len(shape) == 3:
        ap = ap.rearrange("p (a b) -> p a b", a=shape[1])
    return ap


def _program(k, nlayers, stop_after):
    S, ar, I = k.S, k.ar, k.I
    k.identf = ar.alloc('identf', [128, 128], F32)
    k.identb = ar.alloc('identb', [128, 128], BF16)
    k.ones_f = ar.alloc('ones_f', [128, 128], F32)
    k.ones_b = ar.alloc('ones_b', [128, 128], BF16)
    k.epst = ar.alloc('epst', [128, 1], F32)
    S.dma('sp', lambda e: e.dma_start(out=k.identf.ap, in_=I['ident']), writes=[k.identf])
    S.op('dve', lambda e: e.tensor_copy(out=k.identb.ap, in_=k.identf.ap), reads=[k.identf], writes=[k.identb])
    S.op('pool', lambda e: e.memset(k.ones_f.ap, 1.0), writes=[k.ones_f])
    S.op('pool', lambda e: e.memset(k.ones_b.ap, 1.0), writes=[k.ones_b])
    S.op('pool', lambda e: e.memset(k.epst.ap, EPS), writes=[k.epst])
    k.modb = [[(ar.alloc(f'modb{j}{i}', [128, D], F32) if i in (2, 5) else None) for i in range(6)] for j in range(2)]
    base = ar.mark()
    for li in range(nlayers):
        last = li == DEPTH - 1
        xin_lat = Buf('x_in', I['x']) if li == 0 else None
        ar.reset(base)
        phase0_mod(k, li)
        S.barrier()
        ar.reset(base)
        phase1_proj(k, li)
        S.barrier()
        if stop_after == 'p1':
            return
        ar.reset(base)
        phase2_attn(k, li, not last)
        S.barrier()
        if stop_after == 'p2':
            return
        ar.reset(base)
        phase3_gdn(k, li, not last)
        S.barrier()
        if stop_after == 'p3':
            return
        ar.reset(base)
        phase4_merge(k, li, not last)
        S.barrier()
        if stop_after == 'p4':
            return
        ar.reset(base)
        phase5_moe(k, li, not last)
        S.barrier()


def _cp(S, eng, out_b, out_ap, in_b, in_ap):
    if eng == 'act':
        S.op('act', lambda e: e.copy(out=out_ap, in_=in_ap), reads=[in_b], writes=[out_b])
    else:
        S.op(eng, lambda e: e.tensor_copy(out=out_ap, in_=in_ap), reads=[in_b], writes=[out_b])


def phase0_mod(k, li):
    S, ar, I = k.S, k.ar, k.I
    bk = k.banks
    vrow = ar.alloc('vrow', [128, 128], F32)
    S.dma('sp', lambda e: e.dma_start(out=vrow.ap[0:8, :], in_=I['c'].rearrange("(c p) -> c p", p=128)), writes=[vrow])
    S.dma('sp', lambda e: e.dma_start(out=vrow.ap[8:16, :], in_=I['c_ctx'].rearrange("(c p) -> c p", p=128)), writes=[vrow])
    S.dma('sp', lambda e: e.dma_start(out=vrow.ap[16:24, :], in_=I['norm1_w'][li].rearrange("(c p) -> c p", p=128)), writes=[vrow])
    S.dma('sp', lambda e: e.dma_start(out=vrow.ap[24:32, :], in_=I['norm2_w'][li].rearrange("(c p) -> c p", p=128)), writes=[vrow])
    S.dma('sp', lambda e: e.dma_start(out=vrow.ap[32:80, :], in_=I['b_mod'][li].rearrange("(c p) -> c p", p=128)), writes=[vrow])
    vcol = ar.alloc('vcol', [128, 80], F32)
    pv = bank_view(bk[0], F32, [128, 80])
    S.op('pe', lambda e: e.transpose(out=pv, in_=vrow.ap[0:80, :], identity=k.identf.ap[0:80, 0:80]),
         reads=[vrow, k.identf], writes=[bk[0]])
    _cp(S, 'dve', vcol, vcol.ap, bk[0], pv)
    cact = ar.alloc('cact', [128, 8, 2], F32)
    for j in range(2):
        S.op('act', lambda e, j=j: e.activation(out=cact.ap[:, :, j], in_=vcol.ap[:, j * 8:(j + 1) * 8], func=AF.Silu),
             reads=[vcol], writes=[cact])
    brow = ar.alloc('brow', [1, 6 * D], F32)
    S.dma('sp', lambda e: e.dma_start(out=brow.ap, in_=I['b_mod'][li:li + 1, :]), writes=[brow])
    modcol = ar.alloc('modcol', [128, 48, 2], F32)
    wst = [ar.alloc(f'wst{i}', [128, 8, 512], F32) for i in range(2)]
    grow = [ar.alloc(f'grow{i}', [1, 512], F32) for i in range(2)]
    k.A = [[None] * 2 for _ in range(2)]
    wsrc = I['w_mod'][li].rearrange("(c p) n -> p c n", p=128)
    nb = 2
    for n in range(12):
        w = wst[n % 2]
        S.dma('sp', lambda e, w=w, n=n: e.dma_start(out=w.ap, in_=wsrc[:, :, n * 512:(n + 1) * 512]), writes=[w])
        split = n // 2
        if split in (2, 5):
            gi = 2 if split == 2 else 5
            for j in range(2):
                pb = bk[nb % 8]; nb += 1
                for c in range(8):
                    S.op('pe', lambda e, pb=pb, w=w, c=c, j=j: e.matmul(pb.ap[0:1, :], lhsT=cact.ap[:, c, j:j + 1], rhs=w.ap[:, c, :],
                                                                      start=(c == 0), stop=(c == 7)),
                         reads=[cact, w], writes=[pb])
                g = grow[j]
                S.op('dve', lambda e, pb=pb, g=g, n=n: e.tensor_tensor(out=g.ap, in0=pb.ap[0:1, :], in1=brow.ap[:, n * 512:(n + 1) * 512], op=ALU.add),
                     reads=[pb, brow], writes=[g])
                pb2 = bk[nb % 8]; nb += 1
                S.op('pe', lambda e, pb2=pb2, g=g: e.matmul(pb2.ap, lhsT=k.ones_f.ap[0:1, :], rhs=g.ap, start=True, stop=True),
                     reads=[k.ones_f, g], writes=[pb2])
                dst = k.modb[j][gi]
                half = n % 2
                _cp(S, 'act', dst, dst.ap[:, half * 512:(half + 1) * 512], pb2, pb2.ap)
        else:
            pb = bk[nb % 8]; nb += 1
            pvv = bank_view(pb, F32, [128, 4, 2])
            for sub in range(4):
                for c in range(8):
                    S.op('pe', lambda e, pvv=pvv, w=w, c=c, sub=sub: e.matmul(pvv[:, sub, :], lhsT=w.ap[:, c, sub * 128:(sub + 1) * 128], rhs=cact.ap[:, c, :],
                                                                            start=(c == 0), stop=(c == 7)),
                         reads=[cact, w], writes=[pb])
            S.op('dve', lambda e, pvv=pvv, n=n: e.tensor_tensor(out=modcol.ap[:, n * 4:(n + 1) * 4, :], in0=pvv,
                                                              in1=vcol.ap[:, 32 + n * 4:32 + (n + 1) * 4].unsqueeze(2).broadcast_to([128, 4, 2]), op=ALU.add),
                 reads=[pb, vcol], writes=[modcol])
    k.colA1 = ar_persist(k, 'colA1', [128, 8, 2]); k.colB1 = ar_persist(k, 'colB1', [128, 8, 2])
    k.colA2 = ar_persist(k, 'colA2', [128, 8, 2]); k.colB2 = ar_persist(k, 'colB2', [128, 8, 2])
    for (dstA, dstB, sh, sc, nw) in ((k.colA1, k.colB1, 0, 1, 16), (k.colA2, k.colB2, 3, 4, 24)):
        S.op('dve', lambda e, dstA=dstA, sc=sc, nw=nw: e.scalar_tensor_tensor(
            out=dstA.ap, in0=modcol.ap[:, sc * 8:(sc + 1) * 8, :], scalar=1.0,
            in1=vcol.ap[:, nw:nw + 8].unsqueeze(2).broadcast_to([128, 8, 2]), op0=ALU.add, op1=ALU.mult),
            reads=[modcol, vcol], writes=[dstA])
        S.op('dve', lambda e, dstB=dstB, sh=sh: e.tensor_copy(out=dstB.ap, in_=modcol.ap[:, sh * 8:(sh + 1) * 8, :]),
             reads=[modcol], writes=[dstB])


def ar_persist(k, name, shape, dt=F32):
    if not hasattr(k, '_persist'):
        k._persist = {}
    if name not in k._persist:
        t = k.st.enter_context(k.nc.sbuf_tensor("P_" + name, list(shape), dt))
        k._persist[name] = Buf("P_" + name, t[:])
    return k._persist[name]


def _spans(k):
    sp = [(0, k.CTX, True)]
    for i in range(k.N // 512):
        sp.append((k.CTX + i * 512, 512, False))
    return sp


def _xsrc(k, li, tok0):
    if li == 0:
        if tok0 < k.CTX:
            return k.I['ctx'][tok0:tok0 + 128, :], None
        return k.I['x'][tok0 - k.CTX:tok0 - k.CTX + 128, :], None
    b = k.xs[1]
    return b.ap[tok0:tok0 + 128, :], b


def _norm_tile(k, src_ap, src_res, xt, sqs, ss, rstd):
    S = k.S
    S.dma('sp', lambda e: e.dma_start(out=xt.ap, in_=src_ap), reads=[src_res] if src_res else [], writes=[xt])
    S.op('act', lambda e: e.activation(out=sqs.ap.rearrange('p a b -> p (a b)')[:, 0:1024] if len(sqs.ap.shape) == 3 else sqs.ap, in_=xt.ap, func=AF.Square, scale=1.0 / 32, accum_out=ss.ap),
         reads=[xt], writes=[sqs, ss])
    S.op('act', lambda e: e.activation(out=rstd.ap, in_=ss.ap, func=AF.Sqrt, bias=k.epst.ap, scale=1.0), reads=[ss, k.epst], writes=[rstd])
    S.op('dve', lambda e: e.reciprocal(out=rstd.ap, in_=rstd.ap), reads=[rstd], writes=[rstd])


def phase1_proj(k, li):
    S, ar, I = k.S, k.ar, k.I
    bk = k.banks
    N, CTX, T = k.N, k.CTX, k.T
    wb = ar.alloc('wb', [128, 8, IN_COLS], BF16)
    wst = [ar.alloc(f'w1st{i}', [128, 8, 256], F32) for i in range(2)]
    wsrc = I['w_in'][li].rearrange("(c p) n -> p c n", p=128)
    pieces = []
    dst = 0
    for (a, b) in SRC_RANGES:
        s = a
        while s < b:
            wd = min(256, b - s)
            pieces.append((s, dst, wd))
            s += wd
            dst += wd
    for i, (s0, d0, wd) in enumerate(pieces):
        w = wst[i % 2]
        S.dma('sp', lambda e, w=w, s0=s0, wd=wd: e.dma_start(out=w.ap[:, :, 0:wd], in_=wsrc[:, :, s0:s0 + wd]), writes=[w])
        S.op(('pool', 'dve')[i % 2], lambda e, w=w, d0=d0, wd=wd: e.tensor_copy(out=wb.ap[:, :, d0:d0 + wd], in_=w.ap[:, :, 0:wd]),
             reads=[w], writes=[wb])
    nkw = ar.alloc('nkw', [128, NSUB, 64], F32)
    for nm, s0, n in (('gqa_q_norm', 0, 8), ('gqa_k_norm', 8, 2), ('diff_q_norm', 10, 8), ('diff_k_norm', 18, 8)):
        S.dma('sp', lambda e, nm=nm, s0=s0, n=n: e.dma_start(
            out=nkw.ap[:, s0:s0 + n, :], in_=I[nm][li].partition_broadcast(128).unsqueeze(1).broadcast_to([128, n, 64])), writes=[nkw])
    dtb = ar.alloc('dtb', [128, 8], F32)
    nega = ar.alloc('nega', [128, 8], F32)
    S.dma('sp', lambda e: e.dma_start(out=dtb.ap, in_=I['gdn_dt_bias'][li].partition_broadcast(128)), writes=[dtb])
    S.dma('sp', lambda e: e.dma_start(out=nega.ap, in_=I['gdn_a_log'][li].partition_broadcast(128)), writes=[nega])
    S.op('act', lambda e: e.activation(out=nega.ap, in_=nega.ap, func=AF.Exp), reads=[nega], writes=[nega])
    S.op('dve', lambda e: e.tensor_scalar(out=nega.ap, in0=nega.ap, scalar1=-1.0, scalar2=None, op0=ALU.mult), reads=[nega], writes=[nega])

    xtb = [ar.alloc(f'xt{i}', [128, D], F32) for i in range(2)]
    ssb = [ar.alloc(f'ss{i}', [128, 1], F32) for i in range(2)]
    rsb = [ar.alloc(f'rstd{i}', [128, 1], F32) for i in range(2)]
    hbb = [ar.alloc(f'hb{i}', [128, D], BF16) for i in range(2)]
    hTs = ar.alloc('hTs', [128, 8, 512], BF16)
    fst = wst
    fv = lambda f: f.ap.rearrange('p a b -> p (a b)')
    nkq = ar.alloc('nkq', [128, NSUB, 64], F32)
    t1 = ar.alloc('t1', [128, NSUB, 64], F32)
    t2 = ar.alloc('t2', [128, NSUB, 64], F32)
    sqs = t2
    ssq = ar.alloc('ssq', [128, NSUB], F32)
    rcf = ar.alloc('rcf', [128, NSUB, 64], F32)
    rsf = ar.alloc('rsf', [128, NSUB, 64], F32)
    qr = ar.alloc('qr', [128, NSUB, 64], BF16)
    qkts = ar.alloc('qkts', [64, NSUB, 512], BF16)
    vst = [ar.alloc(f'vst{i}', [128, 640], BF16) for i in range(2)]
    zst = [ar.alloc(f'zst{i}', [128, 512], BF16) for i in range(2)]
    gast = [ar.alloc(f'gast{i}', [128, 16], F32) for i in range(2)]
    gtmp = ar.alloc('gtmp', [128, 8], F32)

    tcount = 0
    for (s0, ntok, is_ctx) in _spans(k):
        jj = 1 if is_ctx else 0
        ntile = ntok // 128
        for j in range(ntile):
            tok0 = s0 + j * 128
            b = tcount % 2
            tcount += 1
            xt, ss, rstd, hb = xtb[b], ssb[b], rsb[b], hbb[b]
            src_ap, src_res = _xsrc(k, li, tok0)
            _norm_tile(k, src_ap, src_res, xt, sqs, ss, rstd)
            S.op('dve', lambda e, hb=hb, xt=xt, rstd=rstd: e.tensor_scalar(out=hb.ap, in0=xt.ap, scalar1=rstd.ap, scalar2=None, op0=ALU.mult),
                 reads=[xt, rstd], writes=[hb])
            pT = bank_view(bk[7], BF16, [128, 8, 128])
            for c in range(8):
                S.op('pe', lambda e, hb=hb, c=c: e.transpose(out=pT[:, c, :], in_=hb.ap[:, c * 128:(c + 1) * 128], identity=k.identb.ap),
                     reads=[hb, k.identb], writes=[bk[7]])
            for c in range(8):
                if c % 2 == 0:
                    S.op('dve', lambda e, c=c, j=j, jj=jj: e.tensor_scalar(
                        out=hTs.ap[:, c, j * 128:(j + 1) * 128], in0=pT[:, c, :], scalar1=k.colA1.ap[:, c, jj:jj + 1],
                        scalar2=k.colB1.ap[:, c, jj:jj + 1], op0=ALU.mult, op1=ALU.add), reads=[bk[7], k.colA1, k.colB1], writes=[hTs])
                else:
                    S.op('act', lambda e, c=c, j=j, jj=jj: e.activation(
                        out=hTs.ap[:, c, j * 128:(j + 1) * 128], in_=pT[:, c, :], func=AF.Identity, scale=k.colA1.ap[:, c, jj:jj + 1],
                        bias=k.colB1.ap[:, c, jj:jj + 1]), reads=[bk[7], k.colA1, k.colB1], writes=[hTs])
        S.dma('sp', lambda e, s0=s0, ntok=ntok: e.dma_start(out=k.hT_d.ap[:, :, s0:s0 + ntok], in_=hTs.ap[:, :, 0:ntok]),
              reads=[hTs], writes=[k.hT_d])
        for cc in range(12):
            pb = bk[4 + cc % 3]
            for c in range(8):
                S.op('pe', lambda e, pb=pb, c=c, cc=cc, ntok=ntok: e.matmul(
                    pb.ap[:, 0:ntok], lhsT=wb.ap[:, c, O_C + cc * 128:O_C + (cc + 1) * 128], rhs=hTs.ap[:, c, 0:ntok],
                    start=(c == 0), stop=(c == 7)), reads=[wb, hTs], writes=[pb])
            f = fst[cc % 2]
            _cp(S, 'act' if cc % 2 else 'dve', f, fv(f)[:, 0:ntok], pb, pb.ap[:, 0:ntok])
            S.dma('sp', lambda e, f=f, cc=cc, s0=s0, ntok=ntok: e.dma_start(out=k.cT_d.ap[cc, :, s0:s0 + ntok], in_=fv(f)[:, 0:ntok]),
                  reads=[f], writes=[k.cT_d])
        for j in range(ntile):
            tok0 = s0 + j * 128
            b = j % 2
            lt = lambda c: hTs.ap[:, c, j * 128:(j + 1) * 128]
            for g, (c0, wd) in enumerate(((0, 512), (512, 512), (1024, 512), (1536, 128))):
                for c in range(8):
                    S.op('pe', lambda e, g=g, c=c, c0=c0, wd=wd, j=j: e.matmul(
                        bk[g].ap[:, 0:wd], lhsT=hTs.ap[:, c, j * 128:(j + 1) * 128], rhs=wb.ap[:, c, O_NK + c0:O_NK + c0 + wd],
                        start=(c == 0), stop=(c == 7)), reads=[wb, hTs], writes=[bk[g]])
            nkflat = nkq.ap.rearrange("p a b -> p (a b)")
            for g, (c0, wd) in enumerate(((0, 512), (512, 512), (1024, 512), (1536, 128))):
                _cp(S, 'act' if g % 2 else 'dve', nkq, nkflat[:, c0:c0 + wd], bk[g], bk[g].ap[:, 0:wd])
            for (pb, p0, c0, wd) in ((bk[4], 0, O_V, 512), (bk[5], 0, O_V + 512, 128), (bk[6], 0, O_Z, 512), (bk[5], 128, O_GA, 16)):
                for c in range(8):
                    S.op('pe', lambda e, pb=pb, p0=p0, c=c, c0=c0, wd=wd, j=j: e.matmul(
                        pb.ap[:, p0:p0 + wd], lhsT=hTs.ap[:, c, j * 128:(j + 1) * 128], rhs=wb.ap[:, c, c0:c0 + wd],
                        start=(c == 0), stop=(c == 7)), reads=[wb, hTs], writes=[pb])
            v, z, ga = vst[b], zst[b], gast[b]
            _cp(S, 'dve', v, v.ap[:, 0:512], bk[4], bk[4].ap)
            _cp(S, 'dve', v, v.ap[:, 512:640], bk[5], bk[5].ap[:, 0:128])
            S.dma('sp', lambda e, v=v, tok0=tok0: e.dma_start(out=k.v_d.ap[tok0:tok0 + 128, :], in_=v.ap), reads=[v], writes=[k.v_d])
            S.op('act', lambda e, z=z: e.activation(out=z.ap, in_=bk[6].ap, func=AF.Silu), reads=[bk[6]], writes=[z])
            S.dma('sp', lambda e, z=z, tok0=tok0: e.dma_start(out=k.z_d.ap[tok0:tok0 + 128, :], in_=z.ap), reads=[z], writes=[k.z_d])
            S.op('act', lambda e, ga=ga: e.activation(out=ga.ap[:, 0:8], in_=bk[5].ap[:, 128:136], func=AF.Sigmoid), reads=[bk[5]], writes=[ga])
            S.op('dve', lambda e: e.tensor_tensor(out=gtmp.ap, in0=bk[5].ap[:, 136:144], in1=dtb.ap, op=ALU.add), reads=[bk[5], dtb], writes=[gtmp])
            S.op('act', lambda e: e.activation(out=gtmp.ap, in_=gtmp.ap, func=AF.Exp), reads=[gtmp], writes=[gtmp])
            S.op('act', lambda e: e.activation(out=gtmp.ap, in_=gtmp.ap, func=AF.Ln, bias=1.0, scale=1.0), reads=[gtmp], writes=[gtmp])
            S.op('dve', lambda e, ga=ga: e.tensor_tensor(out=ga.ap[:, 8:16], in0=gtmp.ap, in1=nega.ap, op=ALU.mult), reads=[gtmp, nega], writes=[ga])
            S.dma('sp', lambda e, ga=ga, tok0=tok0: e.dma_start(out=k.gb_d.ap[tok0:tok0 + 128, :], in_=ga.ap), reads=[ga], writes=[k.gb_d])
            S.op('pool', lambda e: e.tensor_tensor(out=t1.ap, in0=nkq.ap, in1=nkq.ap, op=ALU.mult), reads=[nkq], writes=[t1])
            S.op('dve', lambda e: e.tensor_reduce(out=ssq.ap, in_=t1.ap, axis=AX.X, op=ALU.add), reads=[t1], writes=[ssq])
            S.op('act', lambda e: e.activation(out=ssq.ap, in_=ssq.ap, func=AF.Sqrt, bias=k.epst.ap, scale=1.0 / 64), reads=[ssq, k.epst], writes=[ssq])
            S.op('dve', lambda e: e.reciprocal(out=ssq.ap, in_=ssq.ap), reads=[ssq], writes=[ssq])
            S.op('dve', lambda e: e.tensor_tensor(out=t1.ap, in0=nkq.ap, in1=ssq.ap.unsqueeze(2).broadcast_to([128, NSUB, 64]), op=ALU.mult),
                 reads=[nkq, ssq], writes=[t1])
            if is_ctx:
                S.op('pool', lambda e: e.tensor_tensor(out=qr.ap, in0=t1.ap, in1=nkw.ap, op=ALU.mult), reads=[t1, nkw], writes=[qr])
            else:
                S.op('pool', lambda e: e.tensor_tensor(out=nkq.ap, in0=t1.ap, in1=nkw.ap, op=ALU.mult), reads=[t1, nkw], writes=[nkq])
                r0 = tok0 - CTX
                S.dma('sp', lambda e, r0=r0: e.dma_start(out=rcf.ap, in_=I['ropeC'][r0:r0 + 128, :].unsqueeze(1).broadcast_to([128, NSUB, 64])), writes=[rcf])
                S.dma('sp', lambda e, r0=r0: e.dma_start(out=rsf.ap, in_=I['ropeS'][r0:r0 + 128, :].unsqueeze(1).broadcast_to([128, NSUB, 64])), writes=[rsf])
                S.op('dve', lambda e: e.tensor_tensor(out=t1.ap, in0=nkq.ap, in1=rcf.ap, op=ALU.mult), reads=[nkq, rcf], writes=[t1])
                qv = nkq.ap.rearrange("p a (g two s) -> p (a g) two s", g=2, two=2)
                sv = rsf.ap.rearrange("p a (g two s) -> p (a g) two s", g=2, two=2)
                tv = t2.ap.rearrange("p a (g two s) -> p (a g) two s", g=2, two=2)
                S.op('pool', lambda e: e.tensor_tensor(out=tv[:, :, 0, :], in0=qv[:, :, 1, :], in1=sv[:, :, 0, :], op=ALU.mult), reads=[nkq, rsf], writes=[t2])
                S.op('pool', lambda e: e.tensor_tensor(out=tv[:, :, 1, :], in0=qv[:, :, 0, :], in1=sv[:, :, 1, :], op=ALU.mult), reads=[nkq, rsf], writes=[t2])
                S.op('dve', lambda e: e.tensor_tensor(out=qr.ap, in0=t1.ap, in1=t2.ap, op=ALU.add), reads=[t1, t2], writes=[qr])
            for b0, nb_ in ((0, 8), (8, 8), (16, 8), (24, 2)):
                pq = bank_view(bk[7], BF16, [64, 8, 128])
                for i in range(nb_):
                    S.op('pe', lambda e, i=i, b0=b0: e.transpose(out=pq[:, i, :], in_=qr.ap[:, b0 + i, :], identity=k.identb.ap),
                         reads=[qr, k.identb], writes=[bk[7]])
                _cp(S, 'act', qkts, qkts.ap[:, b0:b0 + nb_, j * 128:(j + 1) * 128], bk[7], pq[:, 0:nb_, :])
        S.dma('sp', lambda e, s0=s0, ntok=ntok: e.dma_start(out=k.qkT_d.ap[:, :, s0:s0 + ntok].rearrange("s d t -> d s t"), in_=qkts.ap[:, :, 0:ntok]),
              reads=[qkts], writes=[k.qkT_d])


def lambda_init(li):
    return 0.8 - 0.6 * math.exp(-0.3 * li)


def phase2_attn(k, li, need_ctx):
    S, ar, I = k.S, k.ar, k.I
    bk = k.banks
    N, CTX, T = k.N, k.CTX, k.T
    nkb = T // 128
    linit = lambda_init(li)
    lv = ar.alloc('lv', [128, 4, 64], F32)
    for i, nm in enumerate(('diff_lambda_q1', 'diff_lambda_k1', 'diff_lambda_q2', 'diff_lambda_k2')):
        S.dma('sp', lambda e, i=i, nm=nm: e.dma_start(out=lv.ap[:, i, :], in_=I[nm][li].partition_broadcast(128)), writes=[lv])
    lp = ar.alloc('lp', [128, 2, 64], F32)
    ls = ar.alloc('ls', [128, 2], F32)
    neglam = ar.alloc('neglam', [128, 1], F32)
    sublc = ar.alloc('sublc', [128, 1], F32)
    for i in range(2):
        S.op('dve', lambda e, i=i: e.tensor_tensor(out=lp.ap[:, i, :], in0=lv.ap[:, 2 * i, :], in1=lv.ap[:, 2 * i + 1, :], op=ALU.mult), reads=[lv], writes=[lp])
    S.op('dve', lambda e: e.tensor_reduce(out=ls.ap, in_=lp.ap, axis=AX.X, op=ALU.add), reads=[lp], writes=[ls])
    S.op('act', lambda e: e.activation(out=ls.ap, in_=ls.ap, func=AF.Exp), reads=[ls], writes=[ls])
    S.op('dve', lambda e: e.tensor_tensor(out=neglam.ap, in0=ls.ap[:, 1:2], in1=ls.ap[:, 0:1], op=ALU.subtract), reads=[ls], writes=[neglam])
    S.op('dve', lambda e: e.tensor_scalar(out=neglam.ap, in0=neglam.ap, scalar1=-linit, scalar2=None, op0=ALU.add), reads=[neglam], writes=[neglam])
    S.dma('sp', lambda e: e.dma_start(out=sublc.ap, in_=I['diff_subln'][li].rearrange("(p o) -> p o", o=1)), writes=[sublc])
    S.op('dve', lambda e: e.tensor_scalar(out=sublc.ap, in0=sublc.ap, scalar1=1.0 - linit, scalar2=None, op0=ALU.mult), reads=[sublc], writes=[sublc])

    KT = [ar.alloc(f'KT{i}', [64, 2, T], BF16) for i in range(2)]
    VT = [ar.alloc(f'VT{i}', [128, nkb, 130], BF16) for i in range(2)]
    for i in range(2):
        S.op('pool', lambda e, i=i: e.memset(VT[i].ap[:, :, 64:65], 1.0), writes=[VT[i]])
    QT = [ar.alloc(f'QT{i}', [64, 512], BF16) for i in range(3)]
    PT = [ar.alloc(f'PT{i}', [128, 512], BF16) for i in range(3)]
    rs = ar.alloc('rs', [128, 512], F32)
    bcs = ar.alloc('bcs', [128, 512], F32)
    o1 = ar.alloc('o1', [128, 512], F32)
    o2 = ar.alloc('o2', [128, 512], F32)
    sq = ar.alloc('sq', [128, 512], F32)
    yst = [ar.alloc(f'yst{i}', [128, 512], BF16) for i in range(2)]

    heads = []
    for h in range(8):
        heads.append(dict(kind='gqa', subs=[(h, 8 + h // 4)], vc0=(h // 4) * 64, dv=64, br=0, row0=h * 64))
    for h in range(4):
        heads.append(dict(kind='diff', subs=[(10 + 2 * h, 18 + 2 * h), (10 + 2 * h + 1, 18 + 2 * h + 1)], vc0=128 + h * 128, dv=128, br=1, row0=h * 128))
    chunks = [(CTX + i * 512, 512, 0, nkb) for i in range(N // 512)]
    if need_ctx:
        chunks = [(0, CTX, 0, CTX // 128)] + chunks
    cnt = dict(s=0, o=0, q=0, p=0, y=0)
    for hi, hd in enumerate(heads):
        kt, vt = KT[hi % 2], VT[hi % 2]
        dv = hd['dv']
        gqa = hd['kind'] == 'gqa'
        for ci, (qs, ks) in enumerate(hd['subs']):
            S.dma('sp', lambda e, kt=kt, ci=ci, ks=ks: e.dma_start(out=kt.ap[:, ci, :], in_=k.qkT_d.ap[ks, :, :]), reads=[k.qkT_d], writes=[kt])
        vdst = vt.ap[:, :, 0:64] if gqa else vt.ap[:, :, 0:128]
        S.dma('sp', lambda e, vdst=vdst, hd=hd, dv=dv: e.dma_start(
            out=vdst, in_=k.v_d.ap[:, hd['vc0']:hd['vc0'] + dv].rearrange("(b p) d -> p b d", p=128)), reads=[k.v_d], writes=[vt])
        if not gqa:
            pass
        elif hi > 0 and heads[hi - 2 if hi >= 2 else 0]['kind'] != 'gqa':
            pass
        for (t0, nq, kb0, kb1) in chunks:
            for ci, (qs, ks) in enumerate(hd['subs']):
                qt = QT[cnt['q'] % 3]; cnt['q'] += 1
                S.dma('sp', lambda e, qt=qt, qs=qs, t0=t0, nq=nq: e.dma_start(out=qt.ap[:, 0:nq], in_=k.qkT_d.ap[qs, :, t0:t0 + nq]),
                      reads=[k.qkT_d], writes=[qt])
                po = bk[3 + cnt['o'] % 2]
                psm = bk[5 + cnt['o'] % 2]
                cnt['o'] += 1
                M = 65 if gqa else 128
                for kb in range(kb0, kb1):
                    ps = bk[cnt['s'] % 3]; cnt['s'] += 1
                    pt = PT[cnt['p'] % 3]; cnt['p'] += 1
                    S.op('pe', lambda e, ps=ps, kt=kt, ci=ci, kb=kb, qt=qt, nq=nq: e.matmul(
                        ps.ap[:, 0:nq], lhsT=kt.ap[:, ci, kb * 128:(kb + 1) * 128], rhs=qt.ap[:, 0:nq], start=True, stop=True),
                        reads=[kt, qt], writes=[ps])
                    S.op('act', lambda e, ps=ps, pt=pt, nq=nq: e.activation(out=pt.ap[:, 0:nq], in_=ps.ap[:, 0:nq], func=AF.Exp, scale=0.125),
                         reads=[ps], writes=[pt])
                    S.op('pe', lambda e, po=po, vt=vt, kb=kb, pt=pt, nq=nq, M=M: e.matmul(
                        po.ap[0:M, 0:nq], lhsT=vt.ap[:, kb, 0:M], rhs=pt.ap[:, 0:nq], start=(kb == kb0), stop=(kb == kb1 - 1)),
                        reads=[vt, pt], writes=[po])
                    if not gqa:
                        S.op('pe', lambda e, psm=psm, kb=kb, pt=pt, nq=nq: e.matmul(
                            psm.ap[0:1, 0:nq], lhsT=k.ones_b.ap[:, 0:1], rhs=pt.ap[:, 0:nq], start=(kb == kb0), stop=(kb == kb1 - 1)),
                            reads=[k.ones_b, pt], writes=[psm])
                pbc = bk[7]
                if gqa:
                    S.op('dve', lambda e, po=po, nq=nq: e.reciprocal(out=rs.ap[64:65, 0:nq], in_=po.ap[64:65, 0:nq]), reads=[po], writes=[rs])
                    S.op('pe', lambda e, nq=nq: e.matmul(pbc.ap[0:64, 0:nq], lhsT=k.ones_f.ap[64:65, 0:64], rhs=rs.ap[64:65, 0:nq], start=True, stop=True),
                         reads=[k.ones_f, rs], writes=[pbc])
                    _cp(S, 'act', bcs, bcs.ap[0:64, 0:nq], pbc, pbc.ap[0:64, 0:nq])
                    y = yst[cnt['y'] % 2]; cnt['y'] += 1
                    S.op('dve', lambda e, y=y, po=po, nq=nq: e.tensor_tensor(out=y.ap[0:64, 0:nq], in0=po.ap[0:64, 0:nq], in1=bcs.ap[0:64, 0:nq], op=ALU.mult),
                         reads=[po, bcs], writes=[y])
                    S.dma('sp', lambda e, y=y, hd=hd, t0=t0, nq=nq: e.dma_start(out=k.yT_d.ap[0, hd['row0']:hd['row0'] + 64, t0:t0 + nq], in_=y.ap[0:64, 0:nq]),
                          reads=[y], writes=[k.yT_d])
                else:
                    S.op('dve', lambda e, psm=psm, nq=nq: e.reciprocal(out=rs.ap[0:1, 0:nq], in_=psm.ap[0:1, 0:nq]), reads=[psm], writes=[rs])
                    S.op('pe', lambda e, nq=nq: e.matmul(pbc.ap[:, 0:nq], lhsT=k.ones_f.ap[0:1, :], rhs=rs.ap[0:1, 0:nq], start=True, stop=True),
                         reads=[k.ones_f, rs], writes=[pbc])
                    _cp(S, 'act', bcs, bcs.ap[:, 0:nq], pbc, pbc.ap[:, 0:nq])
                    if ci == 0:
                        S.op('dve', lambda e, po=po, nq=nq: e.tensor_tensor(out=o1.ap[:, 0:nq], in0=po.ap[:, 0:nq], in1=bcs.ap[:, 0:nq], op=ALU.mult),
                             reads=[po, bcs], writes=[o1])
                    else:
                        S.op('dve', lambda e, po=po, nq=nq: e.tensor_tensor(out=o2.ap[:, 0:nq], in0=po.ap[:, 0:nq], in1=bcs.ap[:, 0:nq], op=ALU.mult),
                             reads=[po, bcs], writes=[o2])
                        S.op('dve', lambda e, nq=nq: e.scalar_tensor_tensor(out=o1.ap[:, 0:nq], in0=o2.ap[:, 0:nq], scalar=neglam.ap, in1=o1.ap[:, 0:nq],
                                                                           op0=ALU.mult, op1=ALU.add), reads=[o2, neglam, o1], writes=[o1])
                        S.op('pool', lambda e, nq=nq: e.tensor_tensor(out=sq.ap[:, 0:nq], in0=o1.ap[:, 0:nq], in1=o1.ap[:, 0:nq], op=ALU.mult), reads=[o1], writes=[sq])
                        S.op('pe', lambda e, nq=nq: e.matmul(pbc.ap[:, 0:nq], lhsT=k.ones_f.ap, rhs=sq.ap[:, 0:nq], start=True, stop=True),
                             reads=[k.ones_f, sq], writes=[pbc])
                        S.op('act', lambda e, nq=nq: e.activation(out=bcs.ap[:, 0:nq], in_=pbc.ap[:, 0:nq], func=AF.Sqrt, bias=k.epst.ap, scale=1.0 / 128),
                             reads=[pbc, k.epst], writes=[bcs])
                        S.op('dve', lambda e, nq=nq: e.reciprocal(out=bcs.ap[:, 0:nq], in_=bcs.ap[:, 0:nq]), reads=[bcs], writes=[bcs])
                        S.op('dve', lambda e, nq=nq: e.tensor_tensor(out=o1.ap[:, 0:nq], in0=o1.ap[:, 0:nq], in1=bcs.ap[:, 0:nq], op=ALU.mult), reads=[o1, bcs], writes=[o1])
                        y = yst[cnt['y'] % 2]; cnt['y'] += 1
                        S.op('act', lambda e, y=y, nq=nq: e.activation(out=y.ap[:, 0:nq], in_=o1.ap[:, 0:nq], func=AF.Identity, scale=sublc.ap),
                             reads=[o1, sublc], writes=[y])
                        S.dma('sp', lambda e, y=y, hd=hd, t0=t0, nq=nq: e.dma_start(out=k.yT_d.ap[1, hd['row0']:hd['row0'] + 128, t0:t0 + nq], in_=y.ap[:, 0:nq]),
                              reads=[y], writes=[k.yT_d])


def _load_cast(k, dst, dst_ap_fn, src_ap_fn, nchunks, stage, wd, idx0=0):
    S = k.S
    for i in range(nchunks):
        w = stage[(idx0 + i) % 2]
        sap = src_ap_fn(i)
        kc = sap.shape[1]
        S.dma('sp', lambda e, w=w, sap=sap, kc=kc: e.dma_start(out=w.ap[:, 0:kc, 0:wd], in_=sap), writes=[w])
        S.op(('pool', 'dve')[i % 2], lambda e, w=w, i=i, kc=kc: e.tensor_copy(out=dst_ap_fn(i), in_=w.ap[:, 0:kc, 0:wd]), reads=[w], writes=[dst])


def phase4_merge(k, li, need_ctx):
    S, ar, I = k.S, k.ar, k.I
    bk = k.banks
    N, CTX, T = k.N, k.CTX, k.T
    wg = ar.alloc('wg', [128, 24, D], BF16)
    wbr = ar.alloc('wbr', [128, 12, D], BF16)
    wo = ar.alloc('wo', [128, 8, D], BF16)
    stage = [ar.alloc(f'st4{i}', [128, 8, 256], F32) for i in range(2)]
    for i in range(3):
        src = I['w_merge_gate'][li, i].rearrange("(c p) n -> p c n", p=128)
        _load_cast(k, wg, lambda q, i=i: wg.ap[:, i * 8:(i + 1) * 8, q * 256:(q + 1) * 256], lambda q, src=src: src[:, :, q * 256:(q + 1) * 256], 4, stage, 256)
        srcb = I['w_branch'][li, i].rearrange("(c p) n -> p c n", p=128)
        _load_cast(k, wbr, lambda q, i=i: wbr.ap[:, i * 4:(i + 1) * 4, q * 256:(q + 1) * 256], lambda q, srcb=srcb: srcb[:, :, q * 256:(q + 1) * 256], 4, stage, 256)
    srco = I['w_out'][li].rearrange("(c p) n -> p c n", p=128)
    _load_cast(k, wo, lambda q: wo.ap[:, :, q * 256:(q + 1) * 256], lambda q: srco[:, :, q * 256:(q + 1) * 256], 4, stage, 256)
    hTs = ar.alloc('hTs4', [128, 8, 512], BF16)
    ys = ar.alloc('ys4', [128, 12, 512], BF16)
    mT = ar.alloc('mT', [128, 8, 512], BF16)
    sg = [ar.alloc(f'sg{i}', [128, 512], F32) for i in range(2)]
    acc = ar.alloc('acc4', [128, 512], F32)
    prod = ar.alloc('prod4', [128, 512], F32)
    xtb = [ar.alloc(f'xt4{i}', [128, D], F32) for i in range(2)]
    tmp = ar.alloc('tmp4', [128, D], F32)
    cnt = 0
    tc = 0
    for (s0, ntok, is_ctx) in _spans(k):
        if is_ctx and not need_ctx:
            continue
        jj = 1 if is_ctx else 0
        S.dma('sp', lambda e, s0=s0, ntok=ntok: e.dma_start(out=hTs.ap[:, :, 0:ntok], in_=k.hT_d.ap[:, :, s0:s0 + ntok]), reads=[k.hT_d], writes=[hTs])
        for i in range(3):
            S.dma('sp', lambda e, i=i, s0=s0, ntok=ntok: e.dma_start(
                out=ys.ap[:, i * 4:(i + 1) * 4, 0:ntok], in_=k.yT_d.ap[i, :, s0:s0 + ntok].rearrange("(c p) t -> p c t", p=128)),
                reads=[k.yT_d], writes=[ys])
        for oc in range(8):
            for i in range(3):
                pa = bk[cnt % 2]; pb = bk[2 + cnt % 2]; s_ = sg[cnt % 2]; cnt += 1
                for c in range(8):
                    S.op('pe', lambda e, pa=pa, i=i, c=c, oc=oc, ntok=ntok: e.matmul(
                        pa.ap[:, 0:ntok], lhsT=wg.ap[:, i * 8 + c, oc * 128:(oc + 1) * 128], rhs=hTs.ap[:, c, 0:ntok], start=(c == 0), stop=(c == 7)),
                        reads=[wg, hTs], writes=[pa])
                for c in range(4):
                    S.op('pe', lambda e, pb=pb, i=i, c=c, oc=oc, ntok=ntok: e.matmul(
                        pb.ap[:, 0:ntok], lhsT=wbr.ap[:, i * 4 + c, oc * 128:(oc + 1) * 128], rhs=ys.ap[:, i * 4 + c, 0:ntok], start=(c == 0), stop=(c == 3)),
                        reads=[wbr, ys], writes=[pb])
                S.op('act', lambda e, pa=pa, s_=s_, ntok=ntok: e.activation(out=s_.ap[:, 0:ntok], in_=pa.ap[:, 0:ntok], func=AF.Sigmoid), reads=[pa], writes=[s_])
                if i == 0:
                    S.op('dve', lambda e, pb=pb, s_=s_, ntok=ntok: e.tensor_tensor(out=acc.ap[:, 0:ntok], in0=pb.ap[:, 0:ntok], in1=s_.ap[:, 0:ntok], op=ALU.mult),
                         reads=[pb, s_], writes=[acc])
                else:
                    S.op('dve', lambda e, pb=pb, s_=s_, ntok=ntok: e.tensor_tensor(out=prod.ap[:, 0:ntok], in0=pb.ap[:, 0:ntok], in1=s_.ap[:, 0:ntok], op=ALU.mult),
                         reads=[pb, s_], writes=[prod])
                    if i == 1:
                        S.op('pool', lambda e, ntok=ntok: e.tensor_tensor(out=acc.ap[:, 0:ntok], in0=acc.ap[:, 0:ntok], in1=prod.ap[:, 0:ntok], op=ALU.add),
                             reads=[acc, prod], writes=[acc])
                    else:
                        S.op('pool', lambda e, oc=oc, ntok=ntok: e.tensor_tensor(out=mT.ap[:, oc, 0:ntok], in0=acc.ap[:, 0:ntok], in1=prod.ap[:, 0:ntok], op=ALU.add),
                             reads=[acc, prod], writes=[mT])
        for j in range(ntok // 128):
            tok0 = s0 + j * 128
            xt = xtb[tc % 2]; tc += 1
            src_ap, src_res = _xsrc(k, li, tok0)
            S.dma('sp', lambda e, xt=xt, src_ap=src_ap: e.dma_start(out=xt.ap, in_=src_ap), reads=[src_res] if src_res else [], writes=[xt])
            for g in range(2):
                pb = bk[4 + g]
                for c in range(8):
                    S.op('pe', lambda e, pb=pb, g=g, c=c, j=j: e.matmul(pb.ap, lhsT=mT.ap[:, c, j * 128:(j + 1) * 128], rhs=wo.ap[:, c, g * 512:(g + 1) * 512],
                                                                      start=(c == 0), stop=(c == 7)), reads=[mT, wo], writes=[pb])
                S.op('dve', lambda e, pb=pb, g=g, jj=jj: e.tensor_tensor(out=tmp.ap[:, g * 512:(g + 1) * 512], in0=pb.ap, in1=k.modb[jj][2].ap[:, g * 512:(g + 1) * 512], op=ALU.mult),
                     reads=[pb, k.modb[jj][2]], writes=[tmp])
            S.op('pool', lambda e, xt=xt: e.tensor_tensor(out=xt.ap, in0=xt.ap, in1=tmp.ap, op=ALU.add), reads=[xt, tmp], writes=[xt])
            S.dma('sp', lambda e, xt=xt, tok0=tok0: e.dma_start(out=k.xs[0].ap[tok0:tok0 + 128, :], in_=xt.ap), reads=[xt], writes=[k.xs[0]])


def _breg(k, e, val):
    if not hasattr(k, '_bregs'):
        k._bregs = {}
    if val not in k._bregs:
        k._bregs[val] = e.to_reg(int(val))
    return k._bregs[val]


def phase5_moe(k, li, need_ctx):
    S, ar, I = k.S, k.ar, k.I
    bk = k.banks
    N, CTX, T = k.N, k.CTX, k.T
    cap = N // 8
    capc = CTX // 8
    SLOTS = cap + (capc if need_ctx else 0)
    BIG = 1.0e6
    t_first = 0 if need_ctx else CTX // 128
    ntile = T // 128
    tiles = list(range(t_first, ntile))
    wr = ar.alloc('wr', [128, 8, NEXP], F32)
    S.dma('sp', lambda e: e.dma_start(out=wr.ap, in_=I['w_router'][li].rearrange("(c p) n -> p c n", p=128)), writes=[wr])
    ustr = ar.alloc('ustr', [128, 128], BF16)
    ustf = ar.alloc('ustf', [128, 128], F32)
    S.dma('sp', lambda e: e.dma_start(out=ustf.ap, in_=I['ustrict']), writes=[ustf])
    S.op('dve', lambda e: e.tensor_copy(out=ustr.ap, in_=ustf.ap), reads=[ustf], writes=[ustr])
    affTM = ar.alloc('affTM', [128, ntile, NEXP], F32)
    maskw = ar.alloc('maskw', [128, ntile, NEXP], F32)
    maskb = ar.alloc('maskb', [128, ntile, NEXP], BF16)
    idxf = ar.alloc('idxf', [128, ntile, NEXP], F32)
    idxi = ar.alloc('idxi', [128, ntile, NEXP], I32)
    m_keep = ar.mark()
    affT = ar.alloc('affT', [NEXP, T], F32)
    m5 = ar.mark()
    xtb = [ar.alloc(f'xt5{i}', [128, D], F32) for i in range(2)]
    sqs = ar.alloc('sq5', [128, D], F32)
    ssb = [ar.alloc(f'ss5{i}', [128, 1], F32) for i in range(2)]
    rsb = [ar.alloc(f'rs5{i}', [128, 1], F32) for i in range(2)]
    xnf = [ar.alloc(f'xnf{i}', [128, D], F32) for i in range(2)]
    xnb = [ar.alloc(f'xnb{i}', [128, D], BF16) for i in range(2)]
    h2T = [ar.alloc(f'h2T{i}', [128, 8, 128], F32) for i in range(2)]
    mx = ar.alloc('mx', [128, 1], F32)
    sm = ar.alloc('sm', [128, 1], F32)
    ex = ar.alloc('ex', [128, NEXP], F32)
    for ti, t in enumerate(tiles):
        tok0 = t * 128
        jj = 1 if tok0 < CTX else 0
        b = ti % 2
        xt, ss, rstd = xtb[b], ssb[b], rsb[b]
        _norm_tile(k, k.xs[0].ap[tok0:tok0 + 128, :], k.xs[0], xt, sqs, ss, rstd)
        S.op('dve', lambda e, b=b, xt=xt, rstd=rstd: e.tensor_scalar(out=xnf[b].ap, in0=xt.ap, scalar1=rstd.ap, scalar2=None, op0=ALU.mult),
             reads=[xt, rstd], writes=[xnf[b]])
        S.op('pool', lambda e, b=b: e.tensor_copy(out=xnb[b].ap, in_=xnf[b].ap), reads=[xnf[b]], writes=[xnb[b]])
        S.dma('sp', lambda e, b=b, tok0=tok0: e.dma_start(out=k.xn_d.ap[tok0:tok0 + 128, :], in_=xnb[b].ap), reads=[xnb[b]], writes=[k.xn_d])
        for half in range(2):
            pT = bank_view(bk[half], F32, [128, 4, 128])
            for c4 in range(4):
                c = half * 4 + c4
                S.op('pe', lambda e, b=b, c=c, c4=c4, pT=pT: e.transpose(out=pT[:, c4, :], in_=xnf[b].ap[:, c * 128:(c + 1) * 128], identity=k.identf.ap),
                     reads=[xnf[b], k.identf], writes=[bk[half]])
            for c4 in range(4):
                c = half * 4 + c4
                S.op('dve' if c4 % 2 else 'act', (lambda e, b=b, c=c, c4=c4, pT=pT, jj=jj: e.tensor_scalar(
                    out=h2T[b].ap[:, c, :], in0=pT[:, c4, :], scalar1=k.colA2.ap[:, c, jj:jj + 1], scalar2=k.colB2.ap[:, c, jj:jj + 1], op0=ALU.mult, op1=ALU.add))
                    if c4 % 2 else (lambda e, b=b, c=c, c4=c4, pT=pT, jj=jj: e.activation(
                        out=h2T[b].ap[:, c, :], in_=pT[:, c4, :], func=AF.Identity, scale=k.colA2.ap[:, c, jj:jj + 1], bias=k.colB2.ap[:, c, jj:jj + 1])),
                    reads=[bk[half], k.colA2, k.colB2], writes=[h2T[b]])
        pl = bk[2 + ti % 2]
        for c in range(8):
            S.op('pe', lambda e, pl=pl, b=b, c=c: e.matmul(pl.ap[:, 0:NEXP], lhsT=h2T[b].ap[:, c, :], rhs=wr.ap[:, c, :], start=(c == 0), stop=(c == 7)),
                 reads=[h2T[b], wr], writes=[pl])
        S.op('dve', lambda e, pl=pl: e.tensor_reduce(out=mx.ap, in_=pl.ap[:, 0:NEXP], axis=AX.X, op=ALU.max), reads=[pl], writes=[mx])
        S.op('dve', lambda e: e.tensor_scalar(out=mx.ap, in0=mx.ap, scalar1=-1.0, scalar2=None, op0=ALU.mult), reads=[mx], writes=[mx])
        S.op('act', lambda e, pl=pl: e.activation(out=ex.ap, in_=pl.ap[:, 0:NEXP], func=AF.Exp, bias=mx.ap, scale=1.0, accum_out=sm.ap), reads=[pl, mx], writes=[ex, sm])
        S.op('dve', lambda e: e.reciprocal(out=sm.ap, in_=sm.ap), reads=[sm], writes=[sm])
        S.op('dve', lambda e, t=t: e.tensor_scalar(out=affTM.ap[:, t, :], in0=ex.ap, scalar1=sm.ap, scalar2=None, op0=ALU.mult), reads=[ex, sm], writes=[affTM])
        pa = bk[4 + ti % 2]
        S.op('pe', lambda e, pa=pa, t=t: e.transpose(out=pa.ap[0:NEXP, 0:128], in_=affTM.ap[:, t, :], identity=k.identf.ap), reads=[affTM, k.identf], writes=[pa])
        _cp(S, 'act', affT, affT.ap[:, tok0:tok0 + 128], pa, pa.ap[0:NEXP, 0:128])
    ar.reset(m5)
    scr = ar.alloc('scr5', [NEXP, max(N, CTX)], F32)
    lo = ar.alloc('lo', [NEXP, 2], F32)
    hi = ar.alloc('hi', [NEXP, 2], F32)
    mid = ar.alloc('mid', [NEXP, 2], F32)
    cn = ar.alloc('cn', [NEXP, 2], F32)
    dl = ar.alloc('dl', [NEXP, 2], F32)
    S.op('dve', lambda e: e.memset(lo.ap, 0.0), writes=[lo])
    S.op('dve', lambda e: e.memset(hi.ap, 1.0), writes=[hi])
    segs = [(0, CTX, N, float(cap))]
    if need_ctx:
        segs.append((1, 0, CTX, float(capc)))
    for it in range(30):
        S.op('dve', lambda e: e.tensor_tensor(out=mid.ap, in0=lo.ap, in1=hi.ap, op=ALU.add), reads=[lo, hi], writes=[mid])
        S.op('dve', lambda e: e.tensor_scalar(out=mid.ap, in0=mid.ap, scalar1=0.5, scalar2=None, op0=ALU.mult), reads=[mid], writes=[mid])
        for (col, t0, n, cp_) in segs:
            S.op('dve', lambda e, col=col, t0=t0, n=n: e.tensor_scalar(out=scr.ap[:, 0:n], in0=affT.ap[:, t0:t0 + n], scalar1=mid.ap[:, col:col + 1], scalar2=None, op0=ALU.is_ge),
                 reads=[affT, mid], writes=[scr])
            S.op('dve', lambda e, col=col, n=n: e.tensor_reduce(out=cn.ap[:, col:col + 1], in_=scr.ap[:, 0:n], axis=AX.X, op=ALU.add), reads=[scr], writes=[cn])
            S.op('dve', lambda e, col=col, cp_=cp_: e.tensor_scalar(out=cn.ap[:, col:col + 1], in0=cn.ap[:, col:col + 1], scalar1=cp_, scalar2=None, op0=ALU.is_ge),
                 reads=[cn], writes=[cn])
            S.op('dve', lambda e, col=col: e.tensor_tensor(out=dl.ap[:, col:col + 1], in0=mid.ap[:, col:col + 1], in1=lo.ap[:, col:col + 1], op=ALU.subtract),
                 reads=[mid, lo], writes=[dl])
            S.op('dve', lambda e, col=col: e.scalar_tensor_tensor(out=lo.ap[:, col:col + 1], in0=dl.ap[:, col:col + 1], scalar=cn.ap[:, col:col + 1], in1=lo.ap[:, col:col + 1],
                                                                 op0=ALU.mult, op1=ALU.add), reads=[dl, cn, lo], writes=[lo])
            S.op('dve', lambda e, col=col: e.tensor_tensor(out=dl.ap[:, col:col + 1], in0=hi.ap[:, col:col + 1], in1=mid.ap[:, col:col + 1], op=ALU.subtract),
                 reads=[mid, hi], writes=[dl])
            S.op('dve', lambda e, col=col: e.scalar_tensor_tensor(out=hi.ap[:, col:col + 1], in0=dl.ap[:, col:col + 1], scalar=cn.ap[:, col:col + 1], in1=mid.ap[:, col:col + 1],
                                                                 op0=ALU.mult, op1=ALU.add), reads=[dl, cn, mid], writes=[hi])
    thrB = ar.alloc('thrB', [128, 2, NEXP], F32)
    trow = ar.alloc('trow', [1, 2, NEXP], F32)
    for col in range(2 if need_ctx else 1):
        S.op('pe', lambda e, col=col: e.transpose(out=bk[0].ap[0:1, 0:NEXP], in_=lo.ap[:, col:col + 1], identity=k.identf.ap[0:NEXP, 0:NEXP]),
             reads=[lo, k.identf], writes=[bk[0]])
        _cp(S, 'dve', trow, trow.ap[:, col, :], bk[0], bk[0].ap[0:1, 0:NEXP])
        S.op('pe', lambda e, col=col: e.matmul(bk[1].ap[:, 0:NEXP], lhsT=k.ones_f.ap[0:1, :], rhs=trow.ap[:, col, :], start=True, stop=True),
             reads=[k.ones_f, trow], writes=[bk[1]])
        _cp(S, 'dve', thrB, thrB.ap[:, col, :], bk[1], bk[1].ap[:, 0:NEXP])
    xg = [ar.alloc(f'xg{i}', [128, D], BF16) for i in range(3)]
    for t in tiles:
        col = 1 if t * 128 < CTX else 0
        S.op('dve', lambda e, t=t, col=col: e.tensor_tensor(out=maskb.ap[:, t, :], in0=affTM.ap[:, t, :], in1=thrB.ap[:, col, :], op=ALU.is_ge),
             reads=[affTM, thrB], writes=[maskb])
        S.op('dve', lambda e, t=t: e.tensor_tensor(out=maskw.ap[:, t, :], in0=maskb.ap[:, t, :], in1=affTM.ap[:, t, :], op=ALU.mult),
             reads=[maskb, affTM], writes=[maskw])
    for ti, t in enumerate(tiles):
        tok0 = t * 128
        is_c = tok0 < CTX
        seg0 = 0 if is_c else CTX // 128
        pp = bk[ti % 4]
        S.op('pe', lambda e, pp=pp, t=t, seg0=seg0: e.matmul(pp.ap[:, 0:NEXP], lhsT=ustr.ap, rhs=maskb.ap[:, t, :], start=True, stop=(t == seg0)),
             reads=[ustr, maskb], writes=[pp])
        for t2 in range(seg0, t):
            S.op('pe', lambda e, pp=pp, t2=t2, t=t: e.matmul(pp.ap[:, 0:NEXP], lhsT=k.ones_b.ap, rhs=maskb.ap[:, t2, :], start=False, stop=(t2 == t - 1)),
                 reads=[k.ones_b, maskb], writes=[pp])
        base = float(cap) if is_c else 0.0
        S.op('dve', lambda e, t=t, base=base: e.tensor_scalar(out=idxf.ap[:, t, :], in0=maskb.ap[:, t, :], scalar1=-BIG, scalar2=BIG + base, op0=ALU.mult, op1=ALU.add),
             reads=[maskb], writes=[idxf])
        S.op('dve', lambda e, t=t, pp=pp: e.tensor_tensor(out=idxf.ap[:, t, :], in0=idxf.ap[:, t, :], in1=pp.ap[:, 0:NEXP], op=ALU.add), reads=[idxf, pp], writes=[idxf])
        S.op('dve', lambda e, t=t: e.tensor_copy(out=idxi.ap[:, t, :], in_=idxf.ap[:, t, :]), reads=[idxf], writes=[idxi])
        x_ = xg[ti % 3]
        S.dma('sp', lambda e, x_=x_, tok0=tok0: e.dma_start(out=x_.ap, in_=k.xn_d.ap[tok0:tok0 + 128, :]), reads=[k.xn_d], writes=[x_])
        bound = (cap + capc - 1) if is_c else (cap - 1)
        for ex_ in range(NEXP):
            S.dma('pool', lambda e, x_=x_, t=t, ex_=ex_, bound=bound: e.indirect_dma_start(
                out=k.xin_d[ex_].ap, out_offset=bass.IndirectOffsetOnAxis(ap=idxi.ap[:, t, ex_:ex_ + 1], axis=0), in_=x_.ap, in_offset=None,
                bounds_check=_breg(k, e, bound), oob_is_err=False), reads=[x_, idxi], writes=[k.xin_d[ex_]])
    k.S.barrier()
    ar.reset(m_keep)
    m5d = ar.mark()
    wE = [[ar.alloc(f'wE{b}{m}', [128, 8, D], BF16) for m in range(3)] for b in range(2)]
    stage = [ar.alloc(f'st5{i}', [128, 8, 256], F32) for i in range(2)]
    xe = [ar.alloc(f'xe{i}', [128, D], BF16) for i in range(2)]
    SP = (SLOTS + 127) // 128 * 128
    xT = ar.alloc('xT5', [128, 8, SP], BF16)
    hidT = ar.alloc('hidT', [128, 8, SP], BF16)
    sgb = [ar.alloc(f'sg5{i}', [128, 512], F32) for i in range(2)]
    yst = [ar.alloc('yst5', [128, D], F32)] * 2
    stiles = [(r0, min(128, SLOTS - r0)) for r0 in range(0, SLOTS, 128)]
    schunks = [(c0, min(512, SLOTS - c0)) for c0 in range(0, SLOTS, 512)]
    cnt = 0
    for ex_ in range(NEXP):
        wb_ = wE[ex_ % 2]
        for m, nm in enumerate(('w_exp_gate', 'w_exp_up', 'w_exp_down')):
            src = I[nm][li, ex_].rearrange("(c p) n -> p c n", p=128)
            _load_cast(k, wb_[m], lambda q, m=m, wb_=wb_: wb_[m].ap[:, :, q * 256:(q + 1) * 256], lambda q, src=src: src[:, :, q * 256:(q + 1) * 256], 4, stage, 256)
        for si, (r0, nr) in enumerate(stiles):
            x_ = xe[si % 2]
            S.dma('sp', lambda e, x_=x_, ex_=ex_, r0=r0, nr=nr: e.dma_start(out=x_.ap[0:nr, :], in_=k.xin_d[ex_].ap[r0:r0 + nr, :]), reads=[k.xin_d[ex_]], writes=[x_])
            pT = bank_view(bk[7], BF16, [128, 8, 128])
            for c in range(8):
                S.op('pe', lambda e, x_=x_, c=c, nr=nr: e.transpose(out=pT[:, c, 0:nr], in_=x_.ap[0:nr, c * 128:(c + 1) * 128], identity=k.identb.ap[0:nr, 0:nr]),
                     reads=[x_, k.identb], writes=[bk[7]])
            rngs = []
            if r0 < cap:
                rngs.append((0, min(nr, cap - r0), 0))
            if r0 + nr > cap:
                rngs.append((max(0, cap - r0), nr, 1))
            for c in range(8):
                for (a_, b_, jj) in rngs:
                    S.op('dve' if c % 2 else 'act', (lambda e, c=c, a_=a_, b_=b_, jj=jj, r0=r0: e.tensor_scalar(
                        out=xT.ap[:, c, r0 + a_:r0 + b_], in0=pT[:, c, a_:b_], scalar1=k.colA2.ap[:, c, jj:jj + 1], scalar2=k.colB2.ap[:, c, jj:jj + 1], op0=ALU.mult, op1=ALU.add))
                        if c % 2 else (lambda e, c=c, a_=a_, b_=b_, jj=jj, r0=r0: e.activation(
                            out=xT.ap[:, c, r0 + a_:r0 + b_], in_=pT[:, c, a_:b_], func=AF.Identity, scale=k.colA2.ap[:, c, jj:jj + 1], bias=k.colB2.ap[:, c, jj:jj + 1])),
                        reads=[bk[7], k.colA2, k.colB2], writes=[xT])
        for fc in range(8):
            for (c0, ncol) in schunks:
                pg = bk[cnt % 2]; pu = bk[2 + cnt % 2]; s_ = sgb[cnt % 2]; cnt += 1
                for (pp_, m) in ((pg, 0), (pu, 1)):
                    for c in range(8):
                        S.op('pe', lambda e, pp_=pp_, m=m, c=c, fc=fc, c0=c0, ncol=ncol, wb_=wb_: e.matmul(
                            pp_.ap[:, 0:ncol], lhsT=wb_[m].ap[:, c, fc * 128:(fc + 1) * 128], rhs=xT.ap[:, c, c0:c0 + ncol], start=(c == 0), stop=(c == 7)),
                            reads=[wb_[m], xT], writes=[pp_])
                S.op('act', lambda e, pg=pg, s_=s_, ncol=ncol: e.activation(out=s_.ap[:, 0:ncol], in_=pg.ap[:, 0:ncol], func=AF.Silu), reads=[pg], writes=[s_])
                S.op('dve', lambda e, pu=pu, s_=s_, fc=fc, c0=c0, ncol=ncol: e.tensor_tensor(out=hidT.ap[:, fc, c0:c0 + ncol], in0=pu.ap[:, 0:ncol], in1=s_.ap[:, 0:ncol], op=ALU.mult),
                     reads=[pu, s_], writes=[hidT])
        for si, (r0, nr) in enumerate(stiles):
            y_ = yst[si % 2]
            for g in range(2):
                pb = bk[4 + g]
                for fc in range(8):
                    S.op('pe', lambda e, pb=pb, g=g, fc=fc, r0=r0, nr=nr, wb_=wb_: e.matmul(
                        pb.ap[0:nr, :], lhsT=hidT.ap[:, fc, r0:r0 + nr], rhs=wb_[2].ap[:, fc, g * 512:(g + 1) * 512], start=(fc == 0), stop=(fc == 7)),
                        reads=[hidT, wb_[2]], writes=[pb])
                _cp(S, 'act' if g else 'dve', y_, y_.ap[0:nr, g * 512:(g + 1) * 512], pb, pb.ap[0:nr, :])
            S.dma('sp', lambda e, y_=y_, ex_=ex_, r0=r0, nr=nr: e.dma_start(out=k.yexp_d[ex_].ap[r0:r0 + nr, :], in_=y_.ap[0:nr, :]), reads=[y_], writes=[k.yexp_d[ex_]])
    k.S.barrier()
    ar.reset(m5d)
    gb = [ar.alloc(f'gb5{i}', [128, D], F32) for i in range(4)]
    for g_ in gb:
        S.op('pool', lambda e, g_=g_: e.memset(g_.ap, 0.0), writes=[g_])
    accb = [ar.alloc(f'acc5{i}', [128, D], F32) for i in range(2)]
    xtb = [ar.alloc(f'xt5e{i}', [128, D], F32) for i in range(2)]
    gcnt = 0
    last = not need_ctx
    for ti, t in enumerate(tiles):
        tok0 = t * 128
        is_c = tok0 < CTX
        jj = 1 if is_c else 0
        bound = (cap + capc - 1) if is_c else (cap - 1)
        acc = accb[ti % 2]
        xt = xtb[ti % 2]
        S.dma('sp', lambda e, xt=xt, tok0=tok0: e.dma_start(out=xt.ap, in_=k.xs[0].ap[tok0:tok0 + 128, :]), reads=[k.xs[0]], writes=[xt])
        for ex_ in range(NEXP):
            g_ = gb[gcnt % 4]; gcnt += 1
            S.dma('pool', lambda e, g_=g_, t=t, ex_=ex_, bound=bound: e.indirect_dma_start(
                out=g_.ap, out_offset=None, in_=k.yexp_d[ex_].ap, in_offset=bass.IndirectOffsetOnAxis(ap=idxi.ap[:, t, ex_:ex_ + 1], axis=0),
                bounds_check=_breg(k, e, bound), oob_is_err=False), reads=[k.yexp_d[ex_], idxi], writes=[g_])
            if ex_ == 0:
                S.op('dve', lambda e, g_=g_, acc=acc, t=t, ex_=ex_: e.tensor_scalar(out=acc.ap, in0=g_.ap, scalar1=maskw.ap[:, t, ex_:ex_ + 1], scalar2=None, op0=ALU.mult),
                     reads=[g_, maskw], writes=[acc])
            else:
                S.op('dve', lambda e, g_=g_, acc=acc, t=t, ex_=ex_: e.scalar_tensor_tensor(out=acc.ap, in0=g_.ap, scalar=maskw.ap[:, t, ex_:ex_ + 1], in1=acc.ap, op0=ALU.mult, op1=ALU.add),
                     reads=[g_, maskw, acc], writes=[acc])
        S.op('pool', lambda e, acc=acc, jj=jj: e.tensor_tensor(out=acc.ap, in0=acc.ap, in1=k.modb[jj][5].ap, op=ALU.mult), reads=[acc, k.modb[jj][5]], writes=[acc])
        S.op('pool', lambda e, acc=acc, xt=xt: e.tensor_tensor(out=xt.ap, in0=xt.ap, in1=acc.ap, op=ALU.add), reads=[acc, xt], writes=[xt])
        if last:
            S.dma('sp', lambda e, xt=xt, tok0=tok0: e.dma_start(out=k.out.ap[tok0 - CTX:tok0 - CTX + 128, :], in_=xt.ap), reads=[xt], writes=[k.out])
        else:
            S.dma('sp', lambda e, xt=xt, tok0=tok0: e.dma_start(out=k.xs[1].ap[tok0:tok0 + 128, :], in_=xt.ap), reads=[xt], writes=[k.xs[1]])


def phase3_gdn(k, li, need_ctx):
    S, ar, I = k.S, k.ar, k.I
    bk = k.banks
    N, CTX, T = k.N, k.CTX, k.T
    cw = ar.alloc('cw', [128, 12, 3], F32)
    for cc in range(12):
        S.dma('sp', lambda e, cc=cc: e.dma_start(out=cw.ap[:, cc, :], in_=I['gdn_conv_w'][li][:, cc * 128:(cc + 1) * 128].rearrange("k p -> p k"),
                                                 allow_slow_non_contiguous=True), writes=[cw])
    m3 = ar.mark()
    xin = [ar.alloc(f'cxin{i}', [128, 514], F32) for i in range(2)]
    yb = [ar.alloc(f'cy{i}', [128, 512], F32) for i in range(2)]
    sq = ar.alloc('csq', [128, 512], F32)
    rn = ar.alloc('crn', [128, 512], F32)
    cnt = 0
    for (s0, ntok, is_ctx) in _spans(k):
        seg0, seg1 = (0, CTX) if is_ctx else (CTX, T)
        for cc in range(12):
            x_ = xin[cnt % 2]; y_ = yb[cnt % 2]; cnt += 1
            a0 = max(s0 - 1, seg0); a1 = min(s0 + ntok + 1, seg1)
            off = a0 - (s0 - 1)
            if s0 - 1 < seg0:
                S.op('pool', lambda e, x_=x_: e.memset(x_.ap[:, 0:1], 0.0), writes=[x_])
            if s0 + ntok + 1 > seg1:
                S.op('pool', lambda e, x_=x_, ntok=ntok: e.memset(x_.ap[:, ntok + 1:ntok + 2], 0.0), writes=[x_])
            S.dma('sp', lambda e, x_=x_, cc=cc, a0=a0, a1=a1, off=off: e.dma_start(out=x_.ap[:, off:off + a1 - a0], in_=k.cT_d.ap[cc, :, a0:a1]),
                  reads=[k.cT_d], writes=[x_])
            S.op('dve', lambda e, x_=x_, y_=y_, cc=cc, ntok=ntok: e.tensor_scalar(out=y_.ap[:, 0:ntok], in0=x_.ap[:, 0:ntok], scalar1=cw.ap[:, cc, 0:1], scalar2=None, op0=ALU.mult),
                 reads=[x_, cw], writes=[y_])
            for tap in (1, 2):
                S.op('dve', lambda e, x_=x_, y_=y_, cc=cc, ntok=ntok, tap=tap: e.scalar_tensor_tensor(
                    out=y_.ap[:, 0:ntok], in0=x_.ap[:, tap:tap + ntok], scalar=cw.ap[:, cc, tap:tap + 1], in1=y_.ap[:, 0:ntok], op0=ALU.mult, op1=ALU.add),
                    reads=[x_, cw, y_], writes=[y_])
            S.op('act', lambda e, y_=y_, ntok=ntok: e.activation(out=y_.ap[:, 0:ntok], in_=y_.ap[:, 0:ntok], func=AF.Silu), reads=[y_], writes=[y_])
            if cc < 8:
                pb = bk[cnt % 4]
                S.op('pool', lambda e, y_=y_, ntok=ntok: e.tensor_tensor(out=sq.ap[:, 0:ntok], in0=y_.ap[:, 0:ntok], in1=y_.ap[:, 0:ntok], op=ALU.mult), reads=[y_], writes=[sq])
                S.op('pe', lambda e, pb=pb, ntok=ntok: e.matmul(pb.ap[:, 0:ntok], lhsT=k.ones_f.ap, rhs=sq.ap[:, 0:ntok], start=True, stop=True),
                     reads=[k.ones_f, sq], writes=[pb])
                S.op('act', lambda e, pb=pb, ntok=ntok: e.activation(out=rn.ap[:, 0:ntok], in_=pb.ap[:, 0:ntok], func=AF.Sqrt, bias=k.epst.ap, scale=1.0),
                     reads=[pb, k.epst], writes=[rn])
                S.op('dve', lambda e, ntok=ntok: e.reciprocal(out=rn.ap[:, 0:ntok], in_=rn.ap[:, 0:ntok]), reads=[rn], writes=[rn])
                sc_ = (128.0 ** -0.5) if cc < 4 else 1.0
                S.op('dve', lambda e, y_=y_, ntok=ntok, sc_=sc_: e.scalar_tensor_tensor(out=y_.ap[:, 0:ntok], in0=y_.ap[:, 0:ntok], scalar=sc_, in1=rn.ap[:, 0:ntok],
                                                                                      op0=ALU.mult, op1=ALU.mult), reads=[y_, rn], writes=[y_])
            S.dma('sp', lambda e, y_=y_, cc=cc, s0=s0, ntok=ntok: e.dma_start(out=k.cP_d.ap[cc, :, s0:s0 + ntok], in_=y_.ap[:, 0:ntok]), reads=[y_], writes=[k.cP_d])
    S.barrier()
    ar.reset(m3)
    gc_ = ar.alloc('gdnc', [128, 2, 5, 64], F32)
    S.dma('sp', lambda e: e.dma_start(out=gc_.ap, in_=I['gdnc'].rearrange("d p s c -> p d s c")), writes=[gc_])
    gnw = ar.alloc('gnw', [64, 128], F32)
    S.dma('sp', lambda e: e.dma_start(out=gnw.ap, in_=I['gdn_norm_w'][li].partition_broadcast(64)), writes=[gnw])
    Sf = ar.alloc('Sf', [128, 4, 128], F32)
    Sb = ar.alloc('Sb', [128, 4, 128], BF16)
    A = lambda nm, shape, dt=F32: [ar.alloc(f'{nm}{i}', shape, dt) for i in range(2)]
    qk_in = A('qk_in', [128, 8, 64]); v_in = A('v_in', [128, 4, 64]); gdup = A('gdup', [128, 4]); beta = A('beta', [64, 4])
    qkb = A('qkb', [128, 8, 64], BF16)
    lhsE = A('lhsE', [128, 4, 64])
    Em = A('Em', [64, 4, 64]); DT = A('DT', [64, 4, 64])
    gcs = A('gcs', [64, 4]); egc = A('egc', [64, 4]); negegc = A('negegc', [64, 4]); ekd = A('ekd', [64, 4]); glast = A('glast', [128, 4])
    KKD = A('KKD', [64, 4, 64]); QKD = A('QKD', [64, 4, 64], BF16)
    Mf = A('Mf', [64, 4, 64]); Mb = A('Mb', [64, 4, 64], BF16); Nb = A('Nb', [64, 4, 64], BF16)
    NM = A('NM', [64, 8, 64], BF16)
    Yf = A('Yf', [64, 4, 64]); Yb = A('Yb', [64, 4, 64], BF16)
    kdec = A('kdec', [64, 4, 128], BF16); vtok = A('vtok', [64, 4, 128])
    rp = A('rp', [64, 4, 128], BF16); vnew = A('vnew', [64, 4, 128], BF16)
    o1 = A('o1g', [64, 4, 128]); ost = A('ost', [64, 4, 128])
    ofw = A('ofw', [64, 4, 128]); zc = A('zc', [64, 4, 128], BF16)
    ssq = A('gssq', [64, 4]); yg = A('yg', [64, 4, 128], BF16); ygT = A('ygT', [128, 4, 64], BF16)
    sqg = A('sqg', [64, 4, 128])
    nch = T // 64
    nctx = CTX // 64
    it = 0
    for d in range(2):
        S.op('pool', lambda e: e.memset(Sf.ap, 0.0), writes=[Sf])
        S.op('pool', lambda e: e.memset(Sb.ap, 0.0), writes=[Sb])
        order = list(range(nctx)) + list(range(nctx, nch))
        if d == 1:
            order = list(range(nctx - 1, -1, -1)) + list(range(nch - 1, nctx - 1, -1))
        CM = gc_.ap[:, d, 0, :]; RC = gc_.ap[:, d, 1, :]; UC = gc_.ap[0:64, d, 2, :]; BM = gc_.ap[0:64, d, 3, :]; ST = gc_.ap[0:64, d, 4, :]
        bc4 = lambda ap: ap.unsqueeze(1).broadcast_to([64, 4, 64])
        for c in order:
            b = it % 2; it += 1
            tok0 = c * 64
            want_o = need_ctx or tok0 >= CTX
            qi, vi, gd, be, qb = qk_in[b], v_in[b], gdup[b], beta[b], qkb[b]
            S.dma('sp', lambda e, qi=qi, tok0=tok0: e.dma_start(out=qi.ap, in_=k.cP_d.ap[0:8, :, tok0:tok0 + 64].rearrange("c p t -> p c t")), reads=[k.cP_d], writes=[qi])
            S.dma('sp', lambda e, vi=vi, tok0=tok0: e.dma_start(out=vi.ap, in_=k.cP_d.ap[8:12, :, tok0:tok0 + 64].rearrange("c p t -> p c t")), reads=[k.cP_d], writes=[vi])
            for hf in range(2):
                S.dma('sp', lambda e, gd=gd, tok0=tok0, hf=hf, d=d: e.dma_start(out=gd.ap[hf * 64:(hf + 1) * 64, :], in_=k.gb_d.ap[tok0:tok0 + 64, 8 + d * 4:12 + d * 4]),
                      reads=[k.gb_d], writes=[gd])
            S.dma('sp', lambda e, be=be, tok0=tok0, d=d: e.dma_start(out=be.ap, in_=k.gb_d.ap[tok0:tok0 + 64, d * 4:d * 4 + 4]), reads=[k.gb_d], writes=[be])
            S.op('pool', lambda e, qi=qi, qb=qb: e.tensor_copy(out=qb.ap, in_=qi.ap), reads=[qi], writes=[qb])
            pk = bank_view(bk[0], F32, [64, 4, 128]); pv = bank_view(bk[1], F32, [64, 4, 128])
            for h in range(4):
                S.op('pe', lambda e, h=h, qi=qi, pk=pk: e.transpose(out=pk[:, h, :], in_=qi.ap[:, 4 + h, :], identity=k.identf.ap), reads=[qi, k.identf], writes=[bk[0]])
            for h in range(4):
                S.op('pe', lambda e, h=h, vi=vi, pv=pv: e.transpose(out=pv[:, h, :], in_=vi.ap[:, h, :], identity=k.identf.ap), reads=[vi, k.identf], writes=[bk[1]])
            pc = bank_view(bk[2], F32, [64, 8, 64])
            for h in range(4):
                S.op('pe', lambda e, h=h, qb=qb, pc=pc: e.matmul(pc[:, h, :], lhsT=qb.ap[:, 4 + h, :], rhs=qb.ap[:, 4 + h, :], start=True, stop=True), reads=[qb], writes=[bk[2]])
                S.op('pe', lambda e, h=h, qb=qb, pc=pc: e.matmul(pc[:, 4 + h, :], lhsT=qb.ap[:, 4 + h, :], rhs=qb.ap[:, h, :], start=True, stop=True), reads=[qb], writes=[bk[2]])
            le = lhsE[b]
            S.op('dve', lambda e, le=le, gd=gd, CM=CM: e.tensor_tensor(out=le.ap, in0=CM.unsqueeze(1).broadcast_to([128, 4, 64]), in1=gd.ap.unsqueeze(2).broadcast_to([128, 4, 64]), op=ALU.mult),
                 reads=[gc_, gd], writes=[le])
            pe_ = bank_view(bk[3], F32, [64, 4, 64])
            for h in range(4):
                S.op('pe', lambda e, h=h, le=le, RC=RC, pe_=pe_: e.matmul(pe_[:, h, :], lhsT=le.ap[:, h, :], rhs=RC, start=True, stop=True), reads=[le, gc_], writes=[bk[3]])
            pgc = bk[3].ap[0:64, 256:260]
            S.op('pe', lambda e, gd=gd, UC=UC, pgc=pgc: e.matmul(pgc, lhsT=UC, rhs=gd.ap[0:64, :], start=True, stop=True), reads=[gd, gc_], writes=[bk[3]])
            pgs = bk[3].ap[:, 264:268]
            S.op('pe', lambda e, gd=gd, pgs=pgs: e.matmul(pgs, lhsT=k.ones_f.ap[0:64, :], rhs=gd.ap[0:64, :], start=True, stop=True), reads=[gd, k.ones_f], writes=[bk[3]])
            em, dt_ = Em[b], DT[b]
            S.op('dve', lambda e, em=em, pe_=pe_, BM=BM: e.tensor_tensor(out=em.ap, in0=pe_, in1=bc4(BM), op=ALU.add), reads=[bk[3], gc_], writes=[em])
            S.op('act', lambda e, em=em, dt_=dt_: e.activation(out=dt_.ap, in_=em.ap, func=AF.Exp, scale=-1.0), reads=[em], writes=[dt_])
            S.op('dve', lambda e, b=b, pgc=pgc: e.tensor_copy(out=gcs[b].ap, in_=pgc), reads=[bk[3]], writes=[gcs[b]])
            S.op('act', lambda e, b=b, pgc=pgc: e.activation(out=egc[b].ap, in_=pgc, func=AF.Exp), reads=[bk[3]], writes=[egc[b]])
            S.op('dve', lambda e, b=b: e.tensor_scalar(out=negegc[b].ap, in0=egc[b].ap, scalar1=-1.0, scalar2=None, op0=ALU.mult), reads=[egc[b]], writes=[negegc[b]])
            S.op('dve', lambda e, b=b, pgs=pgs: e.tensor_tensor(out=ekd[b].ap, in0=pgs[0:64, :], in1=gcs[b].ap, op=ALU.subtract), reads=[bk[3], gcs[b]], writes=[ekd[b]])
            S.op('act', lambda e, b=b: e.activation(out=ekd[b].ap, in_=ekd[b].ap, func=AF.Exp), reads=[ekd[b]], writes=[ekd[b]])
            S.op('act', lambda e, b=b, pgs=pgs: e.activation(out=glast[b].ap, in_=pgs, func=AF.Exp), reads=[bk[3]], writes=[glast[b]])
            S.op('dve', lambda e, b=b, pc=pc, dt_=dt_: e.tensor_tensor(out=KKD[b].ap, in0=pc[:, 0:4, :], in1=dt_.ap, op=ALU.mult), reads=[bk[2], dt_], writes=[KKD[b]])
            S.op('dve', lambda e, b=b, pc=pc, dt_=dt_: e.tensor_tensor(out=QKD[b].ap, in0=pc[:, 4:8, :], in1=dt_.ap, op=ALU.mult), reads=[bk[2], dt_], writes=[QKD[b]])
            S.op('pool', lambda e, b=b, be=be: e.tensor_tensor(out=KKD[b].ap, in0=KKD[b].ap, in1=be.ap.unsqueeze(2).broadcast_to([64, 4, 64]), op=ALU.mult), reads=[KKD[b], be], writes=[KKD[b]])
            S.op('pool', lambda e, b=b, ST=ST: e.tensor_tensor(out=Mf[b].ap, in0=KKD[b].ap, in1=bc4(ST), op=ALU.mult), reads=[KKD[b], gc_], writes=[Mf[b]])
            S.op('pool', lambda e, b=b: e.tensor_copy(out=Mb[b].ap, in_=Mf[b].ap), reads=[Mf[b]], writes=[Mb[b]])
            S.op('dve', lambda e, b=b: e.tensor_tensor(out=Yf[b].ap, in0=bc4(k.identf.ap[0:64, 0:64]), in1=Mf[b].ap, op=ALU.subtract), reads=[k.identf, Mf[b]], writes=[Yf[b]])
            S.op('pool', lambda e, b=b: e.tensor_copy(out=Yb[b].ap, in_=Yf[b].ap), reads=[Yf[b]], writes=[Yb[b]])
            pn = bank_view(bk[4], F32, [64, 4, 64])
            for h in range(4):
                S.op('pe', lambda e, h=h, b=b, pn=pn: e.transpose(out=pn[:, h, :], in_=Mf[b].ap[:, h, :], identity=k.identf.ap[0:64, 0:64]), reads=[Mf[b], k.identf], writes=[bk[4]])
            _cp(S, 'act', Nb[b], Nb[b].ap, bk[4], pn)
            curN, curM = Nb[b], Mb[b]
            nm = NM[b]
            for lvl in range(5):
                pnm = bank_view(bk[4 + (lvl % 2)], F32, [64, 8, 64])
                lastl = lvl == 4
                cN, cM = curN, curM
                nview = (lambda cN=cN: cN.ap) if lvl == 0 else (lambda nm=nm: nm.ap[:, 0:4, :])
                mview = (lambda cM=cM: cM.ap) if lvl == 0 else (lambda nm=nm: nm.ap[:, 4:8, :])
                srcs = [curN, curM] if lvl == 0 else [nm]
                for h in range(4):
                    S.op('pe', lambda e, h=h, pnm=pnm, nview=nview, mview=mview: e.matmul(pnm[:, h, :], lhsT=mview()[:, h, :], rhs=nview()[:, h, :], start=True, stop=True),
                         reads=srcs, writes=[bk[4 + (lvl % 2)]])
                    if not lastl:
                        S.op('pe', lambda e, h=h, pnm=pnm, nview=nview, mview=mview: e.matmul(pnm[:, 4 + h, :], lhsT=nview()[:, h, :], rhs=mview()[:, h, :], start=True, stop=True),
                             reads=srcs, writes=[bk[4 + (lvl % 2)]])
                if lastl:
                    _cp(S, 'act', nm, nm.ap[:, 0:4, :], bk[4 + (lvl % 2)], pnm[:, 0:4, :])
                else:
                    _cp(S, 'act', nm, nm.ap, bk[4 + (lvl % 2)], pnm)
                py = bank_view(bk[6], F32, [64, 4, 64])
                for h in range(4):
                    S.op('pe', lambda e, h=h, py=py, nm=nm, b=b: e.matmul(py[:, h, :], lhsT=nm.ap[:, h, :], rhs=Yb[b].ap[:, h, :], start=True, stop=True),
                         reads=[nm, Yb[b]], writes=[bk[6]])
                S.op('dve', lambda e, b=b, py=py: e.tensor_tensor(out=Yf[b].ap, in0=Yf[b].ap, in1=py, op=ALU.add), reads=[Yf[b], bk[6]], writes=[Yf[b]])
                S.op('pool', lambda e, b=b: e.tensor_copy(out=Yb[b].ap, in_=Yf[b].ap), reads=[Yf[b]], writes=[Yb[b]])
            S.op('dve', lambda e, b=b, pk=pk: e.tensor_tensor(out=kdec[b].ap, in0=pk, in1=ekd[b].ap.unsqueeze(2).broadcast_to([64, 4, 128]), op=ALU.mult), reads=[bk[0], ekd[b]], writes=[kdec[b]])
            _cp(S, 'act', vtok[b], vtok[b].ap, bk[1], pv)
            pks = bank_view(bk[7], F32, [64, 4, 128]); pvn = bank_view(bk[6], F32, [64, 4, 128])
            p1 = bank_view(bk[0], F32, [64, 4, 128]); p2 = bank_view(bk[1], F32, [64, 4, 128]); pds = bank_view(bk[2], F32, [128, 4, 128])
            for h in range(4):
                S.op('pe', lambda e, h=h, qb=qb, pks=pks: e.matmul(pks[:, h, :], lhsT=qb.ap[:, 4 + h, :], rhs=Sb.ap[:, h, :], start=True, stop=True), reads=[qb, Sb], writes=[bk[7]])
                S.op('dve', lambda e, h=h, b=b, pks=pks: e.scalar_tensor_tensor(out=rp[b].ap[:, h, :], in0=pks[:, h, :], scalar=negegc[b].ap[:, h:h + 1], in1=vtok[b].ap[:, h, :],
                                                                               op0=ALU.mult, op1=ALU.add), reads=[bk[7], negegc[b], vtok[b]], writes=[rp[b]])
                S.op('pe', lambda e, h=h, b=b, pvn=pvn: e.matmul(pvn[:, h, :], lhsT=Yb[b].ap[:, h, :], rhs=rp[b].ap[:, h, :], start=True, stop=True), reads=[Yb[b], rp[b]], writes=[bk[6]])
                S.op('act', lambda e, h=h, b=b, be=be, pvn=pvn: e.activation(out=vnew[b].ap[:, h, :], in_=pvn[:, h, :], func=AF.Copy, scale=be.ap[:, h:h + 1]), reads=[bk[6], be], writes=[vnew[b]])
                if want_o:
                    S.op('pe', lambda e, h=h, qb=qb, p1=p1: e.matmul(p1[:, h, :], lhsT=qb.ap[:, h, :], rhs=Sb.ap[:, h, :], start=True, stop=True), reads=[qb, Sb], writes=[bk[0]])
                    S.op('pe', lambda e, h=h, b=b, p2=p2: e.matmul(p2[:, h, :], lhsT=QKD[b].ap[:, h, :], rhs=vnew[b].ap[:, h, :], start=True, stop=True), reads=[QKD[b], vnew[b]], writes=[bk[1]])
                S.op('pe', lambda e, h=h, b=b, pds=pds: e.matmul(pds[:, h, :], lhsT=kdec[b].ap[:, h, :], rhs=vnew[b].ap[:, h, :], start=True, stop=True), reads=[kdec[b], vnew[b]], writes=[bk[2]])
                S.op('dve', lambda e, h=h, b=b, pds=pds: e.scalar_tensor_tensor(out=Sf.ap[:, h, :], in0=Sf.ap[:, h, :], scalar=glast[b].ap[:, h:h + 1], in1=pds[:, h, :],
                                                                               op0=ALU.mult, op1=ALU.add), reads=[Sf, glast[b], bk[2]], writes=[Sf])
                S.op('pool', lambda e, h=h: e.tensor_copy(out=Sb.ap[:, h, :], in_=Sf.ap[:, h, :]), reads=[Sf], writes=[Sb])
            if not want_o:
                continue
            S.op('dve', lambda e, b=b, p1=p1: e.tensor_tensor(out=o1[b].ap, in0=p1, in1=egc[b].ap.unsqueeze(2).broadcast_to([64, 4, 128]), op=ALU.mult), reads=[bk[0], egc[b]], writes=[o1[b]])
            S.op('dve', lambda e, b=b, p2=p2: e.tensor_tensor(out=ost[b].ap, in0=o1[b].ap, in1=p2, op=ALU.add), reads=[o1[b], bk[1]], writes=[ost[b]])
            if d == 0:
                S.dma('sp', lambda e, b=b, tok0=tok0: e.dma_start(out=k.of_d.ap[tok0:tok0 + 64, :], in_=ost[b].ap.rearrange("p a b -> p (a b)")), reads=[ost[b]], writes=[k.of_d])
                continue
            S.dma('sp', lambda e, b=b, tok0=tok0: e.dma_start(out=ofw[b].ap.rearrange("p a b -> p (a b)"), in_=k.of_d.ap[tok0:tok0 + 64, :]), reads=[k.of_d], writes=[ofw[b]])
            S.dma('sp', lambda e, b=b, tok0=tok0: e.dma_start(out=zc[b].ap.rearrange("p a b -> p (a b)"), in_=k.z_d.ap[tok0:tok0 + 64, :]), reads=[k.z_d], writes=[zc[b]])
            S.op('pool', lambda e, b=b: e.tensor_tensor(out=ost[b].ap, in0=ost[b].ap, in1=ofw[b].ap, op=ALU.add), reads=[ost[b], ofw[b]], writes=[ost[b]])
            S.op('pool', lambda e, b=b: e.tensor_tensor(out=sqg[b].ap, in0=ost[b].ap, in1=ost[b].ap, op=ALU.mult), reads=[ost[b]], writes=[sqg[b]])
            S.op('dve', lambda e, b=b: e.tensor_reduce(out=ssq[b].ap, in_=sqg[b].ap, axis=AX.X, op=ALU.add), reads=[sqg[b]], writes=[ssq[b]])
            S.op('act', lambda e, b=b: e.activation(out=ssq[b].ap, in_=ssq[b].ap, func=AF.Sqrt, bias=k.epst.ap[0:64, :], scale=1.0 / 128), reads=[ssq[b], k.epst], writes=[ssq[b]])
            S.op('dve', lambda e, b=b: e.reciprocal(out=ssq[b].ap, in_=ssq[b].ap), reads=[ssq[b]], writes=[ssq[b]])
            S.op('dve', lambda e, b=b: e.tensor_tensor(out=ost[b].ap, in0=ost[b].ap, in1=ssq[b].ap.unsqueeze(2).broadcast_to([64, 4, 128]), op=ALU.mult), reads=[ost[b], ssq[b]], writes=[ost[b]])
            S.op('pool', lambda e, b=b: e.tensor_tensor(out=ost[b].ap, in0=ost[b].ap, in1=gnw.ap.unsqueeze(1).broadcast_to([64, 4, 128]), op=ALU.mult), reads=[ost[b], gnw], writes=[ost[b]])
            S.op('dve', lambda e, b=b: e.tensor_tensor(out=yg[b].ap, in0=ost[b].ap, in1=zc[b].ap, op=ALU.mult), reads=[ost[b], zc[b]], writes=[yg[b]])
            pyt = bank_view(bk[5], BF16, [128, 4, 64])
            for h in range(4):
                S.op('pe', lambda e, h=h, b=b, pyt=pyt: e.transpose(out=pyt[:, h, :], in_=yg[b].ap[:, h, :], identity=k.identb.ap[0:64, 0:64]), reads=[yg[b], k.identb], writes=[bk[5]])
            _cp(S, 'act', ygT[b], ygT[b].ap, bk[5], pyt)
            S.dma('sp', lambda e, b=b, tok0=tok0: e.dma_start(out=k.yT_d.ap[2, :, tok0:tok0 + 64].rearrange("(c p) t -> p c t", p=128), in_=ygT[b].ap), reads=[ygT[b]], writes=[k.yT_d])


def gdn_consts():
    g = np.zeros((2, 128, 5, 64), np.float32)
    kk = np.arange(64)[:, None]
    ii = np.arange(64)[None, :]
    for d in range(2):
        U = (kk <= ii) if d == 0 else (kk >= ii)
        U = U.astype(np.float32)
        valid = U
        g[d, 0:64, 0] = U
        g[d, 64:128, 0] = -1.0
        g[d, 0:64, 1] = 1.0
        g[d, 64:128, 1] = U
        g[d, 0:64, 2] = U
        g[d, 0:64, 3] = (1.0 - valid) * 30000.0
        g[d, 0:64, 4] = valid * (1.0 - np.eye(64, dtype=np.float32))
    return g


def _rope_tables(N):
    rows = N // 64
    row = np.repeat(np.arange(rows, dtype=np.float32), 64)
    col = np.tile(np.arange(64, dtype=np.float32), rows)
    inv_freq = (np.float32(10000.0) ** (-np.arange(0, 32, 2, dtype=np.float32) / np.float32(32))).astype(np.float32)
    ang = np.concatenate([row[:, None] * inv_freq, col[:, None] * inv_freq], axis=-1).astype(np.float32)
    cos, sin = np.cos(ang).astype(np.float32), np.sin(ang).astype(np.float32)
    C = np.zeros((N, 64), np.float32)
    Sg = np.zeros((N, 64), np.float32)
    for a in range(2):
        for pr in range(2):
            C[:, a * 32 + pr * 16:a * 32 + pr * 16 + 16] = cos[:, a * 16:(a + 1) * 16]
            Sg[:, a * 32 + pr * 16:a * 32 + pr * 16 + 16] = sin[:, a * 16:(a + 1) * 16] * (-1.0 if pr == 0 else 1.0)
    return C, Sg


_NC_CACHE = {}


def kernel(**inputs):
    B, N, _ = inputs['x'].shape
    CTX = inputs['ctx'].shape[1]
    key = (N, CTX)
    if key not in _NC_CACHE:
        _NC_CACHE[key] = build(N=N, CTX=CTX)
    nc = _NC_CACHE[key]
    f32 = lambda a: np.ascontiguousarray(np.asarray(a, dtype=np.float32))
    shared = {nm: f32(v) for nm, v in inputs.items() if nm not in ('x', 'c', 'ctx')}
    shared['gdn_a_log'] = shared['gdn_a_log'].reshape(DEPTH, 8)
    shared['gdn_dt_bias'] = shared['gdn_dt_bias'].reshape(DEPTH, 8)
    C, Sg = _rope_tables(N)
    shared['ident'] = np.eye(128, dtype=np.float32)
    shared['ropeC'] = C
    shared['ropeS'] = Sg
    shared['ustrict'] = np.triu(np.ones((128, 128), np.float32), 1)
    shared['gdnc'] = gdn_consts()
    x, c, ctx = f32(inputs['x']), f32(inputs['c']), f32(inputs['ctx'])
    in_maps = []
    for b in range(B):
        m = dict(shared)
        m['x'] = x[b]
        m['c'] = c[b]
        m['ctx'] = ctx[b]
        in_maps.append(m)
    res = run_bass_kernel_spmd(nc, in_maps, core_ids=list(range(B)))
    return np.stack([np.asarray(r['out'], dtype=np.float32) for r in res.results], axis=0)
```

```python
import math
from contextlib import ExitStack

import numpy as np
import concourse.bass as bass
import concourse.mybir as mybir
from concourse.bass_utils import run_bass_kernel_spmd

F32 = mybir.dt.float32
BF16 = mybir.dt.bfloat16
I32 = mybir.dt.int32
AF = mybir.ActivationFunctionType
ALU = mybir.AluOpType
AX = mybir.AxisListType

D = 1024
DEPTH = 2
NEXP = 16
IN_COLS = 4368
EPS = 1e-6

COMPUTE = ('pe', 'act', 'dve', 'pool')
QUEUES = ('sp', 'act', 'pool')
EPOCH = 30000
NS = 8


class Sched:
    def __init__(self):
        self.stream = {e: [] for e in ('pe', 'act', 'dve', 'pool', 'sp')}
        self.ncomp = {e: 0 for e in COMPUTE}
        self.ndma = {q: 0 for q in QUEUES}
        self.lastw = {}
        self.readers = {}
        self.waited = {}
        self.semkeys = set()

    def _need(self, eng, tok):
        semkey, val = tok
        if semkey[0] == 'c':
            if semkey[1] == 'pe' and eng == 'pe':
                return
            g = semkey[2] * EPOCH + val
            if semkey[1] == eng and self.ncomp[eng] - g >= 6:
                return
            k = (eng, 'c', semkey[1])
            if self.waited.get(k, 0) >= g:
                return
            self.waited[k] = g
        else:
            k = (eng, semkey)
            if self.waited.get(k, 0) >= val:
                return
            self.waited[k] = val
        self.stream[eng].append(('wait', semkey, val))

    def _deps(self, eng, reads, writes):
        toks = {}
        def add(sk, v):
            if sk[0] == 'c':
                key = ('c', sk[1])
                g = sk[2] * EPOCH + v
                if key not in toks or toks[key][0] < g:
                    toks[key] = (g, sk, v)
            else:
                if sk not in toks or toks[sk][0] < v:
                    toks[sk] = (v, sk, v)
        for r in reads:
            t = self.lastw.get(r)
            if t is not None:
                add(*t)
        for w in writes:
            t = self.lastw.get(w)
            if t is not None:
                add(*t)
            rd = self.readers.get(w)
            if rd:
                for sk, v in rd.items():
                    add(sk, v)
        for _, sk, v in toks.values():
            self._need(eng, (sk, v))

    def _record(self, tok, reads, writes):
        for w in writes:
            self.lastw[w] = tok
            self.readers[w] = {}
        sk, v = tok
        for r in reads:
            if r in writes:
                continue
            d = self.readers.setdefault(r, {})
            if sk[0] == 'c':
                for old in [o for o in d if o[0] == 'c' and o[1] == sk[1]]:
                    del d[old]
                d[sk] = v
            else:
                d[sk] = max(d.get(sk, 0), v)

    def op(self, eng, fn, reads=(), writes=()):
        reads = tuple(reads)
        writes = tuple(writes)
        self._deps(eng, reads, writes)
        seq = self.ncomp[eng]
        self.ncomp[eng] += 1
        semkey = ('c', eng, seq // EPOCH)
        tok = (semkey, seq % EPOCH + 1)
        self.semkeys.add(semkey)
        self.stream[eng].append(('op', fn, semkey))
        self._record(tok, reads, writes)
        return tok

    def dma(self, q, fn, reads=(), writes=()):
        reads = tuple(reads)
        writes = tuple(writes)
        k = self.ndma[q]
        self.ndma[q] += 1
        slot = k % NS
        semkey = ('d', q, slot)
        self.semkeys.add(semkey)
        if k >= NS:
            self._need(q, (semkey, 16 * (k // NS)))
        self._deps(q, reads, writes)
        tok = (semkey, 16 * (k // NS + 1))
        self.stream[q].append(('dma', fn, semkey))
        self._record(tok, reads, writes)
        return tok

    def _all_tokens(self):
        toks = []
        for q in QUEUES:
            n = self.ndma[q]
            for k in range(max(0, n - NS), n):
                toks.append((('d', q, k % NS), 16 * (k // NS + 1)))
        for e in COMPUTE:
            n = self.ncomp[e]
            if n:
                toks.append((('c', e, (n - 1) // EPOCH), (n - 1) % EPOCH + 1))
        return toks

    def barrier(self):
        toks = self._all_tokens()
        for e in ('pe', 'act', 'dve', 'pool', 'sp'):
            for t in toks:
                if e == 'pe' and t[0][0] == 'c' and t[0][1] == 'pe':
                    continue
                self._need(e, t)
        self.lastw.clear()
        self.readers.clear()

    def finish(self):
        for t in self._all_tokens():
            self._need('sp', t)

    def emit(self, nc, stack):
        sems = {}
        for sk in sorted(self.semkeys):
            sems[sk] = stack.enter_context(nc.semaphore("s_" + "_".join(str(x) for x in sk)))
        block = stack.enter_context(nc.Block())
        streams = self.stream

        def run(eng, name):
            pend = []
            for item in streams[name]:
                if item[0] == 'wait':
                    pend.append(item)
                elif item[0] == 'op':
                    for w in pend[:-1]:
                        eng.wait_ge(sems[w[1]], w[2])
                    ins = item[1](eng)
                    if pend:
                        ins._wait_ge(sems[pend[-1][1]], pend[-1][2])
                    ins.then_inc(sems[item[2]], 1)
                    pend = []
                else:
                    for w in pend:
                        eng.wait_ge(sems[w[1]], w[2])
                    pend = []
                    item[1](eng).then_inc(sems[item[2]], 16)
            for w in pend:
                eng.wait_ge(sems[w[1]], w[2])

        @block.tensor
        def _(e):
            run(e, 'pe')

        @block.scalar
        def _(e):
            run(e, 'act')

        @block.vector
        def _(e):
            run(e, 'dve')

        @block.gpsimd
        def _(e):
            run(e, 'pool')

        @block.sync
        def _(e):
            run(e, 'sp')


class Buf:
    __slots__ = ('name', 'ap')

    def __init__(self, name, ap):
        self.name = name
        self.ap = ap

    def __getitem__(self, k):
        return self.ap[k]

    def __repr__(self):
        return self.name


_DTSIZE = {F32: 4, BF16: 2, I32: 4}


class Arena:
    def __init__(self, nc, stack, kib):
        self.words = kib * 256
        self.t = stack.enter_context(nc.sbuf_tensor("arena", [128, self.words], F32))
        self.off = 0
        self.n = 0

    def mark(self):
        return self.off

    def reset(self, m):
        self.off = m

    def alloc(self, name, shape, dt=F32):
        p = shape[0]
        free = int(np.prod(shape[1:]))
        words = (free * _DTSIZE[dt] + 3) // 4
        words = (words + 7) // 8 * 8
        assert self.off + words <= self.words, f"arena overflow at {name}: {self.off}+{words}>{self.words}"
        ap = self.t[0:p, self.off:self.off + words]
        self.off += words
        if dt != F32:
            ap = ap.bitcast(dt)
        ap = ap[:, 0:free]
        if len(shape) == 3:
            ap = ap.rearrange("p (a b) -> p a b", a=shape[1])
        elif len(shape) == 4:
            ap = ap.rearrange("p (a b c) -> p a b c", a=shape[1], b=shape[2])
        self.n += 1
        return Buf(f"{name}#{self.n}", ap)


SRC_RANGES = [(0, 512), (512, 640), (768, 1280), (1280, 1792),
              (640, 768), (1792, 2304),
              (3840, 4352),
              (4352, 4368),
              (2304, 3840)]
O_NK, O_V, O_Z, O_GA, O_C = 0, 1664, 2304, 2816, 2832
NSUB = 26


class K:
    pass


def build(N=8192, CTX=256, nlayers=DEPTH, stop_after=None, debug=(), nexp_decl=NEXP):
    T = CTX + N
    nc = bass.Bass("TRN2", target_bir_lowering=False)
    k = K()
    k.nc, k.N, k.CTX, k.T = nc, N, CTX, T
    k.S = S = Sched()
    global LASTS
    LASTS = S

    def din(name, shape, dt=F32):
        return nc.dram_tensor(name, list(shape), dt, kind="ExternalInput").ap()

    def dscr(name, shape, dt=F32):
        kind = "ExternalOutput" if name in debug else "Internal"
        return Buf(name, nc.dram_tensor(name, list(shape), dt, kind=kind).ap())

    I = {}
    I['x'] = din('x', [N, D])
    I['c'] = din('c', [D])
    I['ctx'] = din('ctx', [CTX, D])
    I['c_ctx'] = din('c_ctx', [D])
    I['w_mod'] = din('w_mod', [DEPTH, D, 6 * D])
    I['b_mod'] = din('b_mod', [DEPTH, 6 * D])
    I['norm1_w'] = din('norm1_w', [DEPTH, D])
    I['norm2_w'] = din('norm2_w', [DEPTH, D])
    I['w_in'] = din('w_in', [DEPTH, D, IN_COLS])
    for nm in ('gqa_q_norm', 'gqa_k_norm', 'diff_q_norm', 'diff_k_norm', 'diff_lambda_q1', 'diff_lambda_k1',
               'diff_lambda_q2', 'diff_lambda_k2'):
        I[nm] = din(nm, [DEPTH, 64])
    I['diff_subln'] = din('diff_subln', [DEPTH, 128])
    I['gdn_conv_w'] = din('gdn_conv_w', [DEPTH, 3, 1536])
    I['gdn_a_log'] = din('gdn_a_log', [DEPTH, 8])
    I['gdn_dt_bias'] = din('gdn_dt_bias', [DEPTH, 8])
    I['gdn_norm_w'] = din('gdn_norm_w', [DEPTH, 128])
    I['w_merge_gate'] = din('w_merge_gate', [DEPTH, 3, D, D])
    I['w_branch'] = din('w_branch', [DEPTH, 3, 512, D])
    I['w_out'] = din('w_out', [DEPTH, D, D])
    I['w_router'] = din('w_router', [DEPTH, D, NEXP])
    I['w_exp_gate'] = din('w_exp_gate', [DEPTH, nexp_decl, D, D])
    I['w_exp_up'] = din('w_exp_up', [DEPTH, nexp_decl, D, D])
    I['w_exp_down'] = din('w_exp_down', [DEPTH, nexp_decl, D, D])
    I['ident'] = din('ident', [128, 128])
    I['ropeC'] = din('ropeC', [N, 64])
    I['ropeS'] = din('ropeS', [N, 64])
    I['ustrict'] = din('ustrict', [128, 128])
    I['gdnc'] = din('gdnc', [2, 128, 5, 64])
    k.I = I
    out = nc.dram_tensor('out', [N, D], F32, kind="ExternalOutput").ap()
    k.out = Buf('out', out)

    k.xs = [dscr(f'xs{i}', [T, D]) for i in range(2)]
    k.hT_d = dscr('hT_d', [128, 8, T], BF16)
    k.qkT_d = dscr('qkT_d', [NSUB, 64, T], BF16)
    k.v_d = dscr('v_d', [T, 640], BF16)
    k.z_d = dscr('z_d', [T, 512], BF16)
    k.gb_d = dscr('gb_d', [T, 16])
    k.cT_d = dscr('cT_d', [12, 128, T])
    k.yT_d = dscr('yT_d', [3, 512, T], BF16)
    k.cP_d = dscr('cP_d', [12, 128, T])
    k.of_d = dscr('of_d', [T, 512])
    k.xn_d = dscr('xn_d', [T, D], BF16)
    SL = N // 8 + CTX // 8
    k.xin_d = [dscr(f'xin_d{e_}', [SL, D], BF16) for e_ in range(NEXP)]
    k.yexp_d = [dscr(f'yexp_d{e_}', [SL, D]) for e_ in range(NEXP)]

    with ExitStack() as st:
        k.st = st
        k.ar = Arena(nc, st, 200)
        k.pairs = [st.enter_context(nc.psum_tensor(f'pp{i}', [128, 1024], F32)) for i in range(4)]
        k.banks = [Buf(f'bank{i}', k.pairs[i // 2][:, (i % 2) * 512:(i % 2) * 512 + 512]) for i in range(8)]
        _program(k, nlayers, stop_after)
        S.finish()
        S.emit(nc, st)
    return nc


def bank_view(b, dt, shape):
    ap = b.ap
    if dt != F32:
        ap = ap.bitcast(dt)
    p = shape[0]
    free = int(np.prod(shape[1:]))
    ap = ap[0:p, 0:free]
    if len(shape) == 3:
        ap = ap.rearrange("p (a b) -> p a b", a=shape[1])
    return ap


def _program(k, nlayers, stop_after):
    S, ar, I = k.S, k.ar, k.I
    k.identf = ar.alloc('identf', [128, 128], F32)
    k.identb = ar.alloc('identb', [128, 128], BF16)
    k.ones_f = ar.alloc('ones_f', [128, 128], F32)
    k.ones_b = ar.alloc('ones_b', [128, 128], BF16)
    k.epst = ar.alloc('epst', [128, 1], F32)
    S.dma('sp', lambda e: e.dma_start(out=k.identf.ap, in_=I['ident']), writes=[k.identf])
    S.op('dve', lambda e: e.tensor_copy(out=k.identb.ap, in_=k.identf.ap), reads=[k.identf], writes=[k.identb])
    S.op('pool', lambda e: e.memset(k.ones_f.ap, 1.0), writes=[k.ones_f])
    S.op('pool', lambda e: e.memset(k.ones_b.ap, 1.0), writes=[k.ones_b])
    S.op('pool', lambda e: e.memset(k.epst.ap, EPS), writes=[k.epst])
    k.modb = [[(ar.alloc(f'modb{j}{i}', [128, D], F32) if i in (2, 5) else None) for i in range(6)] for j in range(2)]
    base = ar.mark()
    for li in range(nlayers):
        last = li == DEPTH - 1
        xin_lat = Buf('x_in', I['x']) if li == 0 else None
        ar.reset(base)
        phase0_mod(k, li)
        S.barrier()
        ar.reset(base)
        phase1_proj(k, li)
        S.barrier()
        if stop_after == 'p1':
            return
        ar.reset(base)
        phase2_attn(k, li, not last)
        S.barrier()
        if stop_after == 'p2':
            return
        ar.reset(base)
        phase3_gdn(k, li, not last)
        S.barrier()
        if stop_after == 'p3':
            return
        ar.reset(base)
        phase4_merge(k, li, not last)
        S.barrier()
        if stop_after == 'p4':
            return
        ar.reset(base)
        phase5_moe(k, li, not last)
        S.barrier()


def _cp(S, eng, out_b, out_ap, in_b, in_ap):
    if eng == 'act':
        S.op('act', lambda e: e.copy(out=out_ap, in_=in_ap), reads=[in_b], writes=[out_b])
    else:
        S.op(eng, lambda e: e.tensor_copy(out=out_ap, in_=in_ap), reads=[in_b], writes=[out_b])


def phase0_mod(k, li):
    S, ar, I = k.S, k.ar, k.I
    bk = k.banks
    vrow = ar.alloc('vrow', [128, 128], F32)
    S.dma('sp', lambda e: e.dma_start(out=vrow.ap[0:8, :], in_=I['c'].rearrange("(c p) -> c p", p=128)), writes=[vrow])
    S.dma('sp', lambda e: e.dma_start(out=vrow.ap[8:16, :], in_=I['c_ctx'].rearrange("(c p) -> c p", p=128)), writes=[vrow])
    S.dma('sp', lambda e: e.dma_start(out=vrow.ap[16:24, :], in_=I['norm1_w'][li].rearrange("(c p) -> c p", p=128)), writes=[vrow])
    S.dma('sp', lambda e: e.dma_start(out=vrow.ap[24:32, :], in_=I['norm2_w'][li].rearrange("(c p) -> c p", p=128)), writes=[vrow])
    S.dma('sp', lambda e: e.dma_start(out=vrow.ap[32:80, :], in_=I['b_mod'][li].rearrange("(c p) -> c p", p=128)), writes=[vrow])
    vcol = ar.alloc('vcol', [128, 80], F32)
    pv = bank_view(bk[0], F32, [128, 80])
    S.op('pe', lambda e: e.transpose(out=pv, in_=vrow.ap[0:80, :], identity=k.identf.ap[0:80, 0:80]),
         reads=[vrow, k.identf], writes=[bk[0]])
    _cp(S, 'dve', vcol, vcol.ap, bk[0], pv)
    cact = ar.alloc('cact', [128, 8, 2], F32)
    for j in range(2):
        S.op('act', lambda e, j=j: e.activation(out=cact.ap[:, :, j], in_=vcol.ap[:, j * 8:(j + 1) * 8], func=AF.Silu),
             reads=[vcol], writes=[cact])
    brow = ar.alloc('brow', [1, 6 * D], F32)
    S.dma('sp', lambda e: e.dma_start(out=brow.ap, in_=I['b_mod'][li:li + 1, :]), writes=[brow])
    modcol = ar.alloc('modcol', [128, 48, 2], F32)
    wst = [ar.alloc(f'wst{i}', [128, 8, 512], F32) for i in range(2)]
    grow = [ar.alloc(f'grow{i}', [1, 512], F32) for i in range(2)]
    k.A = [[None] * 2 for _ in range(2)]
    wsrc = I['w_mod'][li].rearrange("(c p) n -> p c n", p=128)
    nb = 2
    for n in range(12):
        w = wst[n % 2]
        S.dma('sp', lambda e, w=w, n=n: e.dma_start(out=w.ap, in_=wsrc[:, :, n * 512:(n + 1) * 512]), writes=[w])
        split = n // 2
        if split in (2, 5):
            gi = 2 if split == 2 else 5
            for j in range(2):
                pb = bk[nb % 8]; nb += 1
                for c in range(8):
                    S.op('pe', lambda e, pb=pb, w=w, c=c, j=j: e.matmul(pb.ap[0:1, :], lhsT=cact.ap[:, c, j:j + 1], rhs=w.ap[:, c, :],
                                                                      start=(c == 0), stop=(c == 7)),
                         reads=[cact, w], writes=[pb])
                g = grow[j]
                S.op('dve', lambda e, pb=pb, g=g, n=n: e.tensor_tensor(out=g.ap, in0=pb.ap[0:1, :], in1=brow.ap[:, n * 512:(n + 1) * 512], op=ALU.add),
                     reads=[pb, brow], writes=[g])
                pb2 = bk[nb % 8]; nb += 1
                S.op('pe', lambda e, pb2=pb2, g=g: e.matmul(pb2.ap, lhsT=k.ones_f.ap[0:1, :], rhs=g.ap, start=True, stop=True),
                     reads=[k.ones_f, g], writes=[pb2])
                dst = k.modb[j][gi]
                half = n % 2
                _cp(S, 'act', dst, dst.ap[:, half * 512:(half + 1) * 512], pb2, pb2.ap)
        else:
            pb = bk[nb % 8]; nb += 1
            pvv = bank_view(pb, F32, [128, 4, 2])
            for sub in range(4):
                for c in range(8):
                    S.op('pe', lambda e, pvv=pvv, w=w, c=c, sub=sub: e.matmul(pvv[:, sub, :], lhsT=w.ap[:, c, sub * 128:(sub + 1) * 128], rhs=cact.ap[:, c, :],
                                                                            start=(c == 0), stop=(c == 7)),
                         reads=[cact, w], writes=[pb])
            S.op('dve', lambda e, pvv=pvv, n=n: e.tensor_tensor(out=modcol.ap[:, n * 4:(n + 1) * 4, :], in0=pvv,
                                                              in1=vcol.ap[:, 32 + n * 4:32 + (n + 1) * 4].unsqueeze(2).broadcast_to([128, 4, 2]), op=ALU.add),
                 reads=[pb, vcol], writes=[modcol])
    k.colA1 = ar_persist(k, 'colA1', [128, 8, 2]); k.colB1 = ar_persist(k, 'colB1', [128, 8, 2])
    k.colA2 = ar_persist(k, 'colA2', [128, 8, 2]); k.colB2 = ar_persist(k, 'colB2', [128, 8, 2])
    for (dstA, dstB, sh, sc, nw) in ((k.colA1, k.colB1, 0, 1, 16), (k.colA2, k.colB2, 3, 4, 24)):
        S.op('dve', lambda e, dstA=dstA, sc=sc, nw=nw: e.scalar_tensor_tensor(
            out=dstA.ap, in0=modcol.ap[:, sc * 8:(sc + 1) * 8, :], scalar=1.0,
            in1=vcol.ap[:, nw:nw + 8].unsqueeze(2).broadcast_to([128, 8, 2]), op0=ALU.add, op1=ALU.mult),
            reads=[modcol, vcol], writes=[dstA])
        S.op('dve', lambda e, dstB=dstB, sh=sh: e.tensor_copy(out=dstB.ap, in_=modcol.ap[:, sh * 8:(sh + 1) * 8, :]),
             reads=[modcol], writes=[dstB])


def ar_persist(k, name, shape, dt=F32):
    if not hasattr(k, '_persist'):
        k._persist = {}
    if name not in k._persist:
        t = k.st.enter_context(k.nc.sbuf_tensor("P_" + name, list(shape), dt))
        k._persist[name] = Buf("P_" + name, t[:])
    return k._persist[name]


def _spans(k):
    sp = [(0, k.CTX, True)]
    for i in range(k.N // 512):
        sp.append((k.CTX + i * 512, 512, False))
    return sp


def _xsrc(k, li, tok0):
    if li == 0:
        if tok0 < k.CTX:
            return k.I['ctx'][tok0:tok0 + 128, :], None
        return k.I['x'][tok0 - k.CTX:tok0 - k.CTX + 128, :], None
    b = k.xs[1]
    return b.ap[tok0:tok0 + 128, :], b


def _norm_tile(k, src_ap, src_res, xt, sqs, ss, rstd):
    S = k.S
    S.dma('sp', lambda e: e.dma_start(out=xt.ap, in_=src_ap), reads=[src_res] if src_res else [], writes=[xt])
    S.op('act', lambda e: e.activation(out=sqs.ap.rearrange('p a b -> p (a b)')[:, 0:1024] if len(sqs.ap.shape) == 3 else sqs.ap, in_=xt.ap, func=AF.Square, scale=1.0 / 32, accum_out=ss.ap),
         reads=[xt], writes=[sqs, ss])
    S.op('act', lambda e: e.activation(out=rstd.ap, in_=ss.ap, func=AF.Sqrt, bias=k.epst.ap, scale=1.0), reads=[ss, k.epst], writes=[rstd])
    S.op('dve', lambda e: e.reciprocal(out=rstd.ap, in_=rstd.ap), reads=[rstd], writes=[rstd])


def phase1_proj(k, li):
    S, ar, I = k.S, k.ar, k.I
    bk = k.banks
    N, CTX, T = k.N, k.CTX, k.T
    wb = ar.alloc('wb', [128, 8, IN_COLS], BF16)
    wst = [ar.alloc(f'w1st{i}', [128, 8, 256], F32) for i in range(2)]
    wsrc = I['w_in'][li].rearrange("(c p) n -> p c n", p=128)
    pieces = []
    dst = 0
    for (a, b) in SRC_RANGES:
        s = a
        while s < b:
            wd = min(256, b - s)
            pieces.append((s, dst, wd))
            s += wd
            dst += wd
    for i, (s0, d0, wd) in enumerate(pieces):
        w = wst[i % 2]
        S.dma('sp', lambda e, w=w, s0=s0, wd=wd: e.dma_start(out=w.ap[:, :, 0:wd], in_=wsrc[:, :, s0:s0 + wd]), writes=[w])
        S.op(('pool', 'dve')[i % 2], lambda e, w=w, d0=d0, wd=wd: e.tensor_copy(out=wb.ap[:, :, d0:d0 + wd], in_=w.ap[:, :, 0:wd]),
             reads=[w], writes=[wb])
    nkw = ar.alloc('nkw', [128, NSUB, 64], F32)
    for nm, s0, n in (('gqa_q_norm', 0, 8), ('gqa_k_norm', 8, 2), ('diff_q_norm', 10, 8), ('diff_k_norm', 18, 8)):
        S.dma('sp', lambda e, nm=nm, s0=s0, n=n: e.dma_start(
            out=nkw.ap[:, s0:s0 + n, :], in_=I[nm][li].partition_broadcast(128).unsqueeze(1).broadcast_to([128, n, 64])), writes=[nkw])
    dtb = ar.alloc('dtb', [128, 8], F32)
    nega = ar.alloc('nega', [128, 8], F32)
    S.dma('sp', lambda e: e.dma_start(out=dtb.ap, in_=I['gdn_dt_bias'][li].partition_broadcast(128)), writes=[dtb])
    S.dma('sp', lambda e: e.dma_start(out=nega.ap, in_=I['gdn_a_log'][li].partition_broadcast(128)), writes=[nega])
    S.op('act', lambda e: e.activation(out=nega.ap, in_=nega.ap, func=AF.Exp), reads=[nega], writes=[nega])
    S.op('dve', lambda e: e.tensor_scalar(out=nega.ap, in0=nega.ap, scalar1=-1.0, scalar2=None, op0=ALU.mult), reads=[nega], writes=[nega])

    xtb = [ar.alloc(f'xt{i}', [128, D], F32) for i in range(2)]
    ssb = [ar.alloc(f'ss{i}', [128, 1], F32) for i in range(2)]
    rsb = [ar.alloc(f'rstd{i}', [128, 1], F32) for i in range(2)]
    hbb = [ar.alloc(f'hb{i}', [128, D], BF16) for i in range(2)]
    hTs = ar.alloc('hTs', [128, 8, 512], BF16)
    fst = wst
    fv = lambda f: f.ap.rearrange('p a b -> p (a b)')
    nkq = ar.alloc('nkq', [128, NSUB, 64], F32)
    t1 = ar.alloc('t1', [128, NSUB, 64], F32)
    t2 = ar.alloc('t2', [128, NSUB, 64], F32)
    sqs = t2
    ssq = ar.alloc('ssq', [128, NSUB], F32)
    rcf = ar.alloc('rcf', [128, NSUB, 64], F32)
    rsf = ar.alloc('rsf', [128, NSUB, 64], F32)
    qr = ar.alloc('qr', [128, NSUB, 64], BF16)
    qkts = ar.alloc('qkts', [64, NSUB, 512], BF16)
    vst = [ar.alloc(f'vst{i}', [128, 640], BF16) for i in range(2)]
    zst = [ar.alloc(f'zst{i}', [128, 512], BF16) for i in range(2)]
    gast = [ar.alloc(f'gast{i}', [128, 16], F32) for i in range(2)]
    gtmp = ar.alloc('gtmp', [128, 8], F32)

    tcount = 0
    for (s0, ntok, is_ctx) in _spans(k):
        jj = 1 if is_ctx else 0
        ntile = ntok // 128
        for j in range(ntile):
            tok0 = s0 + j * 128
            b = tcount % 2
            tcount += 1
            xt, ss, rstd, hb = xtb[b], ssb[b], rsb[b], hbb[b]
            src_ap, src_res = _xsrc(k, li, tok0)
            _norm_tile(k, src_ap, src_res, xt, sqs, ss, rstd)
            S.op('dve', lambda e, hb=hb, xt=xt, rstd=rstd: e.tensor_scalar(out=hb.ap, in0=xt.ap, scalar1=rstd.ap, scalar2=None, op0=ALU.mult),
                 reads=[xt, rstd], writes=[hb])
            pT = bank_view(bk[7], BF16, [128, 8, 128])
            for c in range(8):
                S.op('pe', lambda e, hb=hb, c=c: e.transpose(out=pT[:, c, :], in_=hb.ap[:, c * 128:(c + 1) * 128], identity=k.identb.ap),
                     reads=[hb, k.identb], writes=[bk[7]])
            for c in range(8):
                if c % 2 == 0:
                    S.op('dve', lambda e, c=c, j=j, jj=jj: e.tensor_scalar(
                        out=hTs.ap[:, c, j * 128:(j + 1) * 128], in0=pT[:, c, :], scalar1=k.colA1.ap[:, c, jj:jj + 1],
                        scalar2=k.colB1.ap[:, c, jj:jj + 1], op0=ALU.mult, op1=ALU.add), reads=[bk[7], k.colA1, k.colB1], writes=[hTs])
                else:
                    S.op('act', lambda e, c=c, j=j, jj=jj: e.activation(
                        out=hTs.ap[:, c, j * 128:(j + 1) * 128], in_=pT[:, c, :], func=AF.Identity, scale=k.colA1.ap[:, c, jj:jj + 1],
                        bias=k.colB1.ap[:, c, jj:jj + 1]), reads=[bk[7], k.colA1, k.colB1], writes=[hTs])
        S.dma('sp', lambda e, s0=s0, ntok=ntok: e.dma_start(out=k.hT_d.ap[:, :, s0:s0 + ntok], in_=hTs.ap[:, :, 0:ntok]),
              reads=[hTs], writes=[k.hT_d])
        for cc in range(12):
            pb = bk[4 + cc % 3]
            for c in range(8):
                S.op('pe', lambda e, pb=pb, c=c, cc=cc, ntok=ntok: e.matmul(
                    pb.ap[:, 0:ntok], lhsT=wb.ap[:, c, O_C + cc * 128:O_C + (cc + 1) * 128], rhs=hTs.ap[:, c, 0:ntok],
                    start=(c == 0), stop=(c == 7)), reads=[wb, hTs], writes=[pb])
            f = fst[cc % 2]
            _cp(S, 'act' if cc % 2 else 'dve', f, fv(f)[:, 0:ntok], pb, pb.ap[:, 0:ntok])
            S.dma('sp', lambda e, f=f, cc=cc, s0=s0, ntok=ntok: e.dma_start(out=k.cT_d.ap[cc, :, s0:s0 + ntok], in_=fv(f)[:, 0:ntok]),
                  reads=[f], writes=[k.cT_d])
        for j in range(ntile):
            tok0 = s0 + j * 128
            b = j % 2
            lt = lambda c: hTs.ap[:, c, j * 128:(j + 1) * 128]
            for g, (c0, wd) in enumerate(((0, 512), (512, 512), (1024, 512), (1536, 128))):
                for c in range(8):
                    S.op('pe', lambda e, g=g, c=c, c0=c0, wd=wd, j=j: e.matmul(
                        bk[g].ap[:, 0:wd], lhsT=hTs.ap[:, c, j * 128:(j + 1) * 128], rhs=wb.ap[:, c, O_NK + c0:O_NK + c0 + wd],
                        start=(c == 0), stop=(c == 7)), reads=[wb, hTs], writes=[bk[g]])
            nkflat = nkq.ap.rearrange("p a b -> p (a b)")
            for g, (c0, wd) in enumerate(((0, 512), (512, 512), (1024, 512), (1536, 128))):
                _cp(S, 'act' if g % 2 else 'dve', nkq, nkflat[:, c0:c0 + wd], bk[g], bk[g].ap[:, 0:wd])
            for (pb, p0, c0, wd) in ((bk[4], 0, O_V, 512), (bk[5], 0, O_V + 512, 128), (bk[6], 0, O_Z, 512), (bk[5], 128, O_GA, 16)):
                for c in range(8):
                    S.op('pe', lambda e, pb=pb, p0=p0, c=c, c0=c0, wd=wd, j=j: e.matmul(
                        pb.ap[:, p0:p0 + wd], lhsT=hTs.ap[:, c, j * 128:(j + 1) * 128], rhs=wb.ap[:, c, c0:c0 + wd],
                        start=(c == 0), stop=(c == 7)), reads=[wb, hTs], writes=[pb])
            v, z, ga = vst[b], zst[b], gast[b]
            _cp(S, 'dve', v, v.ap[:, 0:512], bk[4], bk[4].ap)
            _cp(S, 'dve', v, v.ap[:, 512:640], bk[5], bk[5].ap[:, 0:128])
            S.dma('sp', lambda e, v=v, tok0=tok0: e.dma_start(out=k.v_d.ap[tok0:tok0 + 128, :], in_=v.ap), reads=[v], writes=[k.v_d])
            S.op('act', lambda e, z=z: e.activation(out=z.ap, in_=bk[6].ap, func=AF.Silu), reads=[bk[6]], writes=[z])
            S.dma('sp', lambda e, z=z, tok0=tok0: e.dma_start(out=k.z_d.ap[tok0:tok0 + 128, :], in_=z.ap), reads=[z], writes=[k.z_d])
            S.op('act', lambda e, ga=ga: e.activation(out=ga.ap[:, 0:8], in_=bk[5].ap[:, 128:136], func=AF.Sigmoid), reads=[bk[5]], writes=[ga])
            S.op('dve', lambda e: e.tensor_tensor(out=gtmp.ap, in0=bk[5].ap[:, 136:144], in1=dtb.ap, op=ALU.add), reads=[bk[5], dtb], writes=[gtmp])
            S.op('act', lambda e: e.activation(out=gtmp.ap, in_=gtmp.ap, func=AF.Exp), reads=[gtmp], writes=[gtmp])
            S.op('act', lambda e: e.activation(out=gtmp.ap, in_=gtmp.ap, func=AF.Ln, bias=1.0, scale=1.0), reads=[gtmp], writes=[gtmp])
            S.op('dve', lambda e, ga=ga: e.tensor_tensor(out=ga.ap[:, 8:16], in0=gtmp.ap, in1=nega.ap, op=ALU.mult), reads=[gtmp, nega], writes=[ga])
            S.dma('sp', lambda e, ga=ga, tok0=tok0: e.dma_start(out=k.gb_d.ap[tok0:tok0 + 128, :], in_=ga.ap), reads=[ga], writes=[k.gb_d])
            S.op('pool', lambda e: e.tensor_tensor(out=t1.ap, in0=nkq.ap, in1=nkq.ap, op=ALU.mult), reads=[nkq], writes=[t1])
            S.op('dve', lambda e: e.tensor_reduce(out=ssq.ap, in_=t1.ap, axis=AX.X, op=ALU.add), reads=[t1], writes=[ssq])
            S.op('act', lambda e: e.activation(out=ssq.ap, in_=ssq.ap, func=AF.Sqrt, bias=k.epst.ap, scale=1.0 / 64), reads=[ssq, k.epst], writes=[ssq])
            S.op('dve', lambda e: e.reciprocal(out=ssq.ap, in_=ssq.ap), reads=[ssq], writes=[ssq])
            S.op('dve', lambda e: e.tensor_tensor(out=t1.ap, in0=nkq.ap, in1=ssq.ap.unsqueeze(2).broadcast_to([128, NSUB, 64]), op=ALU.mult),
                 reads=[nkq, ssq], writes=[t1])
            if is_ctx:
                S.op('pool', lambda e: e.tensor_tensor(out=qr.ap, in0=t1.ap, in1=nkw.ap, op=ALU.mult), reads=[t1, nkw], writes=[qr])
            else:
                S.op('pool', lambda e: e.tensor_tensor(out=nkq.ap, in0=t1.ap, in1=nkw.ap, op=ALU.mult), reads=[t1, nkw], writes=[nkq])
                r0 = tok0 - CTX
                S.dma('sp', lambda e, r0=r0: e.dma_start(out=rcf.ap, in_=I['ropeC'][r0:r0 + 128, :].unsqueeze(1).broadcast_to([128, NSUB, 64])), writes=[rcf])
                S.dma('sp', lambda e, r0=r0: e.dma_start(out=rsf.ap, in_=I['ropeS'][r0:r0 + 128, :].unsqueeze(1).broadcast_to([128, NSUB, 64])), writes=[rsf])
                S.op('dve', lambda e: e.tensor_tensor(out=t1.ap, in0=nkq.ap, in1=rcf.ap, op=ALU.mult), reads=[nkq, rcf], writes=[t1])
                qv = nkq.ap.rearrange("p a (g two s) -> p (a g) two s", g=2, two=2)
                sv = rsf.ap.rearrange("p a (g two s) -> p (a g) two s", g=2, two=2)
                tv = t2.ap.rearrange("p a (g two s) -> p (a g) two s", g=2, two=2)
                S.op('pool', lambda e: e.tensor_tensor(out=tv[:, :, 0, :], in0=qv[:, :, 1, :], in1=sv[:, :, 0, :], op=ALU.mult), reads=[nkq, rsf], writes=[t2])
                S.op('pool', lambda e: e.tensor_tensor(out=tv[:, :, 1, :], in0=qv[:, :, 0, :], in1=sv[:, :, 1, :], op=ALU.mult), reads=[nkq, rsf], writes=[t2])
                S.op('dve', lambda e: e.tensor_tensor(out=qr.ap, in0=t1.ap, in1=t2.ap, op=ALU.add), reads=[t1, t2], writes=[qr])
            for b0, nb_ in ((0, 8), (8, 8), (16, 8), (24, 2)):
                pq = bank_view(bk[7], BF16, [64, 8, 128])
                for i in range(nb_):
                    S.op('pe', lambda e, i=i, b0=b0: e.transpose(out=pq[:, i, :], in_=qr.ap[:, b0 + i, :], identity=k.identb.ap),
                         reads=[qr, k.identb], writes=[bk[7]])
                _cp(S, 'act', qkts, qkts.ap[:, b0:b0 + nb_, j * 128:(j + 1) * 128], bk[7], pq[:, 0:nb_, :])
        S.dma('sp', lambda e, s0=s0, ntok=ntok: e.dma_start(out=k.qkT_d.ap[:, :, s0:s0 + ntok].rearrange("s d t -> d s t"), in_=qkts.ap[:, :, 0:ntok]),
              reads=[qkts], writes=[k.qkT_d])


def lambda_init(li):
    return 0.8 - 0.6 * math.exp(-0.3 * li)


def phase2_attn(k, li, need_ctx):
    S, ar, I = k.S, k.ar, k.I
    bk = k.banks
    N, CTX, T = k.N, k.CTX, k.T
    nkb = T // 128
    linit = lambda_init(li)
    lv = ar.alloc('lv', [128, 4, 64], F32)
    for i, nm in enumerate(('diff_lambda_q1', 'diff_lambda_k1', 'diff_lambda_q2', 'diff_lambda_k2')):
        S.dma('sp', lambda e, i=i, nm=nm: e.dma_start(out=lv.ap[:, i, :], in_=I[nm][li].partition_broadcast(128)), writes=[lv])
    lp = ar.alloc('lp', [128, 2, 64], F32)
    ls = ar.alloc('ls', [128, 2], F32)
    neglam = ar.alloc('neglam', [128, 1], F32)
    sublc = ar.alloc('sublc', [128, 1], F32)
    for i in range(2):
        S.op('dve', lambda e, i=i: e.tensor_tensor(out=lp.ap[:, i, :], in0=lv.ap[:, 2 * i, :], in1=lv.ap[:, 2 * i + 1, :], op=ALU.mult), reads=[lv], writes=[lp])
    S.op('dve', lambda e: e.tensor_reduce(out=ls.ap, in_=lp.ap, axis=AX.X, op=ALU.add), reads=[lp], writes=[ls])
    S.op('act', lambda e: e.activation(out=ls.ap, in_=ls.ap, func=AF.Exp), reads=[ls], writes=[ls])
    S.op('dve', lambda e: e.tensor_tensor(out=neglam.ap, in0=ls.ap[:, 1:2], in1=ls.ap[:, 0:1], op=ALU.subtract), reads=[ls], writes=[neglam])
    S.op('dve', lambda e: e.tensor_scalar(out=neglam.ap, in0=neglam.ap, scalar1=-linit, scalar2=None, op0=ALU.add), reads=[neglam], writes=[neglam])
    S.dma('sp', lambda e: e.dma_start(out=sublc.ap, in_=I['diff_subln'][li].rearrange("(p o) -> p o", o=1)), writes=[sublc])
    S.op('dve', lambda e: e.tensor_scalar(out=sublc.ap, in0=sublc.ap, scalar1=1.0 - linit, scalar2=None, op0=ALU.mult), reads=[sublc], writes=[sublc])

    KT = [ar.alloc(f'KT{i}', [64, 2, T], BF16) for i in range(2)]
    VT = [ar.alloc(f'VT{i}', [128, nkb, 130], BF16) for i in range(2)]
    for i in range(2):
        S.op('pool', lambda e, i=i: e.memset(VT[i].ap[:, :, 64:65], 1.0), writes=[VT[i]])
    QT = [ar.alloc(f'QT{i}', [64, 512], BF16) for i in range(3)]
    PT = [ar.alloc(f'PT{i}', [128, 1024], BF16) for i in range(3)]
    rs = ar.alloc('rs', [128, 512], F32)
    bcs = ar.alloc('bcs', [128, 512], F32)
    o1 = ar.alloc('o1', [128, 512], F32)
    o2 = ar.alloc('o2', [128, 512], F32)
    sq = ar.alloc('sq', [128, 512], F32)
    yst = [ar.alloc(f'yst{i}', [128, 512], BF16) for i in range(2)]

    heads = []
    for h in range(8):
        heads.append(dict(kind='gqa', subs=[(h, 8 + h // 4)], vc0=(h // 4) * 64, dv=64, br=0, row0=h * 64))
    for h in range(4):
        heads.append(dict(kind='diff', subs=[(10 + 2 * h, 18 + 2 * h), (10 + 2 * h + 1, 18 + 2 * h + 1)], vc0=128 + h * 128, dv=128, br=1, row0=h * 128))
    chunks = [(CTX + i * 512, 512, 0, nkb) for i in range(N // 512)]
    if need_ctx:
        chunks = [(0, CTX, 0, CTX // 128)] + chunks
    cnt = dict(s=0, o=0, q=0, p=0, y=0)
    for hi, hd in enumerate(heads):
        kt, vt = KT[hi % 2], VT[hi % 2]
        dv = hd['dv']
        gqa = hd['kind'] == 'gqa'
        for ci, (qs, ks) in enumerate(hd['subs']):
            S.dma('sp', lambda e, kt=kt, ci=ci, ks=ks: e.dma_start(out=kt.ap[:, ci, :], in_=k.qkT_d.ap[ks, :, :]), reads=[k.qkT_d], writes=[kt])
        vdst = vt.ap[:, :, 0:64] if gqa else vt.ap[:, :, 0:128]
        S.dma('sp', lambda e, vdst=vdst, hd=hd, dv=dv: e.dma_start(
            out=vdst, in_=k.v_d.ap[:, hd['vc0']:hd['vc0'] + dv].rearrange("(b p) d -> p b d", p=128)), reads=[k.v_d], writes=[vt])
        if not gqa:
            pass
        elif hi > 0 and heads[hi - 2 if hi >= 2 else 0]['kind'] != 'gqa':
            pass
        for (t0, nq, kb0, kb1) in chunks:
            for ci, (qs, ks) in enumerate(hd['subs']):
                qt = QT[cnt['q'] % 3]; cnt['q'] += 1
                S.dma('sp', lambda e, qt=qt, qs=qs, t0=t0, nq=nq: e.dma_start(out=qt.ap[:, 0:nq], in_=k.qkT_d.ap[qs, :, t0:t0 + nq]),
                      reads=[k.qkT_d], writes=[qt])
                po = bk[4 + cnt['o'] % 2]
                psm = bk[6]
                cnt['o'] += 1
                M = 65 if gqa else 128
                for kb in range(kb0, kb1, 2):
                    pi = cnt['s'] % 2; cnt['s'] += 1
                    psA, psB = bk[2 * pi], bk[2 * pi + 1]
                    pt = PT[cnt['p'] % 3]; cnt['p'] += 1
                    for u, ps in enumerate((psA, psB)):
                        S.op('pe', lambda e, ps=ps, kt=kt, ci=ci, kb=kb + u, qt=qt, nq=nq: e.matmul(
                            ps.ap[:, 0:nq], lhsT=kt.ap[:, ci, kb * 128:(kb + 1) * 128], rhs=qt.ap[:, 0:nq], start=True, stop=True),
                            reads=[kt, qt], writes=[ps])
                    if nq == 512:
                        S.op('act', lambda e, pi=pi, pt=pt: e.activation(out=pt.ap, in_=k.pairs[pi][:, :], func=AF.Exp, scale=0.125),
                             reads=[psA, psB], writes=[pt])
                    else:
                        for u, ps in enumerate((psA, psB)):
                            S.op('act', lambda e, ps=ps, pt=pt, nq=nq, u=u: e.activation(out=pt.ap[:, u * 512:u * 512 + nq], in_=ps.ap[:, 0:nq], func=AF.Exp, scale=0.125),
                                 reads=[ps], writes=[pt])
                    for u in range(2):
                        kbu = kb + u
                        S.op('pe', lambda e, po=po, vt=vt, kbu=kbu, pt=pt, nq=nq, M=M, u=u: e.matmul(
                            po.ap[0:M, 0:nq], lhsT=vt.ap[:, kbu, 0:M], rhs=pt.ap[:, u * 512:u * 512 + nq], start=(kbu == kb0), stop=(kbu == kb1 - 1)),
                            reads=[vt, pt], writes=[po])
                        if not gqa:
                            S.op('pe', lambda e, psm=psm, kbu=kbu, pt=pt, nq=nq, u=u: e.matmul(
                                psm.ap[0:1, 0:nq], lhsT=k.ones_b.ap[:, 0:1], rhs=pt.ap[:, u * 512:u * 512 + nq], start=(kbu == kb0), stop=(kbu == kb1 - 1)),
                                reads=[k.ones_b, pt], writes=[psm])
                pbc = bk[7]
                if gqa:
                    S.op('dve', lambda e, po=po, nq=nq: e.reciprocal(out=rs.ap[64:65, 0:nq], in_=po.ap[64:65, 0:nq]), reads=[po], writes=[rs])
                    S.op('pe', lambda e, nq=nq: e.matmul(pbc.ap[0:64, 0:nq], lhsT=k.ones_f.ap[64:65, 0:64], rhs=rs.ap[64:65, 0:nq], start=True, stop=True),
                         reads=[k.ones_f, rs], writes=[pbc])
                    _cp(S, 'act', bcs, bcs.ap[0:64, 0:nq], pbc, pbc.ap[0:64, 0:nq])
                    y = yst[cnt['y'] % 2]; cnt['y'] += 1
                    S.op('dve', lambda e, y=y, po=po, nq=nq: e.tensor_tensor(out=y.ap[0:64, 0:nq], in0=po.ap[0:64, 0:nq], in1=bcs.ap[0:64, 0:nq], op=ALU.mult),
                         reads=[po, bcs], writes=[y])
                    S.dma('sp', lambda e, y=y, hd=hd, t0=t0, nq=nq: e.dma_start(out=k.yT_d.ap[0, hd['row0']:hd['row0'] + 64, t0:t0 + nq], in_=y.ap[0:64, 0:nq]),
                          reads=[y], writes=[k.yT_d])
                else:
                    S.op('dve', lambda e, psm=psm, nq=nq: e.reciprocal(out=rs.ap[0:1, 0:nq], in_=psm.ap[0:1, 0:nq]), reads=[psm], writes=[rs])
                    S.op('pe', lambda e, nq=nq: e.matmul(pbc.ap[:, 0:nq], lhsT=k.ones_f.ap[0:1, :], rhs=rs.ap[0:1, 0:nq], start=True, stop=True),
                         reads=[k.ones_f, rs], writes=[pbc])
                    _cp(S, 'act', bcs, bcs.ap[:, 0:nq], pbc, pbc.ap[:, 0:nq])
                    if ci == 0:
                        S.op('dve', lambda e, po=po, nq=nq: e.tensor_tensor(out=o1.ap[:, 0:nq], in0=po.ap[:, 0:nq], in1=bcs.ap[:, 0:nq], op=ALU.mult),
                             reads=[po, bcs], writes=[o1])
                    else:
                        S.op('dve', lambda e, po=po, nq=nq: e.tensor_tensor(out=o2.ap[:, 0:nq], in0=po.ap[:, 0:nq], in1=bcs.ap[:, 0:nq], op=ALU.mult),
                             reads=[po, bcs], writes=[o2])
                        S.op('dve', lambda e, nq=nq: e.scalar_tensor_tensor(out=o1.ap[:, 0:nq], in0=o2.ap[:, 0:nq], scalar=neglam.ap, in1=o1.ap[:, 0:nq],
                                                                           op0=ALU.mult, op1=ALU.add), reads=[o2, neglam, o1], writes=[o1])
                        S.op('pool', lambda e, nq=nq: e.tensor_tensor(out=sq.ap[:, 0:nq], in0=o1.ap[:, 0:nq], in1=o1.ap[:, 0:nq], op=ALU.mult), reads=[o1], writes=[sq])
                        S.op('pe', lambda e, nq=nq: e.matmul(pbc.ap[:, 0:nq], lhsT=k.ones_f.ap, rhs=sq.ap[:, 0:nq], start=True, stop=True),
                             reads=[k.ones_f, sq], writes=[pbc])
                        S.op('act', lambda e, nq=nq: e.activation(out=bcs.ap[:, 0:nq], in_=pbc.ap[:, 0:nq], func=AF.Sqrt, bias=k.epst.ap, scale=1.0 / 128),
                             reads=[pbc, k.epst], writes=[bcs])
                        S.op('dve', lambda e, nq=nq: e.reciprocal(out=bcs.ap[:, 0:nq], in_=bcs.ap[:, 0:nq]), reads=[bcs], writes=[bcs])
                        S.op('dve', lambda e, nq=nq: e.tensor_tensor(out=o1.ap[:, 0:nq], in0=o1.ap[:, 0:nq], in1=bcs.ap[:, 0:nq], op=ALU.mult), reads=[o1, bcs], writes=[o1])
                        y = yst[cnt['y'] % 2]; cnt['y'] += 1
                        S.op('act', lambda e, y=y, nq=nq: e.activation(out=y.ap[:, 0:nq], in_=o1.ap[:, 0:nq], func=AF.Identity, scale=sublc.ap),
                             reads=[o1, sublc], writes=[y])
                        S.dma('sp', lambda e, y=y, hd=hd, t0=t0, nq=nq: e.dma_start(out=k.yT_d.ap[1, hd['row0']:hd['row0'] + 128, t0:t0 + nq], in_=y.ap[:, 0:nq]),
                              reads=[y], writes=[k.yT_d])


def _load_cast(k, dst, dst_ap_fn, src_ap_fn, nchunks, stage, wd, idx0=0):
    S = k.S
    for i in range(nchunks):
        w = stage[(idx0 + i) % 2]
        sap = src_ap_fn(i)
        kc = sap.shape[1]
        S.dma('sp', lambda e, w=w, sap=sap, kc=kc: e.dma_start(out=w.ap[:, 0:kc, 0:wd], in_=sap), writes=[w])
        S.op(('pool', 'dve')[i % 2], lambda e, w=w, i=i, kc=kc: e.tensor_copy(out=dst_ap_fn(i), in_=w.ap[:, 0:kc, 0:wd]), reads=[w], writes=[dst])


def phase4_merge(k, li, need_ctx):
    S, ar, I = k.S, k.ar, k.I
    bk = k.banks
    N, CTX, T = k.N, k.CTX, k.T
    wg = ar.alloc('wg', [128, 24, D], BF16)
    wbr = ar.alloc('wbr', [128, 12, D], BF16)
    wo = ar.alloc('wo', [128, 8, D], BF16)
    stage = [ar.alloc(f'st4{i}', [128, 8, 256], F32) for i in range(2)]
    for i in range(3):
        src = I['w_merge_gate'][li, i].rearrange("(c p) n -> p c n", p=128)
        _load_cast(k, wg, lambda q, i=i: wg.ap[:, i * 8:(i + 1) * 8, q * 256:(q + 1) * 256], lambda q, src=src: src[:, :, q * 256:(q + 1) * 256], 4, stage, 256)
        srcb = I['w_branch'][li, i].rearrange("(c p) n -> p c n", p=128)
        _load_cast(k, wbr, lambda q, i=i: wbr.ap[:, i * 4:(i + 1) * 4, q * 256:(q + 1) * 256], lambda q, srcb=srcb: srcb[:, :, q * 256:(q + 1) * 256], 4, stage, 256)
    srco = I['w_out'][li].rearrange("(c p) n -> p c n", p=128)
    _load_cast(k, wo, lambda q: wo.ap[:, :, q * 256:(q + 1) * 256], lambda q: srco[:, :, q * 256:(q + 1) * 256], 4, stage, 256)
    hTs = ar.alloc('hTs4', [128, 8, 512], BF16)
    ys = ar.alloc('ys4', [128, 12, 512], BF16)
    mT = ar.alloc('mT', [128, 8, 512], BF16)
    sg = [ar.alloc(f'sg{i}', [128, 512], F32) for i in range(2)]
    acc = ar.alloc('acc4', [128, 512], F32)
    prod = ar.alloc('prod4', [128, 512], F32)
    xtb = [ar.alloc(f'xt4{i}', [128, D], F32) for i in range(2)]
    tmp = ar.alloc('tmp4', [128, D], F32)
    cnt = 0
    tc = 0
    for (s0, ntok, is_ctx) in _spans(k):
        if is_ctx and not need_ctx:
            continue
        jj = 1 if is_ctx else 0
        S.dma('sp', lambda e, s0=s0, ntok=ntok: e.dma_start(out=hTs.ap[:, :, 0:ntok], in_=k.hT_d.ap[:, :, s0:s0 + ntok]), reads=[k.hT_d], writes=[hTs])
        for i in range(3):
            S.dma('sp', lambda e, i=i, s0=s0, ntok=ntok: e.dma_start(
                out=ys.ap[:, i * 4:(i + 1) * 4, 0:ntok], in_=k.yT_d.ap[i, :, s0:s0 + ntok].rearrange("(c p) t -> p c t", p=128)),
                reads=[k.yT_d], writes=[ys])
        for oc in range(8):
            for i in range(3):
                pa = bk[cnt % 2]; pb = bk[2 + cnt % 2]; s_ = sg[cnt % 2]; cnt += 1
                for c in range(8):
                    S.op('pe', lambda e, pa=pa, i=i, c=c, oc=oc, ntok=ntok: e.matmul(
                        pa.ap[:, 0:ntok], lhsT=wg.ap[:, i * 8 + c, oc * 128:(oc + 1) * 128], rhs=hTs.ap[:, c, 0:ntok], start=(c == 0), stop=(c == 7)),
                        reads=[wg, hTs], writes=[pa])
                for c in range(4):
                    S.op('pe', lambda e, pb=pb, i=i, c=c, oc=oc, ntok=ntok: e.matmul(
                        pb.ap[:, 0:ntok], lhsT=wbr.ap[:, i * 4 + c, oc * 128:(oc + 1) * 128], rhs=ys.ap[:, i * 4 + c, 0:ntok], start=(c == 0), stop=(c == 3)),
                        reads=[wbr, ys], writes=[pb])
                S.op('act', lambda e, pa=pa, s_=s_, ntok=ntok: e.activation(out=s_.ap[:, 0:ntok], in_=pa.ap[:, 0:ntok], func=AF.Sigmoid), reads=[pa], writes=[s_])
                if i == 0:
                    S.op('dve', lambda e, pb=pb, s_=s_, ntok=ntok: e.tensor_tensor(out=acc.ap[:, 0:ntok], in0=pb.ap[:, 0:ntok], in1=s_.ap[:, 0:ntok], op=ALU.mult),
                         reads=[pb, s_], writes=[acc])
                else:
                    S.op('dve', lambda e, pb=pb, s_=s_, ntok=ntok: e.tensor_tensor(out=prod.ap[:, 0:ntok], in0=pb.ap[:, 0:ntok], in1=s_.ap[:, 0:ntok], op=ALU.mult),
                         reads=[pb, s_], writes=[prod])
                    if i == 1:
                        S.op('pool', lambda e, ntok=ntok: e.tensor_tensor(out=acc.ap[:, 0:ntok], in0=acc.ap[:, 0:ntok], in1=prod.ap[:, 0:ntok], op=ALU.add),
                             reads=[acc, prod], writes=[acc])
                    else:
                        S.op('pool', lambda e, oc=oc, ntok=ntok: e.tensor_tensor(out=mT.ap[:, oc, 0:ntok], in0=acc.ap[:, 0:ntok], in1=prod.ap[:, 0:ntok], op=ALU.add),
                             reads=[acc, prod], writes=[mT])
        for j in range(ntok // 128):
            tok0 = s0 + j * 128
            xt = xtb[tc % 2]; tc += 1
            src_ap, src_res = _xsrc(k, li, tok0)
            S.dma('sp', lambda e, xt=xt, src_ap=src_ap: e.dma_start(out=xt.ap, in_=src_ap), reads=[src_res] if src_res else [], writes=[xt])
            for g in range(2):
                pb = bk[4 + g]
                for c in range(8):
                    S.op('pe', lambda e, pb=pb, g=g, c=c, j=j: e.matmul(pb.ap, lhsT=mT.ap[:, c, j * 128:(j + 1) * 128], rhs=wo.ap[:, c, g * 512:(g + 1) * 512],
                                                                      start=(c == 0), stop=(c == 7)), reads=[mT, wo], writes=[pb])
                S.op('dve', lambda e, pb=pb, g=g, jj=jj: e.tensor_tensor(out=tmp.ap[:, g * 512:(g + 1) * 512], in0=pb.ap, in1=k.modb[jj][2].ap[:, g * 512:(g + 1) * 512], op=ALU.mult),
                     reads=[pb, k.modb[jj][2]], writes=[tmp])
            S.op('pool', lambda e, xt=xt: e.tensor_tensor(out=xt.ap, in0=xt.ap, in1=tmp.ap, op=ALU.add), reads=[xt, tmp], writes=[xt])
            S.dma('sp', lambda e, xt=xt, tok0=tok0: e.dma_start(out=k.xs[0].ap[tok0:tok0 + 128, :], in_=xt.ap), reads=[xt], writes=[k.xs[0]])


def _breg(k, e, val):
    if not hasattr(k, '_bregs'):
        k._bregs = {}
    if val not in k._bregs:
        k._bregs[val] = e.to_reg(int(val))
    return k._bregs[val]


def phase5_moe(k, li, need_ctx):
    S, ar, I = k.S, k.ar, k.I
    bk = k.banks
    N, CTX, T = k.N, k.CTX, k.T
    cap = N // 8
    capc = CTX // 8
    SLOTS = cap + (capc if need_ctx else 0)
    BIG = 1.0e6
    t_first = 0 if need_ctx else CTX // 128
    ntile = T // 128
    tiles = list(range(t_first, ntile))
    wr = ar.alloc('wr', [128, 8, NEXP], F32)
    S.dma('sp', lambda e: e.dma_start(out=wr.ap, in_=I['w_router'][li].rearrange("(c p) n -> p c n", p=128)), writes=[wr])
    ustr = ar.alloc('ustr', [128, 128], BF16)
    ustf = ar.alloc('ustf', [128, 128], F32)
    S.dma('sp', lambda e: e.dma_start(out=ustf.ap, in_=I['ustrict']), writes=[ustf])
    S.op('dve', lambda e: e.tensor_copy(out=ustr.ap, in_=ustf.ap), reads=[ustf], writes=[ustr])
    affTM = ar.alloc('affTM', [128, ntile, NEXP], F32)
    maskw = ar.alloc('maskw', [128, ntile, NEXP], F32)
    maskb = ar.alloc('maskb', [128, ntile, NEXP], BF16)
    idxf = ar.alloc('idxf', [128, ntile, NEXP], F32)
    idxi = ar.alloc('idxi', [128, ntile, NEXP], I32)
    m_keep = ar.mark()
    affT = ar.alloc('affT', [NEXP, T], F32)
    m5 = ar.mark()
    xtb = [ar.alloc(f'xt5{i}', [128, D], F32) for i in range(2)]
    sqs = ar.alloc('sq5', [128, D], F32)
    ssb = [ar.alloc(f'ss5{i}', [128, 1], F32) for i in range(2)]
    rsb = [ar.alloc(f'rs5{i}', [128, 1], F32) for i in range(2)]
    xnf = [ar.alloc(f'xnf{i}', [128, D], F32) for i in range(2)]
    xnb = [ar.alloc(f'xnb{i}', [128, D], BF16) for i in range(2)]
    h2T = [ar.alloc(f'h2T{i}', [128, 8, 128], F32) for i in range(2)]
    mx = ar.alloc('mx', [128, 1], F32)
    sm = ar.alloc('sm', [128, 1], F32)
    ex = ar.alloc('ex', [128, NEXP], F32)
    for ti, t in enumerate(tiles):
        tok0 = t * 128
        jj = 1 if tok0 < CTX else 0
        b = ti % 2
        xt, ss, rstd = xtb[b], ssb[b], rsb[b]
        _norm_tile(k, k.xs[0].ap[tok0:tok0 + 128, :], k.xs[0], xt, sqs, ss, rstd)
        S.op('dve', lambda e, b=b, xt=xt, rstd=rstd: e.tensor_scalar(out=xnf[b].ap, in0=xt.ap, scalar1=rstd.ap, scalar2=None, op0=ALU.mult),
             reads=[xt, rstd], writes=[xnf[b]])
        S.op('pool', lambda e, b=b: e.tensor_copy(out=xnb[b].ap, in_=xnf[b].ap), reads=[xnf[b]], writes=[xnb[b]])
        S.dma('sp', lambda e, b=b, tok0=tok0: e.dma_start(out=k.xn_d.ap[tok0:tok0 + 128, :], in_=xnb[b].ap), reads=[xnb[b]], writes=[k.xn_d])
        for half in range(2):
            pT = bank_view(bk[half], F32, [128, 4, 128])
            for c4 in range(4):
                c = half * 4 + c4
                S.op('pe', lambda e, b=b, c=c, c4=c4, pT=pT: e.transpose(out=pT[:, c4, :], in_=xnf[b].ap[:, c * 128:(c + 1) * 128], identity=k.identf.ap),
                     reads=[xnf[b], k.identf], writes=[bk[half]])
            for c4 in range(4):
                c = half * 4 + c4
                S.op('dve' if c4 % 2 else 'act', (lambda e, b=b, c=c, c4=c4, pT=pT, jj=jj: e.tensor_scalar(
                    out=h2T[b].ap[:, c, :], in0=pT[:, c4, :], scalar1=k.colA2.ap[:, c, jj:jj + 1], scalar2=k.colB2.ap[:, c, jj:jj + 1], op0=ALU.mult, op1=ALU.add))
                    if c4 % 2 else (lambda e, b=b, c=c, c4=c4, pT=pT, jj=jj: e.activation(
                        out=h2T[b].ap[:, c, :], in_=pT[:, c4, :], func=AF.Identity, scale=k.colA2.ap[:, c, jj:jj + 1], bias=k.colB2.ap[:, c, jj:jj + 1])),
                    reads=[bk[half], k.colA2, k.colB2], writes=[h2T[b]])
        pl = bk[2 + ti % 2]
        for c in range(8):
            S.op('pe', lambda e, pl=pl, b=b, c=c: e.matmul(pl.ap[:, 0:NEXP], lhsT=h2T[b].ap[:, c, :], rhs=wr.ap[:, c, :], start=(c == 0), stop=(c == 7)),
                 reads=[h2T[b], wr], writes=[pl])
        S.op('dve', lambda e, pl=pl: e.tensor_reduce(out=mx.ap, in_=pl.ap[:, 0:NEXP], axis=AX.X, op=ALU.max), reads=[pl], writes=[mx])
        S.op('dve', lambda e: e.tensor_scalar(out=mx.ap, in0=mx.ap, scalar1=-1.0, scalar2=None, op0=ALU.mult), reads=[mx], writes=[mx])
        S.op('act', lambda e, pl=pl: e.activation(out=ex.ap, in_=pl.ap[:, 0:NEXP], func=AF.Exp, bias=mx.ap, scale=1.0, accum_out=sm.ap), reads=[pl, mx], writes=[ex, sm])
        S.op('dve', lambda e: e.reciprocal(out=sm.ap, in_=sm.ap), reads=[sm], writes=[sm])
        S.op('dve', lambda e, t=t: e.tensor_scalar(out=affTM.ap[:, t, :], in0=ex.ap, scalar1=sm.ap, scalar2=None, op0=ALU.mult), reads=[ex, sm], writes=[affTM])
        pa = bk[4 + ti % 2]
        S.op('pe', lambda e, pa=pa, t=t: e.transpose(out=pa.ap[0:NEXP, 0:128], in_=affTM.ap[:, t, :], identity=k.identf.ap), reads=[affTM, k.identf], writes=[pa])
        _cp(S, 'act', affT, affT.ap[:, tok0:tok0 + 128], pa, pa.ap[0:NEXP, 0:128])
    ar.reset(m5)
    scr = ar.alloc('scr5', [NEXP, max(N, CTX)], F32)
    lo = ar.alloc('lo', [NEXP, 2], F32)
    hi = ar.alloc('hi', [NEXP, 2], F32)
    mid = ar.alloc('mid', [NEXP, 2], F32)
    cn = ar.alloc('cn', [NEXP, 2], F32)
    dl = ar.alloc('dl', [NEXP, 2], F32)
    S.op('dve', lambda e: e.memset(lo.ap, 0.0), writes=[lo])
    S.op('dve', lambda e: e.memset(hi.ap, 1.0), writes=[hi])
    segs = [(0, CTX, N, float(cap))]
    if need_ctx:
        segs.append((1, 0, CTX, float(capc)))
    for it in range(30):
        S.op('dve', lambda e: e.tensor_tensor(out=mid.ap, in0=lo.ap, in1=hi.ap, op=ALU.add), reads=[lo, hi], writes=[mid])
        S.op('dve', lambda e: e.tensor_scalar(out=mid.ap, in0=mid.ap, scalar1=0.5, scalar2=None, op0=ALU.mult), reads=[mid], writes=[mid])
        for (col, t0, n, cp_) in segs:
            S.op('dve', lambda e, col=col, t0=t0, n=n: e.tensor_scalar(out=scr.ap[:, 0:n], in0=affT.ap[:, t0:t0 + n], scalar1=mid.ap[:, col:col + 1], scalar2=None, op0=ALU.is_ge),
                 reads=[affT, mid], writes=[scr])
            S.op('dve', lambda e, col=col, n=n: e.tensor_reduce(out=cn.ap[:, col:col + 1], in_=scr.ap[:, 0:n], axis=AX.X, op=ALU.add), reads=[scr], writes=[cn])
            S.op('dve', lambda e, col=col, cp_=cp_: e.tensor_scalar(out=cn.ap[:, col:col + 1], in0=cn.ap[:, col:col + 1], scalar1=cp_, scalar2=None, op0=ALU.is_ge),
                 reads=[cn], writes=[cn])
            S.op('dve', lambda e, col=col: e.tensor_tensor(out=dl.ap[:, col:col + 1], in0=mid.ap[:, col:col + 1], in1=lo.ap[:, col:col + 1], op=ALU.subtract),
                 reads=[mid, lo], writes=[dl])
            S.op('dve', lambda e, col=col: e.scalar_tensor_tensor(out=lo.ap[:, col:col + 1], in0=dl.ap[:, col:col + 1], scalar=cn.ap[:, col:col + 1], in1=lo.ap[:, col:col + 1],
                                                                 op0=ALU.mult, op1=ALU.add), reads=[dl, cn, lo], writes=[lo])
            S.op('dve', lambda e, col=col: e.tensor_tensor(out=dl.ap[:, col:col + 1], in0=hi.ap[:, col:col + 1], in1=mid.ap[:, col:col + 1], op=ALU.subtract),
                 reads=[mid, hi], writes=[dl])
            S.op('dve', lambda e, col=col: e.scalar_tensor_tensor(out=hi.ap[:, col:col + 1], in0=dl.ap[:, col:col + 1], scalar=cn.ap[:, col:col + 1], in1=mid.ap[:, col:col + 1],
                                                                 op0=ALU.mult, op1=ALU.add), reads=[dl, cn, mid], writes=[hi])
    thrB = ar.alloc('thrB', [128, 2, NEXP], F32)
    trow = ar.alloc('trow', [1, 2, NEXP], F32)
    for col in range(2 if need_ctx else 1):
        S.op('pe', lambda e, col=col: e.transpose(out=bk[0].ap[0:1, 0:NEXP], in_=lo.ap[:, col:col + 1], identity=k.identf.ap[0:NEXP, 0:NEXP]),
             reads=[lo, k.identf], writes=[bk[0]])
        _cp(S, 'dve', trow, trow.ap[:, col, :], bk[0], bk[0].ap[0:1, 0:NEXP])
        S.op('pe', lambda e, col=col: e.matmul(bk[1].ap[:, 0:NEXP], lhsT=k.ones_f.ap[0:1, :], rhs=trow.ap[:, col, :], start=True, stop=True),
             reads=[k.ones_f, trow], writes=[bk[1]])
        _cp(S, 'dve', thrB, thrB.ap[:, col, :], bk[1], bk[1].ap[:, 0:NEXP])
    xg = [ar.alloc(f'xg{i}', [128, D], BF16) for i in range(3)]
    for t in tiles:
        col = 1 if t * 128 < CTX else 0
        S.op('dve', lambda e, t=t, col=col: e.tensor_tensor(out=maskb.ap[:, t, :], in0=affTM.ap[:, t, :], in1=thrB.ap[:, col, :], op=ALU.is_ge),
             reads=[affTM, thrB], writes=[maskb])
        S.op('dve', lambda e, t=t: e.tensor_tensor(out=maskw.ap[:, t, :], in0=maskb.ap[:, t, :], in1=affTM.ap[:, t, :], op=ALU.mult),
             reads=[maskb, affTM], writes=[maskw])
    for ti, t in enumerate(tiles):
        tok0 = t * 128
        is_c = tok0 < CTX
        seg0 = 0 if is_c else CTX // 128
        pp = bk[ti % 4]
        S.op('pe', lambda e, pp=pp, t=t, seg0=seg0: e.matmul(pp.ap[:, 0:NEXP], lhsT=ustr.ap, rhs=maskb.ap[:, t, :], start=True, stop=(t == seg0)),
             reads=[ustr, maskb], writes=[pp])
        for t2 in range(seg0, t):
            S.op('pe', lambda e, pp=pp, t2=t2, t=t: e.matmul(pp.ap[:, 0:NEXP], lhsT=k.ones_b.ap, rhs=maskb.ap[:, t2, :], start=False, stop=(t2 == t - 1)),
                 reads=[k.ones_b, maskb], writes=[pp])
        base = float(cap) if is_c else 0.0
        S.op('dve', lambda e, t=t, base=base: e.tensor_scalar(out=idxf.ap[:, t, :], in0=maskb.ap[:, t, :], scalar1=-BIG, scalar2=BIG + base, op0=ALU.mult, op1=ALU.add),
             reads=[maskb], writes=[idxf])
        S.op('dve', lambda e, t=t, pp=pp: e.tensor_tensor(out=idxf.ap[:, t, :], in0=idxf.ap[:, t, :], in1=pp.ap[:, 0:NEXP], op=ALU.add), reads=[idxf, pp], writes=[idxf])
        S.op('dve', lambda e, t=t: e.tensor_copy(out=idxi.ap[:, t, :], in_=idxf.ap[:, t, :]), reads=[idxf], writes=[idxi])
        x_ = xg[ti % 3]
        S.dma('sp', lambda e, x_=x_, tok0=tok0: e.dma_start(out=x_.ap, in_=k.xn_d.ap[tok0:tok0 + 128, :]), reads=[k.xn_d], writes=[x_])
        bound = (cap + capc - 1) if is_c else (cap - 1)
        for ex_ in range(NEXP):
            S.dma('pool', lambda e, x_=x_, t=t, ex_=ex_, bound=bound: e.indirect_dma_start(
                out=k.xin_d[ex_].ap, out_offset=bass.IndirectOffsetOnAxis(ap=idxi.ap[:, t, ex_:ex_ + 1], axis=0), in_=x_.ap, in_offset=None,
                bounds_check=_breg(k, e, bound), oob_is_err=False), reads=[x_, idxi], writes=[k.xin_d[ex_]])
    k.S.barrier()
    ar.reset(m_keep)
    m5d = ar.mark()
    wE = [[ar.alloc(f'wE{b}{m}', [128, 8, D], BF16) for m in range(3)] for b in range(2)]
    stage = [ar.alloc(f'st5{i}', [128, 8, 256], F32) for i in range(2)]
    xe = [ar.alloc(f'xe{i}', [128, D], BF16) for i in range(2)]
    SP = (SLOTS + 127) // 128 * 128
    xT = ar.alloc('xT5', [128, 8, SP], BF16)
    hidT = ar.alloc('hidT', [128, 8, SP], BF16)
    sgb = [ar.alloc(f'sg5{i}', [128, 512], F32) for i in range(2)]
    yst = [ar.alloc('yst5', [128, D], F32)] * 2
    stiles = [(r0, min(128, SLOTS - r0)) for r0 in range(0, SLOTS, 128)]
    schunks = [(c0, min(512, SLOTS - c0)) for c0 in range(0, SLOTS, 512)]
    cnt = 0
    for ex_ in range(NEXP):
        wb_ = wE[ex_ % 2]
        for m, nm in enumerate(('w_exp_gate', 'w_exp_up', 'w_exp_down')):
            src = I[nm][li, ex_].rearrange("(c p) n -> p c n", p=128)
            _load_cast(k, wb_[m], lambda q, m=m, wb_=wb_: wb_[m].ap[:, :, q * 256:(q + 1) * 256], lambda q, src=src: src[:, :, q * 256:(q + 1) * 256], 4, stage, 256)
        for si, (r0, nr) in enumerate(stiles):
            x_ = xe[si % 2]
            S.dma('sp', lambda e, x_=x_, ex_=ex_, r0=r0, nr=nr: e.dma_start(out=x_.ap[0:nr, :], in_=k.xin_d[ex_].ap[r0:r0 + nr, :]), reads=[k.xin_d[ex_]], writes=[x_])
            pT = bank_view(bk[7], BF16, [128, 8, 128])
            for c in range(8):
                S.op('pe', lambda e, x_=x_, c=c, nr=nr: e.transpose(out=pT[:, c, 0:nr], in_=x_.ap[0:nr, c * 128:(c + 1) * 128], identity=k.identb.ap[0:nr, 0:nr]),
                     reads=[x_, k.identb], writes=[bk[7]])
            rngs = []
            if r0 < cap:
                rngs.append((0, min(nr, cap - r0), 0))
            if r0 + nr > cap:
                rngs.append((max(0, cap - r0), nr, 1))
            for c in range(8):
                for (a_, b_, jj) in rngs:
                    S.op('dve' if c % 2 else 'act', (lambda e, c=c, a_=a_, b_=b_, jj=jj, r0=r0: e.tensor_scalar(
                        out=xT.ap[:, c, r0 + a_:r0 + b_], in0=pT[:, c, a_:b_], scalar1=k.colA2.ap[:, c, jj:jj + 1], scalar2=k.colB2.ap[:, c, jj:jj + 1], op0=ALU.mult, op1=ALU.add))
                        if c % 2 else (lambda e, c=c, a_=a_, b_=b_, jj=jj, r0=r0: e.activation(
                            out=xT.ap[:, c, r0 + a_:r0 + b_], in_=pT[:, c, a_:b_], func=AF.Identity, scale=k.colA2.ap[:, c, jj:jj + 1], bias=k.colB2.ap[:, c, jj:jj + 1])),
                        reads=[bk[7], k.colA2, k.colB2], writes=[xT])
        for fc in range(8):
            for (c0, ncol) in schunks:
                pg = bk[cnt % 2]; pu = bk[2 + cnt % 2]; s_ = sgb[cnt % 2]; cnt += 1
                for (pp_, m) in ((pg, 0), (pu, 1)):
                    for c in range(8):
                        S.op('pe', lambda e, pp_=pp_, m=m, c=c, fc=fc, c0=c0, ncol=ncol, wb_=wb_: e.matmul(
                            pp_.ap[:, 0:ncol], lhsT=wb_[m].ap[:, c, fc * 128:(fc + 1) * 128], rhs=xT.ap[:, c, c0:c0 + ncol], start=(c == 0), stop=(c == 7)),
                            reads=[wb_[m], xT], writes=[pp_])
                S.op('act', lambda e, pg=pg, s_=s_, ncol=ncol: e.activation(out=s_.ap[:, 0:ncol], in_=pg.ap[:, 0:ncol], func=AF.Silu), reads=[pg], writes=[s_])
                S.op('dve', lambda e, pu=pu, s_=s_, fc=fc, c0=c0, ncol=ncol: e.tensor_tensor(out=hidT.ap[:, fc, c0:c0 + ncol], in0=pu.ap[:, 0:ncol], in1=s_.ap[:, 0:ncol], op=ALU.mult),
                     reads=[pu, s_], writes=[hidT])
        for si, (r0, nr) in enumerate(stiles):
            y_ = yst[si % 2]
            for g in range(2):
                pb = bk[4 + g]
                for fc in range(8):
                    S.op('pe', lambda e, pb=pb, g=g, fc=fc, r0=r0, nr=nr, wb_=wb_: e.matmul(
                        pb.ap[0:nr, :], lhsT=hidT.ap[:, fc, r0:r0 + nr], rhs=wb_[2].ap[:, fc, g * 512:(g + 1) * 512], start=(fc == 0), stop=(fc == 7)),
                        reads=[hidT, wb_[2]], writes=[pb])
                _cp(S, 'act' if g else 'dve', y_, y_.ap[0:nr, g * 512:(g + 1) * 512], pb, pb.ap[0:nr, :])
            S.dma('sp', lambda e, y_=y_, ex_=ex_, r0=r0, nr=nr: e.dma_start(out=k.yexp_d[ex_].ap[r0:r0 + nr, :], in_=y_.ap[0:nr, :]), reads=[y_], writes=[k.yexp_d[ex_]])
    k.S.barrier()
    ar.reset(m5d)
    gb = [ar.alloc(f'gb5{i}', [128, D], F32) for i in range(4)]
    for g_ in gb:
        S.op('pool', lambda e, g_=g_: e.memset(g_.ap, 0.0), writes=[g_])
    accb = [ar.alloc(f'acc5{i}', [128, D], F32) for i in range(2)]
    xtb = [ar.alloc(f'xt5e{i}', [128, D], F32) for i in range(2)]
    gcnt = 0
    last = not need_ctx
    for ti, t in enumerate(tiles):
        tok0 = t * 128
        is_c = tok0 < CTX
        jj = 1 if is_c else 0
        bound = (cap + capc - 1) if is_c else (cap - 1)
        acc = accb[ti % 2]
        xt = xtb[ti % 2]
        S.dma('sp', lambda e, xt=xt, tok0=tok0: e.dma_start(out=xt.ap, in_=k.xs[0].ap[tok0:tok0 + 128, :]), reads=[k.xs[0]], writes=[xt])
        for ex_ in range(NEXP):
            g_ = gb[gcnt % 4]; gcnt += 1
            S.dma('pool', lambda e, g_=g_, t=t, ex_=ex_, bound=bound: e.indirect_dma_start(
                out=g_.ap, out_offset=None, in_=k.yexp_d[ex_].ap, in_offset=bass.IndirectOffsetOnAxis(ap=idxi.ap[:, t, ex_:ex_ + 1], axis=0),
                bounds_check=_breg(k, e, bound), oob_is_err=False), reads=[k.yexp_d[ex_], idxi], writes=[g_])
            if ex_ == 0:
                S.op('dve', lambda e, g_=g_, acc=acc, t=t, ex_=ex_: e.tensor_scalar(out=acc.ap, in0=g_.ap, scalar1=maskw.ap[:, t, ex_:ex_ + 1], scalar2=None, op0=ALU.mult),
                     reads=[g_, maskw], writes=[acc])
            else:
                S.op('dve', lambda e, g_=g_, acc=acc, t=t, ex_=ex_: e.scalar_tensor_tensor(out=acc.ap, in0=g_.ap, scalar=maskw.ap[:, t, ex_:ex_ + 1], in1=acc.ap, op0=ALU.mult, op1=ALU.add),
                     reads=[g_, maskw, acc], writes=[acc])
        S.op('pool', lambda e, acc=acc, jj=jj: e.tensor_tensor(out=acc.ap, in0=acc.ap, in1=k.modb[jj][5].ap, op=ALU.mult), reads=[acc, k.modb[jj][5]], writes=[acc])
        S.op('pool', lambda e, acc=acc, xt=xt: e.tensor_tensor(out=xt.ap, in0=xt.ap, in1=acc.ap, op=ALU.add), reads=[acc, xt], writes=[xt])
        if last:
            S.dma('sp', lambda e, xt=xt, tok0=tok0: e.dma_start(out=k.out.ap[tok0 - CTX:tok0 - CTX + 128, :], in_=xt.ap), reads=[xt], writes=[k.out])
        else:
            S.dma('sp', lambda e, xt=xt, tok0=tok0: e.dma_start(out=k.xs[1].ap[tok0:tok0 + 128, :], in_=xt.ap), reads=[xt], writes=[k.xs[1]])


def phase3_gdn(k, li, need_ctx):
    S, ar, I = k.S, k.ar, k.I
    bk = k.banks
    N, CTX, T = k.N, k.CTX, k.T
    cw = ar.alloc('cw', [128, 12, 3], F32)
    for cc in range(12):
        S.dma('sp', lambda e, cc=cc: e.dma_start(out=cw.ap[:, cc, :], in_=I['gdn_conv_w'][li][:, cc * 128:(cc + 1) * 128].rearrange("k p -> p k"),
                                                 allow_slow_non_contiguous=True), writes=[cw])
    m3 = ar.mark()
    xin = [ar.alloc(f'cxin{i}', [128, 514], F32) for i in range(2)]
    yb = [ar.alloc(f'cy{i}', [128, 512], F32) for i in range(2)]
    sq = ar.alloc('csq', [128, 512], F32)
    rn = ar.alloc('crn', [128, 512], F32)
    cnt = 0
    for (s0, ntok, is_ctx) in _spans(k):
        seg0, seg1 = (0, CTX) if is_ctx else (CTX, T)
        for cc in range(12):
            x_ = xin[cnt % 2]; y_ = yb[cnt % 2]; cnt += 1
            a0 = max(s0 - 1, seg0); a1 = min(s0 + ntok + 1, seg1)
            off = a0 - (s0 - 1)
            if s0 - 1 < seg0:
                S.op('pool', lambda e, x_=x_: e.memset(x_.ap[:, 0:1], 0.0), writes=[x_])
            if s0 + ntok + 1 > seg1:
                S.op('pool', lambda e, x_=x_, ntok=ntok: e.memset(x_.ap[:, ntok + 1:ntok + 2], 0.0), writes=[x_])
            S.dma('sp', lambda e, x_=x_, cc=cc, a0=a0, a1=a1, off=off: e.dma_start(out=x_.ap[:, off:off + a1 - a0], in_=k.cT_d.ap[cc, :, a0:a1]),
                  reads=[k.cT_d], writes=[x_])
            S.op('dve', lambda e, x_=x_, y_=y_, cc=cc, ntok=ntok: e.tensor_scalar(out=y_.ap[:, 0:ntok], in0=x_.ap[:, 0:ntok], scalar1=cw.ap[:, cc, 0:1], scalar2=None, op0=ALU.mult),
                 reads=[x_, cw], writes=[y_])
            for tap in (1, 2):
                S.op('dve', lambda e, x_=x_, y_=y_, cc=cc, ntok=ntok, tap=tap: e.scalar_tensor_tensor(
                    out=y_.ap[:, 0:ntok], in0=x_.ap[:, tap:tap + ntok], scalar=cw.ap[:, cc, tap:tap + 1], in1=y_.ap[:, 0:ntok], op0=ALU.mult, op1=ALU.add),
                    reads=[x_, cw, y_], writes=[y_])
            S.op('act', lambda e, y_=y_, ntok=ntok: e.activation(out=y_.ap[:, 0:ntok], in_=y_.ap[:, 0:ntok], func=AF.Silu), reads=[y_], writes=[y_])
            if cc < 8:
                pb = bk[cnt % 4]
                S.op('pool', lambda e, y_=y_, ntok=ntok: e.tensor_tensor(out=sq.ap[:, 0:ntok], in0=y_.ap[:, 0:ntok], in1=y_.ap[:, 0:ntok], op=ALU.mult), reads=[y_], writes=[sq])
                S.op('pe', lambda e, pb=pb, ntok=ntok: e.matmul(pb.ap[:, 0:ntok], lhsT=k.ones_f.ap, rhs=sq.ap[:, 0:ntok], start=True, stop=True),
                     reads=[k.ones_f, sq], writes=[pb])
                S.op('act', lambda e, pb=pb, ntok=ntok: e.activation(out=rn.ap[:, 0:ntok], in_=pb.ap[:, 0:ntok], func=AF.Sqrt, bias=k.epst.ap, scale=1.0),
                     reads=[pb, k.epst], writes=[rn])
                S.op('dve', lambda e, ntok=ntok: e.reciprocal(out=rn.ap[:, 0:ntok], in_=rn.ap[:, 0:ntok]), reads=[rn], writes=[rn])
                sc_ = (128.0 ** -0.5) if cc < 4 else 1.0
                S.op('dve', lambda e, y_=y_, ntok=ntok, sc_=sc_: e.scalar_tensor_tensor(out=y_.ap[:, 0:ntok], in0=y_.ap[:, 0:ntok], scalar=sc_, in1=rn.ap[:, 0:ntok],
                                                                                      op0=ALU.mult, op1=ALU.mult), reads=[y_, rn], writes=[y_])
            S.dma('sp', lambda e, y_=y_, cc=cc, s0=s0, ntok=ntok: e.dma_start(out=k.cP_d.ap[cc, :, s0:s0 + ntok], in_=y_.ap[:, 0:ntok]), reads=[y_], writes=[k.cP_d])
    S.barrier()
    ar.reset(m3)
    gc_ = ar.alloc('gdnc', [128, 2, 5, 64], F32)
    S.dma('sp', lambda e: e.dma_start(out=gc_.ap, in_=I['gdnc'].rearrange("d p s c -> p d s c")), writes=[gc_])
    gnw = ar.alloc('gnw', [64, 128], F32)
    S.dma('sp', lambda e: e.dma_start(out=gnw.ap, in_=I['gdn_norm_w'][li].partition_broadcast(64)), writes=[gnw])
    Sf = ar.alloc('Sf', [128, 4, 128], F32)
    Sb = ar.alloc('Sb', [128, 4, 128], BF16)
    A = lambda nm, shape, dt=F32: [ar.alloc(f'{nm}{i}', shape, dt) for i in range(2)]
    qk_in = A('qk_in', [128, 8, 64]); v_in = A('v_in', [128, 4, 64]); gdup = A('gdup', [128, 4]); beta = A('beta', [64, 4])
    qkb = A('qkb', [128, 8, 64], BF16)
    lhsE = A('lhsE', [128, 4, 64])
    Em = A('Em', [64, 4, 64]); DT = A('DT', [64, 4, 64])
    gcs = A('gcs', [64, 4]); egc = A('egc', [64, 4]); negegc = A('negegc', [64, 4]); ekd = A('ekd', [64, 4]); glast = A('glast', [128, 4])
    KKD = A('KKD', [64, 4, 64]); QKD = A('QKD', [64, 4, 64], BF16)
    Mf = A('Mf', [64, 4, 64]); Mb = A('Mb', [64, 4, 64], BF16); Nb = A('Nb', [64, 4, 64], BF16)
    NM = A('NM', [64, 8, 64], BF16)
    Yf = A('Yf', [64, 4, 64]); Yb = A('Yb', [64, 4, 64], BF16)
    kdec = A('kdec', [64, 4, 128], BF16); vtok = A('vtok', [64, 4, 128])
    rp = A('rp', [64, 4, 128], BF16); vnew = A('vnew', [64, 4, 128], BF16)
    o1 = A('o1g', [64, 4, 128]); ost = A('ost', [64, 4, 128])
    ofw = A('ofw', [64, 4, 128]); zc = A('zc', [64, 4, 128], BF16)
    ssq = A('gssq', [64, 4]); yg = A('yg', [64, 4, 128], BF16); ygT = A('ygT', [128, 4, 64], BF16)
    sqg = A('sqg', [64, 4, 128])
    nch = T // 64
    nctx = CTX // 64
    it = 0
    for d in range(2):
        S.op('pool', lambda e: e.memset(Sf.ap, 0.0), writes=[Sf])
        S.op('pool', lambda e: e.memset(Sb.ap, 0.0), writes=[Sb])
        order = list(range(nctx)) + list(range(nctx, nch))
        if d == 1:
            order = list(range(nctx - 1, -1, -1)) + list(range(nch - 1, nctx - 1, -1))
        CM = gc_.ap[:, d, 0, :]; RC = gc_.ap[:, d, 1, :]; UC = gc_.ap[0:64, d, 2, :]; BM = gc_.ap[0:64, d, 3, :]; ST = gc_.ap[0:64, d, 4, :]
        bc4 = lambda ap: ap.unsqueeze(1).broadcast_to([64, 4, 64])
        for c in order:
            b = it % 2; it += 1
            tok0 = c * 64
            want_o = need_ctx or tok0 >= CTX
            qi, vi, gd, be, qb = qk_in[b], v_in[b], gdup[b], beta[b], qkb[b]
            S.dma('sp', lambda e, qi=qi, tok0=tok0: e.dma_start(out=qi.ap, in_=k.cP_d.ap[0:8, :, tok0:tok0 + 64].rearrange("c p t -> p c t")), reads=[k.cP_d], writes=[qi])
            S.dma('sp', lambda e, vi=vi, tok0=tok0: e.dma_start(out=vi.ap, in_=k.cP_d.ap[8:12, :, tok0:tok0 + 64].rearrange("c p t -> p c t")), reads=[k.cP_d], writes=[vi])
            for hf in range(2):
                S.dma('sp', lambda e, gd=gd, tok0=tok0, hf=hf, d=d: e.dma_start(out=gd.ap[hf * 64:(hf + 1) * 64, :], in_=k.gb_d.ap[tok0:tok0 + 64, 8 + d * 4:12 + d * 4]),
                      reads=[k.gb_d], writes=[gd])
            S.dma('sp', lambda e, be=be, tok0=tok0, d=d: e.dma_start(out=be.ap, in_=k.gb_d.ap[tok0:tok0 + 64, d * 4:d * 4 + 4]), reads=[k.gb_d], writes=[be])
            S.op('pool', lambda e, qi=qi, qb=qb: e.tensor_copy(out=qb.ap, in_=qi.ap), reads=[qi], writes=[qb])
            pk = bank_view(bk[0], F32, [64, 4, 128]); pv = bank_view(bk[1], F32, [64, 4, 128])
            for h in range(4):
                S.op('pe', lambda e, h=h, qi=qi, pk=pk: e.transpose(out=pk[:, h, :], in_=qi.ap[:, 4 + h, :], identity=k.identf.ap), reads=[qi, k.identf], writes=[bk[0]])
            for h in range(4):
                S.op('pe', lambda e, h=h, vi=vi, pv=pv: e.transpose(out=pv[:, h, :], in_=vi.ap[:, h, :], identity=k.identf.ap), reads=[vi, k.identf], writes=[bk[1]])
            pc = bank_view(bk[2], F32, [64, 8, 64])
            for h in range(4):
                S.op('pe', lambda e, h=h, qb=qb, pc=pc: e.matmul(pc[:, h, :], lhsT=qb.ap[:, 4 + h, :], rhs=qb.ap[:, 4 + h, :], start=True, stop=True), reads=[qb], writes=[bk[2]])
                S.op('pe', lambda e, h=h, qb=qb, pc=pc: e.matmul(pc[:, 4 + h, :], lhsT=qb.ap[:, 4 + h, :], rhs=qb.ap[:, h, :], start=True, stop=True), reads=[qb], writes=[bk[2]])
            le = lhsE[b]
            S.op('dve', lambda e, le=le, gd=gd, CM=CM: e.tensor_tensor(out=le.ap, in0=CM.unsqueeze(1).broadcast_to([128, 4, 64]), in1=gd.ap.unsqueeze(2).broadcast_to([128, 4, 64]), op=ALU.mult),
                 reads=[gc_, gd], writes=[le])
            pe_ = bank_view(bk[3], F32, [64, 4, 64])
            for h in range(4):
                S.op('pe', lambda e, h=h, le=le, RC=RC, pe_=pe_: e.matmul(pe_[:, h, :], lhsT=le.ap[:, h, :], rhs=RC, start=True, stop=True), reads=[le, gc_], writes=[bk[3]])
            pgc = bk[3].ap[0:64, 256:260]
            S.op('pe', lambda e, gd=gd, UC=UC, pgc=pgc: e.matmul(pgc, lhsT=UC, rhs=gd.ap[0:64, :], start=True, stop=True), reads=[gd, gc_], writes=[bk[3]])
            pgs = bk[3].ap[:, 264:268]
            S.op('pe', lambda e, gd=gd, pgs=pgs: e.matmul(pgs, lhsT=k.ones_f.ap[0:64, :], rhs=gd.ap[0:64, :], start=True, stop=True), reads=[gd, k.ones_f], writes=[bk[3]])
            em, dt_ = Em[b], DT[b]
            S.op('dve', lambda e, em=em, pe_=pe_, BM=BM: e.tensor_tensor(out=em.ap, in0=pe_, in1=bc4(BM), op=ALU.add), reads=[bk[3], gc_], writes=[em])
            S.op('act', lambda e, em=em, dt_=dt_: e.activation(out=dt_.ap, in_=em.ap, func=AF.Exp, scale=-1.0), reads=[em], writes=[dt_])
            S.op('dve', lambda e, b=b, pgc=pgc: e.tensor_copy(out=gcs[b].ap, in_=pgc), reads=[bk[3]], writes=[gcs[b]])
            S.op('act', lambda e, b=b, pgc=pgc: e.activation(out=egc[b].ap, in_=pgc, func=AF.Exp), reads=[bk[3]], writes=[egc[b]])
            S.op('dve', lambda e, b=b: e.tensor_scalar(out=negegc[b].ap, in0=egc[b].ap, scalar1=-1.0, scalar2=None, op0=ALU.mult), reads=[egc[b]], writes=[negegc[b]])
            S.op('dve', lambda e, b=b, pgs=pgs: e.tensor_tensor(out=ekd[b].ap, in0=pgs[0:64, :], in1=gcs[b].ap, op=ALU.subtract), reads=[bk[3], gcs[b]], writes=[ekd[b]])
            S.op('act', lambda e, b=b: e.activation(out=ekd[b].ap, in_=ekd[b].ap, func=AF.Exp), reads=[ekd[b]], writes=[ekd[b]])
            S.op('act', lambda e, b=b, pgs=pgs: e.activation(out=glast[b].ap, in_=pgs, func=AF.Exp), reads=[bk[3]], writes=[glast[b]])
            S.op('dve', lambda e, b=b, pc=pc, dt_=dt_: e.tensor_tensor(out=KKD[b].ap, in0=pc[:, 0:4, :], in1=dt_.ap, op=ALU.mult), reads=[bk[2], dt_], writes=[KKD[b]])
            S.op('dve', lambda e, b=b, pc=pc, dt_=dt_: e.tensor_tensor(out=QKD[b].ap, in0=pc[:, 4:8, :], in1=dt_.ap, op=ALU.mult), reads=[bk[2], dt_], writes=[QKD[b]])
            S.op('pool', lambda e, b=b, be=be: e.tensor_tensor(out=KKD[b].ap, in0=KKD[b].ap, in1=be.ap.unsqueeze(2).broadcast_to([64, 4, 64]), op=ALU.mult), reads=[KKD[b], be], writes=[KKD[b]])
            S.op('pool', lambda e, b=b, ST=ST: e.tensor_tensor(out=Mf[b].ap, in0=KKD[b].ap, in1=bc4(ST), op=ALU.mult), reads=[KKD[b], gc_], writes=[Mf[b]])
            S.op('pool', lambda e, b=b: e.tensor_copy(out=Mb[b].ap, in_=Mf[b].ap), reads=[Mf[b]], writes=[Mb[b]])
            S.op('dve', lambda e, b=b: e.tensor_tensor(out=Yf[b].ap, in0=bc4(k.identf.ap[0:64, 0:64]), in1=Mf[b].ap, op=ALU.subtract), reads=[k.identf, Mf[b]], writes=[Yf[b]])
            S.op('pool', lambda e, b=b: e.tensor_copy(out=Yb[b].ap, in_=Yf[b].ap), reads=[Yf[b]], writes=[Yb[b]])
            pn = bank_view(bk[4], F32, [64, 4, 64])
            for h in range(4):
                S.op('pe', lambda e, h=h, b=b, pn=pn: e.transpose(out=pn[:, h, :], in_=Mf[b].ap[:, h, :], identity=k.identf.ap[0:64, 0:64]), reads=[Mf[b], k.identf], writes=[bk[4]])
            _cp(S, 'act', Nb[b], Nb[b].ap, bk[4], pn)
            curN, curM = Nb[b], Mb[b]
            nm = NM[b]
            for lvl in range(5):
                pnm = bank_view(bk[4 + (lvl % 2)], F32, [64, 8, 64])
                lastl = lvl == 4
                cN, cM = curN, curM
                nview = (lambda cN=cN: cN.ap) if lvl == 0 else (lambda nm=nm: nm.ap[:, 0:4, :])
                mview = (lambda cM=cM: cM.ap) if lvl == 0 else (lambda nm=nm: nm.ap[:, 4:8, :])
                srcs = [curN, curM] if lvl == 0 else [nm]
                for h in range(4):
                    S.op('pe', lambda e, h=h, pnm=pnm, nview=nview, mview=mview: e.matmul(pnm[:, h, :], lhsT=mview()[:, h, :], rhs=nview()[:, h, :], start=True, stop=True),
                         reads=srcs, writes=[bk[4 + (lvl % 2)]])
                    if not lastl:
                        S.op('pe', lambda e, h=h, pnm=pnm, nview=nview, mview=mview: e.matmul(pnm[:, 4 + h, :], lhsT=nview()[:, h, :], rhs=mview()[:, h, :], start=True, stop=True),
                             reads=srcs, writes=[bk[4 + (lvl % 2)]])
                if lastl:
                    _cp(S, 'act', nm, nm.ap[:, 0:4, :], bk[4 + (lvl % 2)], pnm[:, 0:4, :])
                else:
                    _cp(S, 'act', nm, nm.ap, bk[4 + (lvl % 2)], pnm)
                py = bank_view(bk[6], F32, [64, 4, 64])
                for h in range(4):
                    S.op('pe', lambda e, h=h, py=py, nm=nm, b=b: e.matmul(py[:, h, :], lhsT=nm.ap[:, h, :], rhs=Yb[b].ap[:, h, :], start=True, stop=True),
                         reads=[nm, Yb[b]], writes=[bk[6]])
                S.op('dve', lambda e, b=b, py=py: e.tensor_tensor(out=Yf[b].ap, in0=Yf[b].ap, in1=py, op=ALU.add), reads=[Yf[b], bk[6]], writes=[Yf[b]])
                S.op('pool', lambda e, b=b: e.tensor_copy(out=Yb[b].ap, in_=Yf[b].ap), reads=[Yf[b]], writes=[Yb[b]])
            S.op('dve', lambda e, b=b, pk=pk: e.tensor_tensor(out=kdec[b].ap, in0=pk, in1=ekd[b].ap.unsqueeze(2).broadcast_to([64, 4, 128]), op=ALU.mult), reads=[bk[0], ekd[b]], writes=[kdec[b]])
            _cp(S, 'act', vtok[b], vtok[b].ap, bk[1], pv)
            pks = bank_view(bk[7], F32, [64, 4, 128]); pvn = bank_view(bk[6], F32, [64, 4, 128])
            p1 = bank_view(bk[0], F32, [64, 4, 128]); p2 = bank_view(bk[1], F32, [64, 4, 128]); pds = bank_view(bk[2], F32, [128, 4, 128])
            for h in range(4):
                S.op('pe', lambda e, h=h, qb=qb, pks=pks: e.matmul(pks[:, h, :], lhsT=qb.ap[:, 4 + h, :], rhs=Sb.ap[:, h, :], start=True, stop=True), reads=[qb, Sb], writes=[bk[7]])
                S.op('dve', lambda e, h=h, b=b, pks=pks: e.scalar_tensor_tensor(out=rp[b].ap[:, h, :], in0=pks[:, h, :], scalar=negegc[b].ap[:, h:h + 1], in1=vtok[b].ap[:, h, :],
                                                                               op0=ALU.mult, op1=ALU.add), reads=[bk[7], negegc[b], vtok[b]], writes=[rp[b]])
                S.op('pe', lambda e, h=h, b=b, pvn=pvn: e.matmul(pvn[:, h, :], lhsT=Yb[b].ap[:, h, :], rhs=rp[b].ap[:, h, :], start=True, stop=True), reads=[Yb[b], rp[b]], writes=[bk[6]])
                S.op('act', lambda e, h=h, b=b, be=be, pvn=pvn: e.activation(out=vnew[b].ap[:, h, :], in_=pvn[:, h, :], func=AF.Copy, scale=be.ap[:, h:h + 1]), reads=[bk[6], be], writes=[vnew[b]])
                if want_o:
                    S.op('pe', lambda e, h=h, qb=qb, p1=p1: e.matmul(p1[:, h, :], lhsT=qb.ap[:, h, :], rhs=Sb.ap[:, h, :], start=True, stop=True), reads=[qb, Sb], writes=[bk[0]])
                    S.op('pe', lambda e, h=h, b=b, p2=p2: e.matmul(p2[:, h, :], lhsT=QKD[b].ap[:, h, :], rhs=vnew[b].ap[:, h, :], start=True, stop=True), reads=[QKD[b], vnew[b]], writes=[bk[1]])
                S.op('pe', lambda e, h=h, b=b, pds=pds: e.matmul(pds[:, h, :], lhsT=kdec[b].ap[:, h, :], rhs=vnew[b].ap[:, h, :], start=True, stop=True), reads=[kdec[b], vnew[b]], writes=[bk[2]])
                S.op('dve', lambda e, h=h, b=b, pds=pds: e.scalar_tensor_tensor(out=Sf.ap[:, h, :], in0=Sf.ap[:, h, :], scalar=glast[b].ap[:, h:h + 1], in1=pds[:, h, :],
                                                                               op0=ALU.mult, op1=ALU.add), reads=[Sf, glast[b], bk[2]], writes=[Sf])
                S.op('pool', lambda e, h=h: e.tensor_copy(out=Sb.ap[:, h, :], in_=Sf.ap[:, h, :]), reads=[Sf], writes=[Sb])
            if not want_o:
                continue
            S.op('dve', lambda e, b=b, p1=p1: e.tensor_tensor(out=o1[b].ap, in0=p1, in1=egc[b].ap.unsqueeze(2).broadcast_to([64, 4, 128]), op=ALU.mult), reads=[bk[0], egc[b]], writes=[o1[b]])
            S.op('dve', lambda e, b=b, p2=p2: e.tensor_tensor(out=ost[b].ap, in0=o1[b].ap, in1=p2, op=ALU.add), reads=[o1[b], bk[1]], writes=[ost[b]])
            if d == 0:
                S.dma('sp', lambda e, b=b, tok0=tok0: e.dma_start(out=k.of_d.ap[tok0:tok0 + 64, :], in_=ost[b].ap.rearrange("p a b -> p (a b)")), reads=[ost[b]], writes=[k.of_d])
                continue
            S.dma('sp', lambda e, b=b, tok0=tok0: e.dma_start(out=ofw[b].ap.rearrange("p a b -> p (a b)"), in_=k.of_d.ap[tok0:tok0 + 64, :]), reads=[k.of_d], writes=[ofw[b]])
            S.dma('sp', lambda e, b=b, tok0=tok0: e.dma_start(out=zc[b].ap.rearrange("p a b -> p (a b)"), in_=k.z_d.ap[tok0:tok0 + 64, :]), reads=[k.z_d], writes=[zc[b]])
            S.op('pool', lambda e, b=b: e.tensor_tensor(out=ost[b].ap, in0=ost[b].ap, in1=ofw[b].ap, op=ALU.add), reads=[ost[b], ofw[b]], writes=[ost[b]])
            S.op('pool', lambda e, b=b: e.tensor_tensor(out=sqg[b].ap, in0=ost[b].ap, in1=ost[b].ap, op=ALU.mult), reads=[ost[b]], writes=[sqg[b]])
            S.op('dve', lambda e, b=b: e.tensor_reduce(out=ssq[b].ap, in_=sqg[b].ap, axis=AX.X, op=ALU.add), reads=[sqg[b]], writes=[ssq[b]])
            S.op('act', lambda e, b=b: e.activation(out=ssq[b].ap, in_=ssq[b].ap, func=AF.Sqrt, bias=k.epst.ap[0:64, :], scale=1.0 / 128), reads=[ssq[b], k.epst], writes=[ssq[b]])
            S.op('dve', lambda e, b=b: e.reciprocal(out=ssq[b].ap, in_=ssq[b].ap), reads=[ssq[b]], writes=[ssq[b]])
            S.op('dve', lambda e, b=b: e.tensor_tensor(out=ost[b].ap, in0=ost[b].ap, in1=ssq[b].ap.unsqueeze(2).broadcast_to([64, 4, 128]), op=ALU.mult), reads=[ost[b], ssq[b]], writes=[ost[b]])
            S.op('pool', lambda e, b=b: e.tensor_tensor(out=ost[b].ap, in0=ost[b].ap, in1=gnw.ap.unsqueeze(1).broadcast_to([64, 4, 128]), op=ALU.mult), reads=[ost[b], gnw], writes=[ost[b]])
            S.op('dve', lambda e, b=b: e.tensor_tensor(out=yg[b].ap, in0=ost[b].ap, in1=zc[b].ap, op=ALU.mult), reads=[ost[b], zc[b]], writes=[yg[b]])
            pyt = bank_view(bk[5], BF16, [128, 4, 64])
            for h in range(4):
                S.op('pe', lambda e, h=h, b=b, pyt=pyt: e.transpose(out=pyt[:, h, :], in_=yg[b].ap[:, h, :], identity=k.identb.ap[0:64, 0:64]), reads=[yg[b], k.identb], writes=[bk[5]])
            _cp(S, 'act', ygT[b], ygT[b].ap, bk[5], pyt)
            S.dma('sp', lambda e, b=b, tok0=tok0: e.dma_start(out=k.yT_d.ap[2, :, tok0:tok0 + 64].rearrange("(c p) t -> p c t", p=128), in_=ygT[b].ap), reads=[ygT[b]], writes=[k.yT_d])


def gdn_consts():
    g = np.zeros((2, 128, 5, 64), np.float32)
    kk = np.arange(64)[:, None]
    ii = np.arange(64)[None, :]
    for d in range(2):
        U = (kk <= ii) if d == 0 else (kk >= ii)
        U = U.astype(np.float32)
        valid = U
        g[d, 0:64, 0] = U
        g[d, 64:128, 0] = -1.0
        g[d, 0:64, 1] = 1.0
        g[d, 64:128, 1] = U
        g[d, 0:64, 2] = U
        g[d, 0:64, 3] = (1.0 - valid) * 30000.0
        g[d, 0:64, 4] = valid * (1.0 - np.eye(64, dtype=np.float32))
    return g


def _rope_tables(N):
    rows = N // 64
    row = np.repeat(np.arange(rows, dtype=np.float32), 64)
    col = np.tile(np.arange(64, dtype=np.float32), rows)
    inv_freq = (np.float32(10000.0) ** (-np.arange(0, 32, 2, dtype=np.float32) / np.float32(32))).astype(np.float32)
    ang = np.concatenate([row[:, None] * inv_freq, col[:, None] * inv_freq], axis=-1).astype(np.float32)
    cos, sin = np.cos(ang).astype(np.float32), np.sin(ang).astype(np.float32)
    C = np.zeros((N, 64), np.float32)
    Sg = np.zeros((N, 64), np.float32)
    for a in range(2):
        for pr in range(2):
            C[:, a * 32 + pr * 16:a * 32 + pr * 16 + 16] = cos[:, a * 16:(a + 1) * 16]
            Sg[:, a * 32 + pr * 16:a * 32 + pr * 16 + 16] = sin[:, a * 16:(a + 1) * 16] * (-1.0 if pr == 0 else 1.0)
    return C, Sg


_NC_CACHE = {}


def kernel(**inputs):
    B, N, _ = inputs['x'].shape
    CTX = inputs['ctx'].shape[1]
    key = (N, CTX)
    if key not in _NC_CACHE:
        _NC_CACHE[key] = build(N=N, CTX=CTX)
    nc = _NC_CACHE[key]
    f32 = lambda a: np.ascontiguousarray(np.asarray(a, dtype=np.float32))
    shared = {nm: f32(v) for nm, v in inputs.items() if nm not in ('x', 'c', 'ctx')}
    shared['gdn_a_log'] = shared['gdn_a_log'].reshape(DEPTH, 8)
    shared['gdn_dt_bias'] = shared['gdn_dt_bias'].reshape(DEPTH, 8)
    C, Sg = _rope_tables(N)
    shared['ident'] = np.eye(128, dtype=np.float32)
    shared['ropeC'] = C
    shared['ropeS'] = Sg
    shared['ustrict'] = np.triu(np.ones((128, 128), np.float32), 1)
    shared['gdnc'] = gdn_consts()
    x, c, ctx = f32(inputs['x']), f32(inputs['c']), f32(inputs['ctx'])
    in_maps = []
    for b in range(B):
        m = dict(shared)
        m['x'] = x[b]
        m['c'] = c[b]
        m['ctx'] = ctx[b]
        in_maps.append(m)
    res = run_bass_kernel_spmd(nc, in_maps, core_ids=list(range(B)))
    return np.stack([np.asarray(r['out'], dtype=np.float32) for r in res.results], axis=0)
```

```python
import math
from contextlib import ExitStack

import numpy as np
import concourse.bass as bass
import concourse.mybir as mybir
from concourse.bass_utils import run_bass_kernel_spmd

F32 = mybir.dt.float32
BF16 = mybir.dt.bfloat16
I32 = mybir.dt.int32
AF = mybir.ActivationFunctionType
ALU = mybir.AluOpType
AX = mybir.AxisListType

D = 1024
DEPTH = 2
NEXP = 16
IN_COLS = 4368
EPS = 1e-6

COMPUTE = ('pe', 'act', 'dve', 'pool')
QUEUES = ('sp', 'act', 'pool')
EPOCH = 30000
NS = 8


class Sched:
    def __init__(self):
        self.stream = {e: [] for e in ('pe', 'act', 'dve', 'pool', 'sp')}
        self.ncomp = {e: 0 for e in COMPUTE}
        self.ndma = {q: 0 for q in QUEUES}
        self.lastw = {}
        self.readers = {}
        self.waited = {}
        self.semkeys = set()

    def _need(self, eng, tok):
        semkey, val = tok
        if semkey[0] == 'c':
            if semkey[1] == 'pe' and eng == 'pe':
                return
            g = semkey[2] * EPOCH + val
            if semkey[1] == eng and self.ncomp[eng] - g >= 6:
                return
            k = (eng, 'c', semkey[1])
            if self.waited.get(k, 0) >= g:
                return
            self.waited[k] = g
        else:
            k = (eng, semkey)
            if self.waited.get(k, 0) >= val:
                return
            self.waited[k] = val
        self.stream[eng].append(('wait', semkey, val))

    def _deps(self, eng, reads, writes):
        toks = {}
        def add(sk, v):
            if sk[0] == 'c':
                key = ('c', sk[1])
                g = sk[2] * EPOCH + v
                if key not in toks or toks[key][0] < g:
                    toks[key] = (g, sk, v)
            else:
                if sk not in toks or toks[sk][0] < v:
                    toks[sk] = (v, sk, v)
        for r in reads:
            t = self.lastw.get(r)
            if t is not None:
                add(*t)
        for w in writes:
            t = self.lastw.get(w)
            if t is not None:
                add(*t)
            rd = self.readers.get(w)
            if rd:
                for sk, v in rd.items():
                    add(sk, v)
        for _, sk, v in toks.values():
            self._need(eng, (sk, v))

    def _record(self, tok, reads, writes):
        for w in writes:
            self.lastw[w] = tok
            self.readers[w] = {}
        sk, v = tok
        for r in reads:
            if r in writes:
                continue
            d = self.readers.setdefault(r, {})
            if sk[0] == 'c':
                for old in [o for o in d if o[0] == 'c' and o[1] == sk[1]]:
                    del d[old]
                d[sk] = v
            else:
                d[sk] = max(d.get(sk, 0), v)

    def op(self, eng, fn, reads=(), writes=()):
        reads = tuple(reads)
        writes = tuple(writes)
        self._deps(eng, reads, writes)
        seq = self.ncomp[eng]
        self.ncomp[eng] += 1
        semkey = ('c', eng, seq // EPOCH)
        tok = (semkey, seq % EPOCH + 1)
        self.semkeys.add(semkey)
        self.stream[eng].append(('op', fn, semkey))
        self._record(tok, reads, writes)
        return tok

    def dma(self, q, fn, reads=(), writes=()):
        reads = tuple(reads)
        writes = tuple(writes)
        k = self.ndma[q]
        self.ndma[q] += 1
        slot = k % NS
        semkey = ('d', q, slot)
        self.semkeys.add(semkey)
        if k >= NS:
            self._need(q, (semkey, 16 * (k // NS)))
        self._deps(q, reads, writes)
        tok = (semkey, 16 * (k // NS + 1))
        self.stream[q].append(('dma', fn, semkey))
        self._record(tok, reads, writes)
        return tok

    def _all_tokens(self):
        toks = []
        for q in QUEUES:
            n = self.ndma[q]
            for k in range(max(0, n - NS), n):
                toks.append((('d', q, k % NS), 16 * (k // NS + 1)))
        for e in COMPUTE:
            n = self.ncomp[e]
            if n:
                toks.append((('c', e, (n - 1) // EPOCH), (n - 1) % EPOCH + 1))
        return toks

    def barrier(self):
        toks = self._all_tokens()
        for e in ('pe', 'act', 'dve', 'pool', 'sp'):
            for t in toks:
                if e == 'pe' and t[0][0] == 'c' and t[0][1] == 'pe':
                    continue
                self._need(e, t)
        self.lastw.clear()
        self.readers.clear()

    def finish(self):
        for t in self._all_tokens():
            self._need('sp', t)

    def emit(self, nc, stack):
        sems = {}
        for sk in sorted(self.semkeys):
            sems[sk] = stack.enter_context(nc.semaphore("s_" + "_".join(str(x) for x in sk)))
        block = stack.enter_context(nc.Block())
        streams = self.stream

        def run(eng, name):
            pend = []
            for item in streams[name]:
                if item[0] == 'wait':
                    pend.append(item)
                elif item[0] == 'op':
                    for w in pend[:-1]:
                        eng.wait_ge(sems[w[1]], w[2])
                    ins = item[1](eng)
                    if pend:
                        ins._wait_ge(sems[pend[-1][1]], pend[-1][2])
                    ins.then_inc(sems[item[2]], 1)
                    pend = []
                else:
                    for w in pend:
                        eng.wait_ge(sems[w[1]], w[2])
                    pend = []
                    item[1](eng).then_inc(sems[item[2]], 16)
            for w in pend:
                eng.wait_ge(sems[w[1]], w[2])

        @block.tensor
        def _(e):
            run(e, 'pe')

        @block.scalar
        def _(e):
            run(e, 'act')

        @block.vector
        def _(e):
            run(e, 'dve')

        @block.gpsimd
        def _(e):
            run(e, 'pool')

        @block.sync
        def _(e):
            run(e, 'sp')


class Buf:
    __slots__ = ('name', 'ap')

    def __init__(self, name, ap):
        self.name = name
        self.ap = ap

    def __getitem__(self, k):
        return self.ap[k]

    def __repr__(self):
        return self.name


_DTSIZE = {F32: 4, BF16: 2, I32: 4}


class Arena:
    def __init__(self, nc, stack, kib):
        self.words = kib * 256
        self.t = stack.enter_context(nc.sbuf_tensor("arena", [128, self.words], F32))
        self.off = 0
        self.n = 0

    def mark(self):
        return self.off

    def reset(self, m):
        self.off = m

    def alloc(self, name, shape, dt=F32):
        p = shape[0]
        free = int(np.prod(shape[1:]))
        words = (free * _DTSIZE[dt] + 3) // 4
        words = (words + 7) // 8 * 8
        assert self.off + words <= self.words, f"arena overflow at {name}: {self.off}+{words}>{self.words}"
        ap = self.t[0:p, self.off:self.off + words]
        self.off += words
        if dt != F32:
            ap = ap.bitcast(dt)
        ap = ap[:, 0:free]
        if len(shape) == 3:
            ap = ap.rearrange("p (a b) -> p a b", a=shape[1])
        elif len(shape) == 4:
            ap = ap.rearrange("p (a b c) -> p a b c", a=shape[1], b=shape[2])
        self.n += 1
        return Buf(f"{name}#{self.n}", ap)


SRC_RANGES = [(0, 512), (512, 640), (768, 1280), (1280, 1792),
              (640, 768), (1792, 2304),
              (3840, 4352),
              (4352, 4368),
              (2304, 3840)]
O_NK, O_V, O_Z, O_GA, O_C = 0, 1664, 2304, 2816, 2832
NSUB = 26


class K:
    pass


def build(N=8192, CTX=256, nlayers=DEPTH, stop_after=None, debug=(), nexp_decl=NEXP):
    T = CTX + N
    nc = bass.Bass("TRN2", target_bir_lowering=False)
    k = K()
    k.nc, k.N, k.CTX, k.T = nc, N, CTX, T
    k.S = S = Sched()
    global LASTS
    LASTS = S

    def din(name, shape, dt=F32):
        return nc.dram_tensor(name, list(shape), dt, kind="ExternalInput").ap()

    def dscr(name, shape, dt=F32):
        kind = "ExternalOutput" if name in debug else "Internal"
        return Buf(name, nc.dram_tensor(name, list(shape), dt, kind=kind).ap())

    I = {}
    I['x'] = din('x', [N, D])
    I['c'] = din('c', [D])
    I['ctx'] = din('ctx', [CTX, D])
    I['c_ctx'] = din('c_ctx', [D])
    I['w_mod'] = din('w_mod', [DEPTH, D, 6 * D])
    I['b_mod'] = din('b_mod', [DEPTH, 6 * D])
    I['norm1_w'] = din('norm1_w', [DEPTH, D])
    I['norm2_w'] = din('norm2_w', [DEPTH, D])
    I['w_in'] = din('w_in', [DEPTH, D, IN_COLS])
    for nm in ('gqa_q_norm', 'gqa_k_norm', 'diff_q_norm', 'diff_k_norm', 'diff_lambda_q1', 'diff_lambda_k1',
               'diff_lambda_q2', 'diff_lambda_k2'):
        I[nm] = din(nm, [DEPTH, 64])
    I['diff_subln'] = din('diff_subln', [DEPTH, 128])
    I['gdn_conv_w'] = din('gdn_conv_w', [DEPTH, 3, 1536])
    I['gdn_a_log'] = din('gdn_a_log', [DEPTH, 8])
    I['gdn_dt_bias'] = din('gdn_dt_bias', [DEPTH, 8])
    I['gdn_norm_w'] = din('gdn_norm_w', [DEPTH, 128])
    I['w_merge_gate'] = din('w_merge_gate', [DEPTH, 3, D, D])
    I['w_branch'] = din('w_branch', [DEPTH, 3, 512, D])
    I['w_out'] = din('w_out', [DEPTH, D, D])
    I['w_router'] = din('w_router', [DEPTH, D, NEXP])
    I['w_exp_gate'] = din('w_exp_gate', [DEPTH, nexp_decl, D, D])
    I['w_exp_up'] = din('w_exp_up', [DEPTH, nexp_decl, D, D])
    I['w_exp_down'] = din('w_exp_down', [DEPTH, nexp_decl, D, D])
    I['ident'] = din('ident', [128, 128])
    I['ropeC'] = din('ropeC', [N, 64])
    I['ropeS'] = din('ropeS', [N, 64])
    I['ustrict'] = din('ustrict', [128, 128])
    I['gdnc'] = din('gdnc', [2, 128, 5, 64])
    k.I = I
    out = nc.dram_tensor('out', [N, D], F32, kind="ExternalOutput").ap()
    k.out = Buf('out', out)

    k.xs = [dscr(f'xs{i}', [T, D]) for i in range(2)]
    k.hT_d = dscr('hT_d', [128, 8, T], BF16)
    k.qkT_d = dscr('qkT_d', [NSUB, 64, T], BF16)
    k.v_d = dscr('v_d', [T, 640], BF16)
    k.z_d = dscr('z_d', [T, 512], BF16)
    k.gb_d = dscr('gb_d', [T, 16])
    k.cT_d = dscr('cT_d', [12, 128, T])
    k.yT_d = dscr('yT_d', [3, 512, T], BF16)
    k.cP_d = dscr('cP_d', [12, 128, T])
    k.of_d = dscr('of_d', [T, 512])
    k.xn_d = dscr('xn_d', [T, D], BF16)
    SL = N // 8 + CTX // 8
    k.xin_d = [dscr(f'xin_d{e_}', [SL, D], BF16) for e_ in range(NEXP)]
    k.yexp_d = [dscr(f'yexp_d{e_}', [SL, D]) for e_ in range(NEXP)]

    with ExitStack() as st:
        k.st = st
        k.ar = Arena(nc, st, 200)
        k.pairs = [st.enter_context(nc.psum_tensor(f'pp{i}', [128, 1024], F32)) for i in range(4)]
        k.banks = [Buf(f'bank{i}', k.pairs[i // 2][:, (i % 2) * 512:(i % 2) * 512 + 512]) for i in range(8)]
        _program(k, nlayers, stop_after)
        S.finish()
        S.emit(nc, st)
    return nc


def bank_view(b, dt, shape):
    ap = b.ap
    if dt != F32:
        ap = ap.bitcast(dt)
    p = shape[0]
    free = int(np.prod(shape[1:]))
    ap = ap[0:p, 0:free]
    if len(shape) == 3:
        ap = ap.rearrange("p (a b) -> p a b", a=shape[1])
    return ap


def _program(k, nlayers, stop_after):
    S, ar, I = k.S, k.ar, k.I
    k.identf = ar.alloc('identf', [128, 128], F32)
    k.identb = ar.alloc('identb', [128, 128], BF16)
    k.ones_f = ar.alloc('ones_f', [128, 128], F32)
    k.ones_b = ar.alloc('ones_b', [128, 128], BF16)
    k.epst = ar.alloc('epst', [128, 1], F32)
    S.dma('sp', lambda e: e.dma_start(out=k.identf.ap, in_=I['ident']), writes=[k.identf])
    S.op('dve', lambda e: e.tensor_copy(out=k.identb.ap, in_=k.identf.ap), reads=[k.identf], writes=[k.identb])
    S.op('pool', lambda e: e.memset(k.ones_f.ap, 1.0), writes=[k.ones_f])
    S.op('pool', lambda e: e.memset(k.ones_b.ap, 1.0), writes=[k.ones_b])
    S.op('pool', lambda e: e.memset(k.epst.ap, EPS), writes=[k.epst])
    k.modb = [[(ar.alloc(f'modb{j}{i}', [128, D], F32) if i in (2, 5) else None) for i in range(6)] for j in range(2)]
    base = ar.mark()
    for li in range(nlayers):
        last = li == DEPTH - 1
        xin_lat = Buf('x_in', I['x']) if li == 0 else None
        ar.reset(base)
        phase0_mod(k, li)
        S.barrier()
        ar.reset(base)
        phase1_proj(k, li)
        S.barrier()
        if stop_after == 'p1':
            return
        ar.reset(base)
        phase2_attn(k, li, not last)
        S.barrier()
        if stop_after == 'p2':
            return
        ar.reset(base)
        phase3_gdn(k, li, not last)
        S.barrier()
        if stop_after == 'p3':
            return
        ar.reset(base)
        phase4_merge(k, li, not last)
        S.barrier()
        if stop_after == 'p4':
            return
        ar.reset(base)
        phase5_moe(k, li, not last)
        S.barrier()


def _cp(S, eng, out_b, out_ap, in_b, in_ap):
    if eng == 'act':
        S.op('act', lambda e: e.copy(out=out_ap, in_=in_ap), reads=[in_b], writes=[out_b])
    else:
        S.op(eng, lambda e: e.tensor_copy(out=out_ap, in_=in_ap), reads=[in_b], writes=[out_b])


def phase0_mod(k, li):
    S, ar, I = k.S, k.ar, k.I
    bk = k.banks
    vrow = ar.alloc('vrow', [128, 128], F32)
    S.dma('sp', lambda e: e.dma_start(out=vrow.ap[0:8, :], in_=I['c'].rearrange("(c p) -> c p", p=128)), writes=[vrow])
    S.dma('sp', lambda e: e.dma_start(out=vrow.ap[8:16, :], in_=I['c_ctx'].rearrange("(c p) -> c p", p=128)), writes=[vrow])
    S.dma('sp', lambda e: e.dma_start(out=vrow.ap[16:24, :], in_=I['norm1_w'][li].rearrange("(c p) -> c p", p=128)), writes=[vrow])
    S.dma('sp', lambda e: e.dma_start(out=vrow.ap[24:32, :], in_=I['norm2_w'][li].rearrange("(c p) -> c p", p=128)), writes=[vrow])
    S.dma('sp', lambda e: e.dma_start(out=vrow.ap[32:80, :], in_=I['b_mod'][li].rearrange("(c p) -> c p", p=128)), writes=[vrow])
    vcol = ar.alloc('vcol', [128, 80], F32)
    pv = bank_view(bk[0], F32, [128, 80])
    S.op('pe', lambda e: e.transpose(out=pv, in_=vrow.ap[0:80, :], identity=k.identf.ap[0:80, 0:80]),
         reads=[vrow, k.identf], writes=[bk[0]])
    _cp(S, 'dve', vcol, vcol.ap, bk[0], pv)
    cact = ar.alloc('cact', [128, 8, 2], F32)
    for j in range(2):
        S.op('act', lambda e, j=j: e.activation(out=cact.ap[:, :, j], in_=vcol.ap[:, j * 8:(j + 1) * 8], func=AF.Silu),
             reads=[vcol], writes=[cact])
    brow = ar.alloc('brow', [1, 6 * D], F32)
    S.dma('sp', lambda e: e.dma_start(out=brow.ap, in_=I['b_mod'][li:li + 1, :]), writes=[brow])
    modcol = ar.alloc('modcol', [128, 48, 2], F32)
    wst = [ar.alloc(f'wst{i}', [128, 8, 512], F32) for i in range(2)]
    grow = [ar.alloc(f'grow{i}', [1, 512], F32) for i in range(2)]
    k.A = [[None] * 2 for _ in range(2)]
    wsrc = I['w_mod'][li].rearrange("(c p) n -> p c n", p=128)
    nb = 2
    for n in range(12):
        w = wst[n % 2]
        S.dma('sp', lambda e, w=w, n=n: e.dma_start(out=w.ap, in_=wsrc[:, :, n * 512:(n + 1) * 512]), writes=[w])
        split = n // 2
        if split in (2, 5):
            gi = 2 if split == 2 else 5
            for j in range(2):
                pb = bk[nb % 8]; nb += 1
                for c in range(8):
                    S.op('pe', lambda e, pb=pb, w=w, c=c, j=j: e.matmul(pb.ap[0:1, :], lhsT=cact.ap[:, c, j:j + 1], rhs=w.ap[:, c, :],
                                                                      start=(c == 0), stop=(c == 7)),
                         reads=[cact, w], writes=[pb])
                g = grow[j]
                S.op('dve', lambda e, pb=pb, g=g, n=n: e.tensor_tensor(out=g.ap, in0=pb.ap[0:1, :], in1=brow.ap[:, n * 512:(n + 1) * 512], op=ALU.add),
                     reads=[pb, brow], writes=[g])
                pb2 = bk[nb % 8]; nb += 1
                S.op('pe', lambda e, pb2=pb2, g=g: e.matmul(pb2.ap, lhsT=k.ones_f.ap[0:1, :], rhs=g.ap, start=True, stop=True),
                     reads=[k.ones_f, g], writes=[pb2])
                dst = k.modb[j][gi]
                half = n % 2
                _cp(S, 'act', dst, dst.ap[:, half * 512:(half + 1) * 512], pb2, pb2.ap)
        else:
            pb = bk[nb % 8]; nb += 1
            pvv = bank_view(pb, F32, [128, 4, 2])
            for sub in range(4):
                for c in range(8):
                    S.op('pe', lambda e, pvv=pvv, w=w, c=c, sub=sub: e.matmul(pvv[:, sub, :], lhsT=w.ap[:, c, sub * 128:(sub + 1) * 128], rhs=cact.ap[:, c, :],
                                                                            start=(c == 0), stop=(c == 7)),
                         reads=[cact, w], writes=[pb])
            S.op('dve', lambda e, pvv=pvv, n=n: e.tensor_tensor(out=modcol.ap[:, n * 4:(n + 1) * 4, :], in0=pvv,
                                                              in1=vcol.ap[:, 32 + n * 4:32 + (n + 1) * 4].unsqueeze(2).broadcast_to([128, 4, 2]), op=ALU.add),
                 reads=[pb, vcol], writes=[modcol])
    k.colA1 = ar_persist(k, 'colA1', [128, 8, 2]); k.colB1 = ar_persist(k, 'colB1', [128, 8, 2])
    k.colA2 = ar_persist(k, 'colA2', [128, 8, 2]); k.colB2 = ar_persist(k, 'colB2', [128, 8, 2])
    for (dstA, dstB, sh, sc, nw) in ((k.colA1, k.colB1, 0, 1, 16), (k.colA2, k.colB2, 3, 4, 24)):
        S.op('dve', lambda e, dstA=dstA, sc=sc, nw=nw: e.scalar_tensor_tensor(
            out=dstA.ap, in0=modcol.ap[:, sc * 8:(sc + 1) * 8, :], scalar=1.0,
            in1=vcol.ap[:, nw:nw + 8].unsqueeze(2).broadcast_to([128, 8, 2]), op0=ALU.add, op1=ALU.mult),
            reads=[modcol, vcol], writes=[dstA])
        S.op('dve', lambda e, dstB=dstB, sh=sh: e.tensor_copy(out=dstB.ap, in_=modcol.ap[:, sh * 8:(sh + 1) * 8, :]),
             reads=[modcol], writes=[dstB])


def ar_persist(k, name, shape, dt=F32):
    if not hasattr(k, '_persist'):
        k._persist = {}
    if name not in k._persist:
        t = k.st.enter_context(k.nc.sbuf_tensor("P_" + name, list(shape), dt))
        k._persist[name] = Buf("P_" + name, t[:])
    return k._persist[name]


def _spans(k):
    sp = [(0, k.CTX, True)]
    for i in range(k.N // 512):
        sp.append((k.CTX + i * 512, 512, False))
    return sp


def _xsrc(k, li, tok0):
    if li == 0:
        if tok0 < k.CTX:
            return k.I['ctx'][tok0:tok0 + 128, :], None
        return k.I['x'][tok0 - k.CTX:tok0 - k.CTX + 128, :], None
    b = k.xs[1]
    return b.ap[tok0:tok0 + 128, :], b


def _norm_tile(k, src_ap, src_res, xt, sqs, ss, rstd):
    S = k.S
    S.dma('sp', lambda e: e.dma_start(out=xt.ap, in_=src_ap), reads=[src_res] if src_res else [], writes=[xt])
    S.op('act', lambda e: e.activation(out=sqs.ap.rearrange('p a b -> p (a b)')[:, 0:1024] if len(sqs.ap.shape) == 3 else sqs.ap, in_=xt.ap, func=AF.Square, scale=1.0 / 32, accum_out=ss.ap),
         reads=[xt], writes=[sqs, ss])
    S.op('act', lambda e: e.activation(out=rstd.ap, in_=ss.ap, func=AF.Sqrt, bias=k.epst.ap, scale=1.0), reads=[ss, k.epst], writes=[rstd])
    S.op('dve', lambda e: e.reciprocal(out=rstd.ap, in_=rstd.ap), reads=[rstd], writes=[rstd])


def phase1_proj(k, li):
    S, ar, I = k.S, k.ar, k.I
    bk = k.banks
    N, CTX, T = k.N, k.CTX, k.T
    wb = ar.alloc('wb', [128, 8, IN_COLS], BF16)
    wst = [ar.alloc(f'w1st{i}', [128, 8, 256], F32) for i in range(2)]
    wsrc = I['w_in'][li].rearrange("(c p) n -> p c n", p=128)
    pieces = []
    dst = 0
    for (a, b) in SRC_RANGES:
        s = a
        while s < b:
            wd = min(256, b - s)
            pieces.append((s, dst, wd))
            s += wd
            dst += wd
    for i, (s0, d0, wd) in enumerate(pieces):
        w = wst[i % 2]
        S.dma('sp', lambda e, w=w, s0=s0, wd=wd: e.dma_start(out=w.ap[:, :, 0:wd], in_=wsrc[:, :, s0:s0 + wd]), writes=[w])
        S.op(('pool', 'dve')[i % 2], lambda e, w=w, d0=d0, wd=wd: e.tensor_copy(out=wb.ap[:, :, d0:d0 + wd], in_=w.ap[:, :, 0:wd]),
             reads=[w], writes=[wb])
    nkw = ar.alloc('nkw', [128, NSUB, 64], F32)
    for nm, s0, n in (('gqa_q_norm', 0, 8), ('gqa_k_norm', 8, 2), ('diff_q_norm', 10, 8), ('diff_k_norm', 18, 8)):
        S.dma('sp', lambda e, nm=nm, s0=s0, n=n: e.dma_start(
            out=nkw.ap[:, s0:s0 + n, :], in_=I[nm][li].partition_broadcast(128).unsqueeze(1).broadcast_to([128, n, 64])), writes=[nkw])
    dtb = ar.alloc('dtb', [128, 8], F32)
    nega = ar.alloc('nega', [128, 8], F32)
    S.dma('sp', lambda e: e.dma_start(out=dtb.ap, in_=I['gdn_dt_bias'][li].partition_broadcast(128)), writes=[dtb])
    S.dma('sp', lambda e: e.dma_start(out=nega.ap, in_=I['gdn_a_log'][li].partition_broadcast(128)), writes=[nega])
    S.op('act', lambda e: e.activation(out=nega.ap, in_=nega.ap, func=AF.Exp), reads=[nega], writes=[nega])
    S.op('dve', lambda e: e.tensor_scalar(out=nega.ap, in0=nega.ap, scalar1=-1.0, scalar2=None, op0=ALU.mult), reads=[nega], writes=[nega])

    xtb = [ar.alloc(f'xt{i}', [128, D], F32) for i in range(2)]
    ssb = [ar.alloc(f'ss{i}', [128, 1], F32) for i in range(2)]
    rsb = [ar.alloc(f'rstd{i}', [128, 1], F32) for i in range(2)]
    hbb = [ar.alloc(f'hb{i}', [128, D], BF16) for i in range(2)]
    hTs = ar.alloc('hTs', [128, 8, 512], BF16)
    fst = wst
    fv = lambda f: f.ap.rearrange('p a b -> p (a b)')
    nkq = ar.alloc('nkq', [128, NSUB, 64], F32)
    t1 = ar.alloc('t1', [128, NSUB, 64], F32)
    t2 = ar.alloc('t2', [128, NSUB, 64], F32)
    sqs = t2
    ssq = ar.alloc('ssq', [128, NSUB], F32)
    rcf = ar.alloc('rcf', [128, NSUB, 64], F32)
    rsf = ar.alloc('rsf', [128, NSUB, 64], F32)
    qr = ar.alloc('qr', [128, NSUB, 64], BF16)
    qkts = ar.alloc('qkts', [64, NSUB, 512], BF16)
    vst = [ar.alloc(f'vst{i}', [128, 640], BF16) for i in range(2)]
    zst = [ar.alloc(f'zst{i}', [128, 512], BF16) for i in range(2)]
    gast = [ar.alloc(f'gast{i}', [128, 16], F32) for i in range(2)]
    gtmp = ar.alloc('gtmp', [128, 8], F32)

    tcount = 0
    for (s0, ntok, is_ctx) in _spans(k):
        jj = 1 if is_ctx else 0
        ntile = ntok // 128
        for j in range(ntile):
            tok0 = s0 + j * 128
            b = tcount % 2
            tcount += 1
            xt, ss, rstd, hb = xtb[b], ssb[b], rsb[b], hbb[b]
            src_ap, src_res = _xsrc(k, li, tok0)
            _norm_tile(k, src_ap, src_res, xt, sqs, ss, rstd)
            S.op('dve', lambda e, hb=hb, xt=xt, rstd=rstd: e.tensor_scalar(out=hb.ap, in0=xt.ap, scalar1=rstd.ap, scalar2=None, op0=ALU.mult),
                 reads=[xt, rstd], writes=[hb])
            pT = bank_view(bk[7], BF16, [128, 8, 128])
            for c in range(8):
                S.op('pe', lambda e, hb=hb, c=c: e.transpose(out=pT[:, c, :], in_=hb.ap[:, c * 128:(c + 1) * 128], identity=k.identb.ap),
                     reads=[hb, k.identb], writes=[bk[7]])
            for c in range(8):
                if c % 2 == 0:
                    S.op('dve', lambda e, c=c, j=j, jj=jj: e.tensor_scalar(
                        out=hTs.ap[:, c, j * 128:(j + 1) * 128], in0=pT[:, c, :], scalar1=k.colA1.ap[:, c, jj:jj + 1],
                        scalar2=k.colB1.ap[:, c, jj:jj + 1], op0=ALU.mult, op1=ALU.add), reads=[bk[7], k.colA1, k.colB1], writes=[hTs])
                else:
                    S.op('act', lambda e, c=c, j=j, jj=jj: e.activation(
                        out=hTs.ap[:, c, j * 128:(j + 1) * 128], in_=pT[:, c, :], func=AF.Identity, scale=k.colA1.ap[:, c, jj:jj + 1],
                        bias=k.colB1.ap[:, c, jj:jj + 1]), reads=[bk[7], k.colA1, k.colB1], writes=[hTs])
        S.dma('sp', lambda e, s0=s0, ntok=ntok: e.dma_start(out=k.hT_d.ap[:, :, s0:s0 + ntok], in_=hTs.ap[:, :, 0:ntok]),
              reads=[hTs], writes=[k.hT_d])
        for cc in range(12):
            pb = bk[4 + cc % 3]
            for c in range(8):
                S.op('pe', lambda e, pb=pb, c=c, cc=cc, ntok=ntok: e.matmul(
                    pb.ap[:, 0:ntok], lhsT=wb.ap[:, c, O_C + cc * 128:O_C + (cc + 1) * 128], rhs=hTs.ap[:, c, 0:ntok],
                    start=(c == 0), stop=(c == 7)), reads=[wb, hTs], writes=[pb])
            f = fst[cc % 2]
            _cp(S, 'act' if cc % 2 else 'dve', f, fv(f)[:, 0:ntok], pb, pb.ap[:, 0:ntok])
            S.dma('sp', lambda e, f=f, cc=cc, s0=s0, ntok=ntok: e.dma_start(out=k.cT_d.ap[cc, :, s0:s0 + ntok], in_=fv(f)[:, 0:ntok]),
                  reads=[f], writes=[k.cT_d])
        for j in range(ntile):
            tok0 = s0 + j * 128
            b = j % 2
            lt = lambda c: hTs.ap[:, c, j * 128:(j + 1) * 128]
            for g, (c0, wd) in enumerate(((0, 512), (512, 512), (1024, 512), (1536, 128))):
                for c in range(8):
                    S.op('pe', lambda e, g=g, c=c, c0=c0, wd=wd, j=j: e.matmul(
                        bk[g].ap[:, 0:wd], lhsT=hTs.ap[:, c, j * 128:(j + 1) * 128], rhs=wb.ap[:, c, O_NK + c0:O_NK + c0 + wd],
                        start=(c == 0), stop=(c == 7)), reads=[wb, hTs], writes=[bk[g]])
            nkflat = nkq.ap.rearrange("p a b -> p (a b)")
            for g, (c0, wd) in enumerate(((0, 512), (512, 512), (1024, 512), (1536, 128))):
                _cp(S, 'act' if g % 2 else 'dve', nkq, nkflat[:, c0:c0 + wd], bk[g], bk[g].ap[:, 0:wd])
            for (pb, p0, c0, wd) in ((bk[4], 0, O_V, 512), (bk[5], 0, O_V + 512, 128), (bk[6], 0, O_Z, 512), (bk[5], 128, O_GA, 16)):
                for c in range(8):
                    S.op('pe', lambda e, pb=pb, p0=p0, c=c, c0=c0, wd=wd, j=j: e.matmul(
                        pb.ap[:, p0:p0 + wd], lhsT=hTs.ap[:, c, j * 128:(j + 1) * 128], rhs=wb.ap[:, c, c0:c0 + wd],
                        start=(c == 0), stop=(c == 7)), reads=[wb, hTs], writes=[pb])
            v, z, ga = vst[b], zst[b], gast[b]
            _cp(S, 'dve', v, v.ap[:, 0:512], bk[4], bk[4].ap)
            _cp(S, 'dve', v, v.ap[:, 512:640], bk[5], bk[5].ap[:, 0:128])
            S.dma('sp', lambda e, v=v, tok0=tok0: e.dma_start(out=k.v_d.ap[tok0:tok0 + 128, :], in_=v.ap), reads=[v], writes=[k.v_d])
            S.op('act', lambda e, z=z: e.activation(out=z.ap, in_=bk[6].ap, func=AF.Silu), reads=[bk[6]], writes=[z])
            S.dma('sp', lambda e, z=z, tok0=tok0: e.dma_start(out=k.z_d.ap[tok0:tok0 + 128, :], in_=z.ap), reads=[z], writes=[k.z_d])
            S.op('act', lambda e, ga=ga: e.activation(out=ga.ap[:, 0:8], in_=bk[5].ap[:, 128:136], func=AF.Sigmoid), reads=[bk[5]], writes=[ga])
            S.op('dve', lambda e: e.tensor_tensor(out=gtmp.ap, in0=bk[5].ap[:, 136:144], in1=dtb.ap, op=ALU.add), reads=[bk[5], dtb], writes=[gtmp])
            S.op('act', lambda e: e.activation(out=gtmp.ap, in_=gtmp.ap, func=AF.Exp), reads=[gtmp], writes=[gtmp])
            S.op('act', lambda e: e.activation(out=gtmp.ap, in_=gtmp.ap, func=AF.Ln, bias=1.0, scale=1.0), reads=[gtmp], writes=[gtmp])
            S.op('dve', lambda e, ga=ga: e.tensor_tensor(out=ga.ap[:, 8:16], in0=gtmp.ap, in1=nega.ap, op=ALU.mult), reads=[gtmp, nega], writes=[ga])
            S.dma('sp', lambda e, ga=ga, tok0=tok0: e.dma_start(out=k.gb_d.ap[tok0:tok0 + 128, :], in_=ga.ap), reads=[ga], writes=[k.gb_d])
            S.op('pool', lambda e: e.tensor_tensor(out=t1.ap, in0=nkq.ap, in1=nkq.ap, op=ALU.mult), reads=[nkq], writes=[t1])
            S.op('dve', lambda e: e.tensor_reduce(out=ssq.ap, in_=t1.ap, axis=AX.X, op=ALU.add), reads=[t1], writes=[ssq])
            S.op('act', lambda e: e.activation(out=ssq.ap, in_=ssq.ap, func=AF.Sqrt, bias=k.epst.ap, scale=1.0 / 64), reads=[ssq, k.epst], writes=[ssq])
            S.op('dve', lambda e: e.reciprocal(out=ssq.ap, in_=ssq.ap), reads=[ssq], writes=[ssq])
            S.op('dve', lambda e: e.tensor_tensor(out=t1.ap, in0=nkq.ap, in1=ssq.ap.unsqueeze(2).broadcast_to([128, NSUB, 64]), op=ALU.mult),
                 reads=[nkq, ssq], writes=[t1])
            if is_ctx:
                S.op('pool', lambda e: e.tensor_tensor(out=qr.ap, in0=t1.ap, in1=nkw.ap, op=ALU.mult), reads=[t1, nkw], writes=[qr])
            else:
                S.op('pool', lambda e: e.tensor_tensor(out=nkq.ap, in0=t1.ap, in1=nkw.ap, op=ALU.mult), reads=[t1, nkw], writes=[nkq])
                r0 = tok0 - CTX
                S.dma('sp', lambda e, r0=r0: e.dma_start(out=rcf.ap, in_=I['ropeC'][r0:r0 + 128, :].unsqueeze(1).broadcast_to([128, NSUB, 64])), writes=[rcf])
                S.dma('sp', lambda e, r0=r0: e.dma_start(out=rsf.ap, in_=I['ropeS'][r0:r0 + 128, :].unsqueeze(1).broadcast_to([128, NSUB, 64])), writes=[rsf])
                S.op('dve', lambda e: e.tensor_tensor(out=t1.ap, in0=nkq.ap, in1=rcf.ap, op=ALU.mult), reads=[nkq, rcf], writes=[t1])
                qv = nkq.ap.rearrange("p a (g two s) -> p (a g) two s", g=2, two=2)
                sv = rsf.ap.rearrange("p a (g two s) -> p (a g) two s", g=2, two=2)
                tv = t2.ap.rearrange("p a (g two s) -> p (a g) two s", g=2, two=2)
                S.op('pool', lambda e: e.tensor_tensor(out=tv[:, :, 0, :], in0=qv[:, :, 1, :], in1=sv[:, :, 0, :], op=ALU.mult), reads=[nkq, rsf], writes=[t2])
                S.op('pool', lambda e: e.tensor_tensor(out=tv[:, :, 1, :], in0=qv[:, :, 0, :], in1=sv[:, :, 1, :], op=ALU.mult), reads=[nkq, rsf], writes=[t2])
                S.op('dve', lambda e: e.tensor_tensor(out=qr.ap, in0=t1.ap, in1=t2.ap, op=ALU.add), reads=[t1, t2], writes=[qr])
            for b0, nb_ in ((0, 8), (8, 8), (16, 8), (24, 2)):
                pq = bank_view(bk[7], BF16, [64, 8, 128])
                for i in range(nb_):
                    S.op('pe', lambda e, i=i, b0=b0: e.transpose(out=pq[:, i, :], in_=qr.ap[:, b0 + i, :], identity=k.identb.ap),
                         reads=[qr, k.identb], writes=[bk[7]])
                _cp(S, 'act', qkts, qkts.ap[:, b0:b0 + nb_, j * 128:(j + 1) * 128], bk[7], pq[:, 0:nb_, :])
        S.dma('sp', lambda e, s0=s0, ntok=ntok: e.dma_start(out=k.qkT_d.ap[:, :, s0:s0 + ntok].rearrange("s d t -> d s t"), in_=qkts.ap[:, :, 0:ntok]),
              reads=[qkts], writes=[k.qkT_d])


def lambda_init(li):
    return 0.8 - 0.6 * math.exp(-0.3 * li)


def phase2_attn(k, li, need_ctx):
    S, ar, I = k.S, k.ar, k.I
    bk = k.banks
    N, CTX, T = k.N, k.CTX, k.T
    nkb = T // 128
    linit = lambda_init(li)
    lv = ar.alloc('lv', [128, 4, 64], F32)
    for i, nm in enumerate(('diff_lambda_q1', 'diff_lambda_k1', 'diff_lambda_q2', 'diff_lambda_k2')):
        S.dma('sp', lambda e, i=i, nm=nm: e.dma_start(out=lv.ap[:, i, :], in_=I[nm][li].partition_broadcast(128)), writes=[lv])
    lp = ar.alloc('lp', [128, 2, 64], F32)
    ls = ar.alloc('ls', [128, 2], F32)
    neglam = ar.alloc('neglam', [128, 1], F32)
    sublc = ar.alloc('sublc', [128, 1], F32)
    for i in range(2):
        S.op('dve', lambda e, i=i: e.tensor_tensor(out=lp.ap[:, i, :], in0=lv.ap[:, 2 * i, :], in1=lv.ap[:, 2 * i + 1, :], op=ALU.mult), reads=[lv], writes=[lp])
    S.op('dve', lambda e: e.tensor_reduce(out=ls.ap, in_=lp.ap, axis=AX.X, op=ALU.add), reads=[lp], writes=[ls])
    S.op('act', lambda e: e.activation(out=ls.ap, in_=ls.ap, func=AF.Exp), reads=[ls], writes=[ls])
    S.op('dve', lambda e: e.tensor_tensor(out=neglam.ap, in0=ls.ap[:, 1:2], in1=ls.ap[:, 0:1], op=ALU.subtract), reads=[ls], writes=[neglam])
    S.op('dve', lambda e: e.tensor_scalar(out=neglam.ap, in0=neglam.ap, scalar1=-linit, scalar2=None, op0=ALU.add), reads=[neglam], writes=[neglam])
    S.dma('sp', lambda e: e.dma_start(out=sublc.ap, in_=I['diff_subln'][li].rearrange("(p o) -> p o", o=1)), writes=[sublc])
    S.op('dve', lambda e: e.tensor_scalar(out=sublc.ap, in0=sublc.ap, scalar1=1.0 - linit, scalar2=None, op0=ALU.mult), reads=[sublc], writes=[sublc])

    KT = [ar.alloc(f'KT{i}', [64, 2, T], BF16) for i in range(2)]
    VT = [ar.alloc(f'VT{i}', [128, nkb, 130], BF16) for i in range(2)]
    for i in range(2):
        S.op('pool', lambda e, i=i: e.memset(VT[i].ap[:, :, 64:65], 1.0), writes=[VT[i]])
    QT = [ar.alloc(f'QT{i}', [64, 512], BF16) for i in range(3)]
    PT = [ar.alloc(f'PT{i}', [128, 1024], BF16) for i in range(3)]
    rs = ar.alloc('rs', [128, 512], F32)
    bcs = ar.alloc('bcs', [128, 512], F32)
    o1 = ar.alloc('o1', [128, 512], F32)
    o2 = ar.alloc('o2', [128, 512], F32)
    sq = ar.alloc('sq', [128, 512], F32)
    yst = [ar.alloc(f'yst{i}', [128, 512], BF16) for i in range(2)]

    heads = []
    for h in range(8):
        heads.append(dict(kind='gqa', subs=[(h, 8 + h // 4)], vc0=(h // 4) * 64, dv=64, br=0, row0=h * 64))
    for h in range(4):
        heads.append(dict(kind='diff', subs=[(10 + 2 * h, 18 + 2 * h), (10 + 2 * h + 1, 18 + 2 * h + 1)], vc0=128 + h * 128, dv=128, br=1, row0=h * 128))
    chunks = [(CTX + i * 512, 512, 0, nkb) for i in range(N // 512)]
    if need_ctx:
        chunks = [(0, CTX, 0, CTX // 128)] + chunks
    cnt = dict(s=0, o=0, q=0, p=0, y=0)
    for hi, hd in enumerate(heads):
        kt, vt = KT[hi % 2], VT[hi % 2]
        dv = hd['dv']
        gqa = hd['kind'] == 'gqa'
        for ci, (qs, ks) in enumerate(hd['subs']):
            S.dma('sp', lambda e, kt=kt, ci=ci, ks=ks: e.dma_start(out=kt.ap[:, ci, :], in_=k.qkT_d.ap[ks, :, :]), reads=[k.qkT_d], writes=[kt])
        vdst = vt.ap[:, :, 0:64] if gqa else vt.ap[:, :, 0:128]
        S.dma('sp', lambda e, vdst=vdst, hd=hd, dv=dv: e.dma_start(
            out=vdst, in_=k.v_d.ap[:, hd['vc0']:hd['vc0'] + dv].rearrange("(b p) d -> p b d", p=128)), reads=[k.v_d], writes=[vt])
        if not gqa:
            pass
        elif hi > 0 and heads[hi - 2 if hi >= 2 else 0]['kind'] != 'gqa':
            pass
        for (t0, nq, kb0, kb1) in chunks:
            for ci, (qs, ks) in enumerate(hd['subs']):
                qt = QT[cnt['q'] % 3]; cnt['q'] += 1
                S.dma('sp', lambda e, qt=qt, qs=qs, t0=t0, nq=nq: e.dma_start(out=qt.ap[:, 0:nq], in_=k.qkT_d.ap[qs, :, t0:t0 + nq]),
                      reads=[k.qkT_d], writes=[qt])
                po = bk[4 + cnt['o'] % 2]
                psm = bk[6]
                cnt['o'] += 1
                M = 65 if gqa else 128
                def emit_scores(kb):
                    pi = cnt['s'] % 2; cnt['s'] += 1
                    psA, psB = bk[2 * pi], bk[2 * pi + 1]
                    pt = PT[cnt['p'] % 3]; cnt['p'] += 1
                    for u, ps in enumerate((psA, psB)):
                        S.op('pe', lambda e, ps=ps, kt=kt, ci=ci, kb=kb + u, qt=qt, nq=nq: e.matmul(
                            ps.ap[:, 0:nq], lhsT=kt.ap[:, ci, kb * 128:(kb + 1) * 128], rhs=qt.ap[:, 0:nq], start=True, stop=True),
                            reads=[kt, qt], writes=[ps])
                    if nq == 512:
                        S.op('act', lambda e, pi=pi, pt=pt: e.activation(out=pt.ap, in_=k.pairs[pi][:, :], func=AF.Exp, scale=0.125),
                             reads=[psA, psB], writes=[pt])
                    else:
                        for u, ps in enumerate((psA, psB)):
                            S.op('act', lambda e, ps=ps, pt=pt, nq=nq, u=u: e.activation(out=pt.ap[:, u * 512:u * 512 + nq], in_=ps.ap[:, 0:nq], func=AF.Exp, scale=0.125),
                                 reads=[ps], writes=[pt])
                    return pt

                def emit_pv(kb, pt):
                    for u in range(2):
                        kbu = kb + u
                        S.op('pe', lambda e, po=po, vt=vt, kbu=kbu, pt=pt, nq=nq, M=M, u=u: e.matmul(
                            po.ap[0:M, 0:nq], lhsT=vt.ap[:, kbu, 0:M], rhs=pt.ap[:, u * 512:u * 512 + nq], start=(kbu == kb0), stop=(kbu == kb1 - 1)),
                            reads=[vt, pt], writes=[po])
                        if not gqa:
                            S.op('pe', lambda e, psm=psm, kbu=kbu, pt=pt, nq=nq, u=u: e.matmul(
                                psm.ap[0:1, 0:nq], lhsT=k.ones_b.ap[:, 0:1], rhs=pt.ap[:, u * 512:u * 512 + nq], start=(kbu == kb0), stop=(kbu == kb1 - 1)),
                                reads=[k.ones_b, pt], writes=[psm])

                prev = None
                for kb in range(kb0, kb1, 2):
                    pt_ = emit_scores(kb)
                    if prev is not None:
                        emit_pv(*prev)
                    prev = (kb, pt_)
                emit_pv(*prev)
                pbc = bk[7]
                if gqa:
                    S.op('dve', lambda e, po=po, nq=nq: e.reciprocal(out=rs.ap[64:65, 0:nq], in_=po.ap[64:65, 0:nq]), reads=[po], writes=[rs])
                    S.op('pe', lambda e, nq=nq: e.matmul(pbc.ap[0:64, 0:nq], lhsT=k.ones_f.ap[64:65, 0:64], rhs=rs.ap[64:65, 0:nq], start=True, stop=True),
                         reads=[k.ones_f, rs], writes=[pbc])
                    _cp(S, 'act', bcs, bcs.ap[0:64, 0:nq], pbc, pbc.ap[0:64, 0:nq])
                    y = yst[cnt['y'] % 2]; cnt['y'] += 1
                    S.op('dve', lambda e, y=y, po=po, nq=nq: e.tensor_tensor(out=y.ap[0:64, 0:nq], in0=po.ap[0:64, 0:nq], in1=bcs.ap[0:64, 0:nq], op=ALU.mult),
                         reads=[po, bcs], writes=[y])
                    S.dma('sp', lambda e, y=y, hd=hd, t0=t0, nq=nq: e.dma_start(out=k.yT_d.ap[0, hd['row0']:hd['row0'] + 64, t0:t0 + nq], in_=y.ap[0:64, 0:nq]),
                          reads=[y], writes=[k.yT_d])
                else:
                    S.op('dve', lambda e, psm=psm, nq=nq: e.reciprocal(out=rs.ap[0:1, 0:nq], in_=psm.ap[0:1, 0:nq]), reads=[psm], writes=[rs])
                    S.op('pe', lambda e, nq=nq: e.matmul(pbc.ap[:, 0:nq], lhsT=k.ones_f.ap[0:1, :], rhs=rs.ap[0:1, 0:nq], start=True, stop=True),
                         reads=[k.ones_f, rs], writes=[pbc])
                    _cp(S, 'act', bcs, bcs.ap[:, 0:nq], pbc, pbc.ap[:, 0:nq])
                    if ci == 0:
                        S.op('dve', lambda e, po=po, nq=nq: e.tensor_tensor(out=o1.ap[:, 0:nq], in0=po.ap[:, 0:nq], in1=bcs.ap[:, 0:nq], op=ALU.mult),
                             reads=[po, bcs], writes=[o1])
                    else:
                        S.op('dve', lambda e, po=po, nq=nq: e.tensor_tensor(out=o2.ap[:, 0:nq], in0=po.ap[:, 0:nq], in1=bcs.ap[:, 0:nq], op=ALU.mult),
                             reads=[po, bcs], writes=[o2])
                        S.op('dve', lambda e, nq=nq: e.scalar_tensor_tensor(out=o1.ap[:, 0:nq], in0=o2.ap[:, 0:nq], scalar=neglam.ap, in1=o1.ap[:, 0:nq],
                                                                           op0=ALU.mult, op1=ALU.add), reads=[o2, neglam, o1], writes=[o1])
                        S.op('pool', lambda e, nq=nq: e.tensor_tensor(out=sq.ap[:, 0:nq], in0=o1.ap[:, 0:nq], in1=o1.ap[:, 0:nq], op=ALU.mult), reads=[o1], writes=[sq])
                        S.op('pe', lambda e, nq=nq: e.matmul(pbc.ap[:, 0:nq], lhsT=k.ones_f.ap, rhs=sq.ap[:, 0:nq], start=True, stop=True),
                             reads=[k.ones_f, sq], writes=[pbc])
                        S.op('act', lambda e, nq=nq: e.activation(out=bcs.ap[:, 0:nq], in_=pbc.ap[:, 0:nq], func=AF.Sqrt, bias=k.epst.ap, scale=1.0 / 128),
                             reads=[pbc, k.epst], writes=[bcs])
                        S.op('dve', lambda e, nq=nq: e.reciprocal(out=bcs.ap[:, 0:nq], in_=bcs.ap[:, 0:nq]), reads=[bcs], writes=[bcs])
                        S.op('dve', lambda e, nq=nq: e.tensor_tensor(out=o1.ap[:, 0:nq], in0=o1.ap[:, 0:nq], in1=bcs.ap[:, 0:nq], op=ALU.mult), reads=[o1, bcs], writes=[o1])
                        y = yst[cnt['y'] % 2]; cnt['y'] += 1
                        S.op('act', lambda e, y=y, nq=nq: e.activation(out=y.ap[:, 0:nq], in_=o1.ap[:, 0:nq], func=AF.Identity, scale=sublc.ap),
                             reads=[o1, sublc], writes=[y])
                        S.dma('sp', lambda e, y=y, hd=hd, t0=t0, nq=nq: e.dma_start(out=k.yT_d.ap[1, hd['row0']:hd['row0'] + 128, t0:t0 + nq], in_=y.ap[:, 0:nq]),
                              reads=[y], writes=[k.yT_d])


def _load_cast(k, dst, dst_ap_fn, src_ap_fn, nchunks, stage, wd, idx0=0):
    S = k.S
    for i in range(nchunks):
        w = stage[(idx0 + i) % 2]
        sap = src_ap_fn(i)
        kc = sap.shape[1]
        S.dma('sp', lambda e, w=w, sap=sap, kc=kc: e.dma_start(out=w.ap[:, 0:kc, 0:wd], in_=sap), writes=[w])
        S.op(('pool', 'dve')[i % 2], lambda e, w=w, i=i, kc=kc: e.tensor_copy(out=dst_ap_fn(i), in_=w.ap[:, 0:kc, 0:wd]), reads=[w], writes=[dst])


def phase4_merge(k, li, need_ctx):
    S, ar, I = k.S, k.ar, k.I
    bk = k.banks
    N, CTX, T = k.N, k.CTX, k.T
    wg = ar.alloc('wg', [128, 24, D], BF16)
    wbr = ar.alloc('wbr', [128, 12, D], BF16)
    wo = ar.alloc('wo', [128, 8, D], BF16)
    stage = [ar.alloc(f'st4{i}', [128, 8, 256], F32) for i in range(2)]
    for i in range(3):
        src = I['w_merge_gate'][li, i].rearrange("(c p) n -> p c n", p=128)
        _load_cast(k, wg, lambda q, i=i: wg.ap[:, i * 8:(i + 1) * 8, q * 256:(q + 1) * 256], lambda q, src=src: src[:, :, q * 256:(q + 1) * 256], 4, stage, 256)
        srcb = I['w_branch'][li, i].rearrange("(c p) n -> p c n", p=128)
        _load_cast(k, wbr, lambda q, i=i: wbr.ap[:, i * 4:(i + 1) * 4, q * 256:(q + 1) * 256], lambda q, srcb=srcb: srcb[:, :, q * 256:(q + 1) * 256], 4, stage, 256)
    srco = I['w_out'][li].rearrange("(c p) n -> p c n", p=128)
    _load_cast(k, wo, lambda q: wo.ap[:, :, q * 256:(q + 1) * 256], lambda q: srco[:, :, q * 256:(q + 1) * 256], 4, stage, 256)
    hTs = ar.alloc('hTs4', [128, 8, 512], BF16)
    ys = ar.alloc('ys4', [128, 12, 512], BF16)
    mT = ar.alloc('mT', [128, 8, 512], BF16)
    sg = [ar.alloc(f'sg{i}', [128, 512], F32) for i in range(2)]
    acc = ar.alloc('acc4', [128, 512], F32)
    prod = ar.alloc('prod4', [128, 512], F32)
    xtb = [ar.alloc(f'xt4{i}', [128, D], F32) for i in range(2)]
    tmp = ar.alloc('tmp4', [128, D], F32)
    cnt = 0
    tc = 0
    for (s0, ntok, is_ctx) in _spans(k):
        if is_ctx and not need_ctx:
            continue
        jj = 1 if is_ctx else 0
        S.dma('sp', lambda e, s0=s0, ntok=ntok: e.dma_start(out=hTs.ap[:, :, 0:ntok], in_=k.hT_d.ap[:, :, s0:s0 + ntok]), reads=[k.hT_d], writes=[hTs])
        for i in range(3):
            S.dma('sp', lambda e, i=i, s0=s0, ntok=ntok: e.dma_start(
                out=ys.ap[:, i * 4:(i + 1) * 4, 0:ntok], in_=k.yT_d.ap[i, :, s0:s0 + ntok].rearrange("(c p) t -> p c t", p=128)),
                reads=[k.yT_d], writes=[ys])
        for oc in range(8):
            for i in range(3):
                pa = bk[cnt % 2]; pb = bk[2 + cnt % 2]; s_ = sg[cnt % 2]; cnt += 1
                for c in range(8):
                    S.op('pe', lambda e, pa=pa, i=i, c=c, oc=oc, ntok=ntok: e.matmul(
                        pa.ap[:, 0:ntok], lhsT=wg.ap[:, i * 8 + c, oc * 128:(oc + 1) * 128], rhs=hTs.ap[:, c, 0:ntok], start=(c == 0), stop=(c == 7)),
                        reads=[wg, hTs], writes=[pa])
                for c in range(4):
                    S.op('pe', lambda e, pb=pb, i=i, c=c, oc=oc, ntok=ntok: e.matmul(
                        pb.ap[:, 0:ntok], lhsT=wbr.ap[:, i * 4 + c, oc * 128:(oc + 1) * 128], rhs=ys.ap[:, i * 4 + c, 0:ntok], start=(c == 0), stop=(c == 3)),
                        reads=[wbr, ys], writes=[pb])
                S.op('act', lambda e, pa=pa, s_=s_, ntok=ntok: e.activation(out=s_.ap[:, 0:ntok], in_=pa.ap[:, 0:ntok], func=AF.Sigmoid), reads=[pa], writes=[s_])
                if i == 0:
                    S.op('dve', lambda e, pb=pb, s_=s_, ntok=ntok: e.tensor_tensor(out=acc.ap[:, 0:ntok], in0=pb.ap[:, 0:ntok], in1=s_.ap[:, 0:ntok], op=ALU.mult),
                         reads=[pb, s_], writes=[acc])
                else:
                    S.op('dve', lambda e, pb=pb, s_=s_, ntok=ntok: e.tensor_tensor(out=prod.ap[:, 0:ntok], in0=pb.ap[:, 0:ntok], in1=s_.ap[:, 0:ntok], op=ALU.mult),
                         reads=[pb, s_], writes=[prod])
                    if i == 1:
                        S.op('pool', lambda e, ntok=ntok: e.tensor_tensor(out=acc.ap[:, 0:ntok], in0=acc.ap[:, 0:ntok], in1=prod.ap[:, 0:ntok], op=ALU.add),
                             reads=[acc, prod], writes=[acc])
                    else:
                        S.op('pool', lambda e, oc=oc, ntok=ntok: e.tensor_tensor(out=mT.ap[:, oc, 0:ntok], in0=acc.ap[:, 0:ntok], in1=prod.ap[:, 0:ntok], op=ALU.add),
                             reads=[acc, prod], writes=[mT])
        for j in range(ntok // 128):
            tok0 = s0 + j * 128
            xt = xtb[tc % 2]; tc += 1
            src_ap, src_res = _xsrc(k, li, tok0)
            S.dma('sp', lambda e, xt=xt, src_ap=src_ap: e.dma_start(out=xt.ap, in_=src_ap), reads=[src_res] if src_res else [], writes=[xt])
            for g in range(2):
                pb = bk[4 + g]
                for c in range(8):
                    S.op('pe', lambda e, pb=pb, g=g, c=c, j=j: e.matmul(pb.ap, lhsT=mT.ap[:, c, j * 128:(j + 1) * 128], rhs=wo.ap[:, c, g * 512:(g + 1) * 512],
                                                                      start=(c == 0), stop=(c == 7)), reads=[mT, wo], writes=[pb])
                S.op('dve', lambda e, pb=pb, g=g, jj=jj: e.tensor_tensor(out=tmp.ap[:, g * 512:(g + 1) * 512], in0=pb.ap, in1=k.modb[jj][2].ap[:, g * 512:(g + 1) * 512], op=ALU.mult),
                     reads=[pb, k.modb[jj][2]], writes=[tmp])
            S.op('pool', lambda e, xt=xt: e.tensor_tensor(out=xt.ap, in0=xt.ap, in1=tmp.ap, op=ALU.add), reads=[xt, tmp], writes=[xt])
            S.dma('sp', lambda e, xt=xt, tok0=tok0: e.dma_start(out=k.xs[0].ap[tok0:tok0 + 128, :], in_=xt.ap), reads=[xt], writes=[k.xs[0]])


def _breg(k, e, val):
    if not hasattr(k, '_bregs'):
        k._bregs = {}
    if val not in k._bregs:
        k._bregs[val] = e.to_reg(int(val))
    return k._bregs[val]


def phase5_moe(k, li, need_ctx):
    S, ar, I = k.S, k.ar, k.I
    bk = k.banks
    N, CTX, T = k.N, k.CTX, k.T
    cap = N // 8
    capc = CTX // 8
    SLOTS = cap + (capc if need_ctx else 0)
    BIG = 1.0e6
    t_first = 0 if need_ctx else CTX // 128
    ntile = T // 128
    tiles = list(range(t_first, ntile))
    wr = ar.alloc('wr', [128, 8, NEXP], F32)
    S.dma('sp', lambda e: e.dma_start(out=wr.ap, in_=I['w_router'][li].rearrange("(c p) n -> p c n", p=128)), writes=[wr])
    ustr = ar.alloc('ustr', [128, 128], BF16)
    ustf = ar.alloc('ustf', [128, 128], F32)
    S.dma('sp', lambda e: e.dma_start(out=ustf.ap, in_=I['ustrict']), writes=[ustf])
    S.op('dve', lambda e: e.tensor_copy(out=ustr.ap, in_=ustf.ap), reads=[ustf], writes=[ustr])
    affTM = ar.alloc('affTM', [128, ntile, NEXP], F32)
    maskw = ar.alloc('maskw', [128, ntile, NEXP], F32)
    maskb = ar.alloc('maskb', [128, ntile, NEXP], BF16)
    idxf = ar.alloc('idxf', [128, ntile, NEXP], F32)
    idxi = ar.alloc('idxi', [128, ntile, NEXP], I32)
    m_keep = ar.mark()
    affT = ar.alloc('affT', [NEXP, T], F32)
    m5 = ar.mark()
    xtb = [ar.alloc(f'xt5{i}', [128, D], F32) for i in range(2)]
    sqs = ar.alloc('sq5', [128, D], F32)
    ssb = [ar.alloc(f'ss5{i}', [128, 1], F32) for i in range(2)]
    rsb = [ar.alloc(f'rs5{i}', [128, 1], F32) for i in range(2)]
    xnf = [ar.alloc(f'xnf{i}', [128, D], F32) for i in range(2)]
    xnb = [ar.alloc(f'xnb{i}', [128, D], BF16) for i in range(2)]
    h2T = [ar.alloc(f'h2T{i}', [128, 8, 128], F32) for i in range(2)]
    mx = ar.alloc('mx', [128, 1], F32)
    sm = ar.alloc('sm', [128, 1], F32)
    ex = ar.alloc('ex', [128, NEXP], F32)
    for ti, t in enumerate(tiles):
        tok0 = t * 128
        jj = 1 if tok0 < CTX else 0
        b = ti % 2
        xt, ss, rstd = xtb[b], ssb[b], rsb[b]
        _norm_tile(k, k.xs[0].ap[tok0:tok0 + 128, :], k.xs[0], xt, sqs, ss, rstd)
        S.op('dve', lambda e, b=b, xt=xt, rstd=rstd: e.tensor_scalar(out=xnf[b].ap, in0=xt.ap, scalar1=rstd.ap, scalar2=None, op0=ALU.mult),
             reads=[xt, rstd], writes=[xnf[b]])
        S.op('pool', lambda e, b=b: e.tensor_copy(out=xnb[b].ap, in_=xnf[b].ap), reads=[xnf[b]], writes=[xnb[b]])
        S.dma('sp', lambda e, b=b, tok0=tok0: e.dma_start(out=k.xn_d.ap[tok0:tok0 + 128, :], in_=xnb[b].ap), reads=[xnb[b]], writes=[k.xn_d])
        for half in range(2):
            pT = bank_view(bk[half], F32, [128, 4, 128])
            for c4 in range(4):
                c = half * 4 + c4
                S.op('pe', lambda e, b=b, c=c, c4=c4, pT=pT: e.transpose(out=pT[:, c4, :], in_=xnf[b].ap[:, c * 128:(c + 1) * 128], identity=k.identf.ap),
                     reads=[xnf[b], k.identf], writes=[bk[half]])
            for c4 in range(4):
                c = half * 4 + c4
                S.op('dve' if c4 % 2 else 'act', (lambda e, b=b, c=c, c4=c4, pT=pT, jj=jj: e.tensor_scalar(
                    out=h2T[b].ap[:, c, :], in0=pT[:, c4, :], scalar1=k.colA2.ap[:, c, jj:jj + 1], scalar2=k.colB2.ap[:, c, jj:jj + 1], op0=ALU.mult, op1=ALU.add))
                    if c4 % 2 else (lambda e, b=b, c=c, c4=c4, pT=pT, jj=jj: e.activation(
                        out=h2T[b].ap[:, c, :], in_=pT[:, c4, :], func=AF.Identity, scale=k.colA2.ap[:, c, jj:jj + 1], bias=k.colB2.ap[:, c, jj:jj + 1])),
                    reads=[bk[half], k.colA2, k.colB2], writes=[h2T[b]])
        pl = bk[2 + ti % 2]
        for c in range(8):
            S.op('pe', lambda e, pl=pl, b=b, c=c: e.matmul(pl.ap[:, 0:NEXP], lhsT=h2T[b].ap[:, c, :], rhs=wr.ap[:, c, :], start=(c == 0), stop=(c == 7)),
                 reads=[h2T[b], wr], writes=[pl])
        S.op('dve', lambda e, pl=pl: e.tensor_reduce(out=mx.ap, in_=pl.ap[:, 0:NEXP], axis=AX.X, op=ALU.max), reads=[pl], writes=[mx])
        S.op('dve', lambda e: e.tensor_scalar(out=mx.ap, in0=mx.ap, scalar1=-1.0, scalar2=None, op0=ALU.mult), reads=[mx], writes=[mx])
        S.op('act', lambda e, pl=pl: e.activation(out=ex.ap, in_=pl.ap[:, 0:NEXP], func=AF.Exp, bias=mx.ap, scale=1.0, accum_out=sm.ap), reads=[pl, mx], writes=[ex, sm])
        S.op('dve', lambda e: e.reciprocal(out=sm.ap, in_=sm.ap), reads=[sm], writes=[sm])
        S.op('dve', lambda e, t=t: e.tensor_scalar(out=affTM.ap[:, t, :], in0=ex.ap, scalar1=sm.ap, scalar2=None, op0=ALU.mult), reads=[ex, sm], writes=[affTM])
        pa = bk[4 + ti % 2]
        S.op('pe', lambda e, pa=pa, t=t: e.transpose(out=pa.ap[0:NEXP, 0:128], in_=affTM.ap[:, t, :], identity=k.identf.ap), reads=[affTM, k.identf], writes=[pa])
        _cp(S, 'act', affT, affT.ap[:, tok0:tok0 + 128], pa, pa.ap[0:NEXP, 0:128])
    ar.reset(m5)
    scr = ar.alloc('scr5', [NEXP, max(N, CTX)], F32)
    lo = ar.alloc('lo', [NEXP, 2], F32)
    hi = ar.alloc('hi', [NEXP, 2], F32)
    mid = ar.alloc('mid', [NEXP, 2], F32)
    cn = ar.alloc('cn', [NEXP, 2], F32)
    dl = ar.alloc('dl', [NEXP, 2], F32)
    S.op('dve', lambda e: e.memset(lo.ap, 0.0), writes=[lo])
    S.op('dve', lambda e: e.memset(hi.ap, 1.0), writes=[hi])
    segs = [(0, CTX, N, float(cap))]
    if need_ctx:
        segs.append((1, 0, CTX, float(capc)))
    for it in range(30):
        S.op('dve', lambda e: e.tensor_tensor(out=mid.ap, in0=lo.ap, in1=hi.ap, op=ALU.add), reads=[lo, hi], writes=[mid])
        S.op('dve', lambda e: e.tensor_scalar(out=mid.ap, in0=mid.ap, scalar1=0.5, scalar2=None, op0=ALU.mult), reads=[mid], writes=[mid])
        for (col, t0, n, cp_) in segs:
            S.op('dve', lambda e, col=col, t0=t0, n=n: e.tensor_scalar(out=scr.ap[:, 0:n], in0=affT.ap[:, t0:t0 + n], scalar1=mid.ap[:, col:col + 1], scalar2=None, op0=ALU.is_ge),
                 reads=[affT, mid], writes=[scr])
            S.op('dve', lambda e, col=col, n=n: e.tensor_reduce(out=cn.ap[:, col:col + 1], in_=scr.ap[:, 0:n], axis=AX.X, op=ALU.add), reads=[scr], writes=[cn])
            S.op('dve', lambda e, col=col, cp_=cp_: e.tensor_scalar(out=cn.ap[:, col:col + 1], in0=cn.ap[:, col:col + 1], scalar1=cp_, scalar2=None, op0=ALU.is_ge),
                 reads=[cn], writes=[cn])
            S.op('dve', lambda e, col=col: e.tensor_tensor(out=dl.ap[:, col:col + 1], in0=mid.ap[:, col:col + 1], in1=lo.ap[:, col:col + 1], op=ALU.subtract),
                 reads=[mid, lo], writes=[dl])
            S.op('dve', lambda e, col=col: e.scalar_tensor_tensor(out=lo.ap[:, col:col + 1], in0=dl.ap[:, col:col + 1], scalar=cn.ap[:, col:col + 1], in1=lo.ap[:, col:col + 1],
                                                                 op0=ALU.mult, op1=ALU.add), reads=[dl, cn, lo], writes=[lo])
            S.op('dve', lambda e, col=col: e.tensor_tensor(out=dl.ap[:, col:col + 1], in0=hi.ap[:, col:col + 1], in1=mid.ap[:, col:col + 1], op=ALU.subtract),
                 reads=[mid, hi], writes=[dl])
            S.op('dve', lambda e, col=col: e.scalar_tensor_tensor(out=hi.ap[:, col:col + 1], in0=dl.ap[:, col:col + 1], scalar=cn.ap[:, col:col + 1], in1=mid.ap[:, col:col + 1],
                                                                 op0=ALU.mult, op1=ALU.add), reads=[dl, cn, mid], writes=[hi])
    thrB = ar.alloc('thrB', [128, 2, NEXP], F32)
    trow = ar.alloc('trow', [1, 2, NEXP], F32)
    for col in range(2 if need_ctx else 1):
        S.op('pe', lambda e, col=col: e.transpose(out=bk[0].ap[0:1, 0:NEXP], in_=lo.ap[:, col:col + 1], identity=k.identf.ap[0:NEXP, 0:NEXP]),
             reads=[lo, k.identf], writes=[bk[0]])
        _cp(S, 'dve', trow, trow.ap[:, col, :], bk[0], bk[0].ap[0:1, 0:NEXP])
        S.op('pe', lambda e, col=col: e.matmul(bk[1].ap[:, 0:NEXP], lhsT=k.ones_f.ap[0:1, :], rhs=trow.ap[:, col, :], start=True, stop=True),
             reads=[k.ones_f, trow], writes=[bk[1]])
        _cp(S, 'dve', thrB, thrB.ap[:, col, :], bk[1], bk[1].ap[:, 0:NEXP])
    xg = [ar.alloc(f'xg{i}', [128, D], BF16) for i in range(3)]
    for t in tiles:
        col = 1 if t * 128 < CTX else 0
        S.op('dve', lambda e, t=t, col=col: e.tensor_tensor(out=maskb.ap[:, t, :], in0=affTM.ap[:, t, :], in1=thrB.ap[:, col, :], op=ALU.is_ge),
             reads=[affTM, thrB], writes=[maskb])
        S.op('dve', lambda e, t=t: e.tensor_tensor(out=maskw.ap[:, t, :], in0=maskb.ap[:, t, :], in1=affTM.ap[:, t, :], op=ALU.mult),
             reads=[maskb, affTM], writes=[maskw])
    for ti, t in enumerate(tiles):
        tok0 = t * 128
        is_c = tok0 < CTX
        seg0 = 0 if is_c else CTX // 128
        pp = bk[ti % 4]
        S.op('pe', lambda e, pp=pp, t=t, seg0=seg0: e.matmul(pp.ap[:, 0:NEXP], lhsT=ustr.ap, rhs=maskb.ap[:, t, :], start=True, stop=(t == seg0)),
             reads=[ustr, maskb], writes=[pp])
        for t2 in range(seg0, t):
            S.op('pe', lambda e, pp=pp, t2=t2, t=t: e.matmul(pp.ap[:, 0:NEXP], lhsT=k.ones_b.ap, rhs=maskb.ap[:, t2, :], start=False, stop=(t2 == t - 1)),
                 reads=[k.ones_b, maskb], writes=[pp])
        base = float(cap) if is_c else 0.0
        S.op('dve', lambda e, t=t, base=base: e.tensor_scalar(out=idxf.ap[:, t, :], in0=maskb.ap[:, t, :], scalar1=-BIG, scalar2=BIG + base, op0=ALU.mult, op1=ALU.add),
             reads=[maskb], writes=[idxf])
        S.op('dve', lambda e, t=t, pp=pp: e.tensor_tensor(out=idxf.ap[:, t, :], in0=idxf.ap[:, t, :], in1=pp.ap[:, 0:NEXP], op=ALU.add), reads=[idxf, pp], writes=[idxf])
        S.op('dve', lambda e, t=t: e.tensor_copy(out=idxi.ap[:, t, :], in_=idxf.ap[:, t, :]), reads=[idxf], writes=[idxi])
        x_ = xg[ti % 3]
        S.dma('sp', lambda e, x_=x_, tok0=tok0: e.dma_start(out=x_.ap, in_=k.xn_d.ap[tok0:tok0 + 128, :]), reads=[k.xn_d], writes=[x_])
        bound = (cap + capc - 1) if is_c else (cap - 1)
        for ex_ in range(NEXP):
            S.dma('pool', lambda e, x_=x_, t=t, ex_=ex_, bound=bound: e.indirect_dma_start(
                out=k.xin_d[ex_].ap, out_offset=bass.IndirectOffsetOnAxis(ap=idxi.ap[:, t, ex_:ex_ + 1], axis=0), in_=x_.ap, in_offset=None,
                bounds_check=_breg(k, e, bound), oob_is_err=False), reads=[x_, idxi], writes=[k.xin_d[ex_]])
    k.S.barrier()
    ar.reset(m_keep)
    m5d = ar.mark()
    wE = [[ar.alloc(f'wE{b}{m}', [128, 8, D], BF16) for m in range(3)] for b in range(2)]
    stage = [ar.alloc(f'st5{i}', [128, 8, 256], F32) for i in range(2)]
    xe = [ar.alloc(f'xe{i}', [128, D], BF16) for i in range(2)]
    SP = (SLOTS + 127) // 128 * 128
    xT = ar.alloc('xT5', [128, 8, SP], BF16)
    hidT = ar.alloc('hidT', [128, 8, SP], BF16)
    sgb = [ar.alloc(f'sg5{i}', [128, 512], F32) for i in range(2)]
    yst = [ar.alloc('yst5', [128, D], F32)] * 2
    stiles = [(r0, min(128, SLOTS - r0)) for r0 in range(0, SLOTS, 128)]
    schunks = [(c0, min(512, SLOTS - c0)) for c0 in range(0, SLOTS, 512)]
    cnt = 0
    for ex_ in range(NEXP):
        wb_ = wE[ex_ % 2]
        for m, nm in enumerate(('w_exp_gate', 'w_exp_up', 'w_exp_down')):
            src = I[nm][li, ex_].rearrange("(c p) n -> p c n", p=128)
            _load_cast(k, wb_[m], lambda q, m=m, wb_=wb_: wb_[m].ap[:, :, q * 256:(q + 1) * 256], lambda q, src=src: src[:, :, q * 256:(q + 1) * 256], 4, stage, 256)
        for si, (r0, nr) in enumerate(stiles):
            x_ = xe[si % 2]
            S.dma('sp', lambda e, x_=x_, ex_=ex_, r0=r0, nr=nr: e.dma_start(out=x_.ap[0:nr, :], in_=k.xin_d[ex_].ap[r0:r0 + nr, :]), reads=[k.xin_d[ex_]], writes=[x_])
            pT = bank_view(bk[7], BF16, [128, 8, 128])
            for c in range(8):
                S.op('pe', lambda e, x_=x_, c=c, nr=nr: e.transpose(out=pT[:, c, 0:nr], in_=x_.ap[0:nr, c * 128:(c + 1) * 128], identity=k.identb.ap[0:nr, 0:nr]),
                     reads=[x_, k.identb], writes=[bk[7]])
            rngs = []
            if r0 < cap:
                rngs.append((0, min(nr, cap - r0), 0))
            if r0 + nr > cap:
                rngs.append((max(0, cap - r0), nr, 1))
            for c in range(8):
                for (a_, b_, jj) in rngs:
                    S.op('dve' if c % 2 else 'act', (lambda e, c=c, a_=a_, b_=b_, jj=jj, r0=r0: e.tensor_scalar(
                        out=xT.ap[:, c, r0 + a_:r0 + b_], in0=pT[:, c, a_:b_], scalar1=k.colA2.ap[:, c, jj:jj + 1], scalar2=k.colB2.ap[:, c, jj:jj + 1], op0=ALU.mult, op1=ALU.add))
                        if c % 2 else (lambda e, c=c, a_=a_, b_=b_, jj=jj, r0=r0: e.activation(
                            out=xT.ap[:, c, r0 + a_:r0 + b_], in_=pT[:, c, a_:b_], func=AF.Identity, scale=k.colA2.ap[:, c, jj:jj + 1], bias=k.colB2.ap[:, c, jj:jj + 1])),
                        reads=[bk[7], k.colA2, k.colB2], writes=[xT])
        for fc in range(8):
            for (c0, ncol) in schunks:
                pg = bk[cnt % 2]; pu = bk[2 + cnt % 2]; s_ = sgb[cnt % 2]; cnt += 1
                for (pp_, m) in ((pg, 0), (pu, 1)):
                    for c in range(8):
                        S.op('pe', lambda e, pp_=pp_, m=m, c=c, fc=fc, c0=c0, ncol=ncol, wb_=wb_: e.matmul(
                            pp_.ap[:, 0:ncol], lhsT=wb_[m].ap[:, c, fc * 128:(fc + 1) * 128], rhs=xT.ap[:, c, c0:c0 + ncol], start=(c == 0), stop=(c == 7)),
                            reads=[wb_[m], xT], writes=[pp_])
                S.op('act', lambda e, pg=pg, s_=s_, ncol=ncol: e.activation(out=s_.ap[:, 0:ncol], in_=pg.ap[:, 0:ncol], func=AF.Silu), reads=[pg], writes=[s_])
                S.op('dve', lambda e, pu=pu, s_=s_, fc=fc, c0=c0, ncol=ncol: e.tensor_tensor(out=hidT.ap[:, fc, c0:c0 + ncol], in0=pu.ap[:, 0:ncol], in1=s_.ap[:, 0:ncol], op=ALU.mult),
                     reads=[pu, s_], writes=[hidT])
        for si, (r0, nr) in enumerate(stiles):
            y_ = yst[si % 2]
            for g in range(2):
                pb = bk[4 + g]
                for fc in range(8):
                    S.op('pe', lambda e, pb=pb, g=g, fc=fc, r0=r0, nr=nr, wb_=wb_: e.matmul(
                        pb.ap[0:nr, :], lhsT=hidT.ap[:, fc, r0:r0 + nr], rhs=wb_[2].ap[:, fc, g * 512:(g + 1) * 512], start=(fc == 0), stop=(fc == 7)),
                        reads=[hidT, wb_[2]], writes=[pb])
                _cp(S, 'act' if g else 'dve', y_, y_.ap[0:nr, g * 512:(g + 1) * 512], pb, pb.ap[0:nr, :])
            S.dma('sp', lambda e, y_=y_, ex_=ex_, r0=r0, nr=nr: e.dma_start(out=k.yexp_d[ex_].ap[r0:r0 + nr, :], in_=y_.ap[0:nr, :]), reads=[y_], writes=[k.yexp_d[ex_]])
    k.S.barrier()
    ar.reset(m5d)
    gb = [ar.alloc(f'gb5{i}', [128, D], F32) for i in range(4)]
    for g_ in gb:
        S.op('pool', lambda e, g_=g_: e.memset(g_.ap, 0.0), writes=[g_])
    accb = [ar.alloc(f'acc5{i}', [128, D], F32) for i in range(2)]
    xtb = [ar.alloc(f'xt5e{i}', [128, D], F32) for i in range(2)]
    gcnt = 0
    last = not need_ctx
    for ti, t in enumerate(tiles):
        tok0 = t * 128
        is_c = tok0 < CTX
        jj = 1 if is_c else 0
        bound = (cap + capc - 1) if is_c else (cap - 1)
        acc = accb[ti % 2]
        xt = xtb[ti % 2]
        S.dma('sp', lambda e, xt=xt, tok0=tok0: e.dma_start(out=xt.ap, in_=k.xs[0].ap[tok0:tok0 + 128, :]), reads=[k.xs[0]], writes=[xt])
        for ex_ in range(NEXP):
            g_ = gb[gcnt % 4]; gcnt += 1
            S.dma('pool', lambda e, g_=g_, t=t, ex_=ex_, bound=bound: e.indirect_dma_start(
                out=g_.ap, out_offset=None, in_=k.yexp_d[ex_].ap, in_offset=bass.IndirectOffsetOnAxis(ap=idxi.ap[:, t, ex_:ex_ + 1], axis=0),
                bounds_check=_breg(k, e, bound), oob_is_err=False), reads=[k.yexp_d[ex_], idxi], writes=[g_])
            if ex_ == 0:
                S.op('dve', lambda e, g_=g_, acc=acc, t=t, ex_=ex_: e.tensor_scalar(out=acc.ap, in0=g_.ap, scalar1=maskw.ap[:, t, ex_:ex_ + 1], scalar2=None, op0=ALU.mult),
                     reads=[g_, maskw], writes=[acc])
            else:
                S.op('dve', lambda e, g_=g_, acc=acc, t=t, ex_=ex_: e.scalar_tensor_tensor(out=acc.ap, in0=g_.ap, scalar=maskw.ap[:, t, ex_:ex_ + 1], in1=acc.ap, op0=ALU.mult, op1=ALU.add),
                     reads=[g_, maskw, acc], writes=[acc])
        S.op('pool', lambda e, acc=acc, jj=jj: e.tensor_tensor(out=acc.ap, in0=acc.ap, in1=k.modb[jj][5].ap, op=ALU.mult), reads=[acc, k.modb[jj][5]], writes=[acc])
        S.op('pool', lambda e, acc=acc, xt=xt: e.tensor_tensor(out=xt.ap, in0=xt.ap, in1=acc.ap, op=ALU.add), reads=[acc, xt], writes=[xt])
        if last:
            S.dma('sp', lambda e, xt=xt, tok0=tok0: e.dma_start(out=k.out.ap[tok0 - CTX:tok0 - CTX + 128, :], in_=xt.ap), reads=[xt], writes=[k.out])
        else:
            S.dma('sp', lambda e, xt=xt, tok0=tok0: e.dma_start(out=k.xs[1].ap[tok0:tok0 + 128, :], in_=xt.ap), reads=[xt], writes=[k.xs[1]])


def phase3_gdn(k, li, need_ctx):
    S, ar, I = k.S, k.ar, k.I
    bk = k.banks
    N, CTX, T = k.N, k.CTX, k.T
    cw = ar.alloc('cw', [128, 12, 3], F32)
    for cc in range(12):
        S.dma('sp', lambda e, cc=cc: e.dma_start(out=cw.ap[:, cc, :], in_=I['gdn_conv_w'][li][:, cc * 128:(cc + 1) * 128].rearrange("k p -> p k"),
                                                 allow_slow_non_contiguous=True), writes=[cw])
    m3 = ar.mark()
    xin = [ar.alloc(f'cxin{i}', [128, 514], F32) for i in range(2)]
    yb = [ar.alloc(f'cy{i}', [128, 512], F32) for i in range(2)]
    sq = ar.alloc('csq', [128, 512], F32)
    rn = ar.alloc('crn', [128, 512], F32)
    cnt = 0
    for (s0, ntok, is_ctx) in _spans(k):
        seg0, seg1 = (0, CTX) if is_ctx else (CTX, T)
        for cc in range(12):
            x_ = xin[cnt % 2]; y_ = yb[cnt % 2]; cnt += 1
            a0 = max(s0 - 1, seg0); a1 = min(s0 + ntok + 1, seg1)
            off = a0 - (s0 - 1)
            if s0 - 1 < seg0:
                S.op('pool', lambda e, x_=x_: e.memset(x_.ap[:, 0:1], 0.0), writes=[x_])
            if s0 + ntok + 1 > seg1:
                S.op('pool', lambda e, x_=x_, ntok=ntok: e.memset(x_.ap[:, ntok + 1:ntok + 2], 0.0), writes=[x_])
            S.dma('sp', lambda e, x_=x_, cc=cc, a0=a0, a1=a1, off=off: e.dma_start(out=x_.ap[:, off:off + a1 - a0], in_=k.cT_d.ap[cc, :, a0:a1]),
                  reads=[k.cT_d], writes=[x_])
            S.op('dve', lambda e, x_=x_, y_=y_, cc=cc, ntok=ntok: e.tensor_scalar(out=y_.ap[:, 0:ntok], in0=x_.ap[:, 0:ntok], scalar1=cw.ap[:, cc, 0:1], scalar2=None, op0=ALU.mult),
                 reads=[x_, cw], writes=[y_])
            for tap in (1, 2):
                S.op('dve', lambda e, x_=x_, y_=y_, cc=cc, ntok=ntok, tap=tap: e.scalar_tensor_tensor(
                    out=y_.ap[:, 0:ntok], in0=x_.ap[:, tap:tap + ntok], scalar=cw.ap[:, cc, tap:tap + 1], in1=y_.ap[:, 0:ntok], op0=ALU.mult, op1=ALU.add),
                    reads=[x_, cw, y_], writes=[y_])
            S.op('act', lambda e, y_=y_, ntok=ntok: e.activation(out=y_.ap[:, 0:ntok], in_=y_.ap[:, 0:ntok], func=AF.Silu), reads=[y_], writes=[y_])
            if cc < 8:
                pb = bk[cnt % 4]
                S.op('pool', lambda e, y_=y_, ntok=ntok: e.tensor_tensor(out=sq.ap[:, 0:ntok], in0=y_.ap[:, 0:ntok], in1=y_.ap[:, 0:ntok], op=ALU.mult), reads=[y_], writes=[sq])
                S.op('pe', lambda e, pb=pb, ntok=ntok: e.matmul(pb.ap[:, 0:ntok], lhsT=k.ones_f.ap, rhs=sq.ap[:, 0:ntok], start=True, stop=True),
                     reads=[k.ones_f, sq], writes=[pb])
                S.op('act', lambda e, pb=pb, ntok=ntok: e.activation(out=rn.ap[:, 0:ntok], in_=pb.ap[:, 0:ntok], func=AF.Sqrt, bias=k.epst.ap, scale=1.0),
                     reads=[pb, k.epst], writes=[rn])
                S.op('dve', lambda e, ntok=ntok: e.reciprocal(out=rn.ap[:, 0:ntok], in_=rn.ap[:, 0:ntok]), reads=[rn], writes=[rn])
                sc_ = (128.0 ** -0.5) if cc < 4 else 1.0
                S.op('dve', lambda e, y_=y_, ntok=ntok, sc_=sc_: e.scalar_tensor_tensor(out=y_.ap[:, 0:ntok], in0=y_.ap[:, 0:ntok], scalar=sc_, in1=rn.ap[:, 0:ntok],
                                                                                      op0=ALU.mult, op1=ALU.mult), reads=[y_, rn], writes=[y_])
            S.dma('sp', lambda e, y_=y_, cc=cc, s0=s0, ntok=ntok: e.dma_start(out=k.cP_d.ap[cc, :, s0:s0 + ntok], in_=y_.ap[:, 0:ntok]), reads=[y_], writes=[k.cP_d])
    S.barrier()
    ar.reset(m3)
    gc_ = ar.alloc('gdnc', [128, 2, 5, 64], F32)
    S.dma('sp', lambda e: e.dma_start(out=gc_.ap, in_=I['gdnc'].rearrange("d p s c -> p d s c")), writes=[gc_])
    gnw = ar.alloc('gnw', [64, 128], F32)
    S.dma('sp', lambda e: e.dma_start(out=gnw.ap, in_=I['gdn_norm_w'][li].partition_broadcast(64)), writes=[gnw])
    Sf = ar.alloc('Sf', [128, 4, 128], F32)
    Sb = ar.alloc('Sb', [128, 4, 128], BF16)
    A = lambda nm, shape, dt=F32: [ar.alloc(f'{nm}{i}', shape, dt) for i in range(2)]
    qk_in = A('qk_in', [128, 8, 64]); v_in = A('v_in', [128, 4, 64]); gdup = A('gdup', [128, 4]); beta = A('beta', [64, 4])
    qkb = A('qkb', [128, 8, 64], BF16)
    lhsE = A('lhsE', [128, 4, 64])
    Em = A('Em', [64, 4, 64]); DT = A('DT', [64, 4, 64])
    gcs = A('gcs', [64, 4]); egc = A('egc', [64, 4]); negegc = A('negegc', [64, 4]); ekd = A('ekd', [64, 4]); glast = A('glast', [128, 4])
    KKD = A('KKD', [64, 4, 64]); QKD = A('QKD', [64, 4, 64], BF16)
    Mf = A('Mf', [64, 4, 64]); Mb = A('Mb', [64, 4, 64], BF16); Nb = A('Nb', [64, 4, 64], BF16)
    NM = A('NM', [64, 8, 64], BF16)
    Yf = A('Yf', [64, 4, 64]); Yb = A('Yb', [64, 4, 64], BF16)
    kdec = A('kdec', [64, 4, 128], BF16); vtok = A('vtok', [64, 4, 128])
    rp = A('rp', [64, 4, 128], BF16); vnew = A('vnew', [64, 4, 128], BF16)
    o1 = A('o1g', [64, 4, 128]); ost = A('ost', [64, 4, 128])
    ofw = A('ofw', [64, 4, 128]); zc = A('zc', [64, 4, 128], BF16)
    ssq = A('gssq', [64, 4]); yg = A('yg', [64, 4, 128], BF16); ygT = A('ygT', [128, 4, 64], BF16)
    sqg = A('sqg', [64, 4, 128])
    nch = T // 64
    nctx = CTX // 64
    it = 0
    for d in range(2):
        S.op('pool', lambda e: e.memset(Sf.ap, 0.0), writes=[Sf])
        S.op('pool', lambda e: e.memset(Sb.ap, 0.0), writes=[Sb])
        order = list(range(nctx)) + list(range(nctx, nch))
        if d == 1:
            order = list(range(nctx - 1, -1, -1)) + list(range(nch - 1, nctx - 1, -1))
        CM = gc_.ap[:, d, 0, :]; RC = gc_.ap[:, d, 1, :]; UC = gc_.ap[0:64, d, 2, :]; BM = gc_.ap[0:64, d, 3, :]; ST = gc_.ap[0:64, d, 4, :]
        bc4 = lambda ap: ap.unsqueeze(1).broadcast_to([64, 4, 64])
        for c in order:
            b = it % 2; it += 1
            tok0 = c * 64
            want_o = need_ctx or tok0 >= CTX
            qi, vi, gd, be, qb = qk_in[b], v_in[b], gdup[b], beta[b], qkb[b]
            S.dma('sp', lambda e, qi=qi, tok0=tok0: e.dma_start(out=qi.ap, in_=k.cP_d.ap[0:8, :, tok0:tok0 + 64].rearrange("c p t -> p c t")), reads=[k.cP_d], writes=[qi])
            S.dma('sp', lambda e, vi=vi, tok0=tok0: e.dma_start(out=vi.ap, in_=k.cP_d.ap[8:12, :, tok0:tok0 + 64].rearrange("c p t -> p c t")), reads=[k.cP_d], writes=[vi])
            for hf in range(2):
                S.dma('sp', lambda e, gd=gd, tok0=tok0, hf=hf, d=d: e.dma_start(out=gd.ap[hf * 64:(hf + 1) * 64, :], in_=k.gb_d.ap[tok0:tok0 + 64, 8 + d * 4:12 + d * 4]),
                      reads=[k.gb_d], writes=[gd])
            S.dma('sp', lambda e, be=be, tok0=tok0, d=d: e.dma_start(out=be.ap, in_=k.gb_d.ap[tok0:tok0 + 64, d * 4:d * 4 + 4]), reads=[k.gb_d], writes=[be])
            S.op('pool', lambda e, qi=qi, qb=qb: e.tensor_copy(out=qb.ap, in_=qi.ap), reads=[qi], writes=[qb])
            pk = bank_view(bk[0], F32, [64, 4, 128]); pv = bank_view(bk[1], F32, [64, 4, 128])
            for h in range(4):
                S.op('pe', lambda e, h=h, qi=qi, pk=pk: e.transpose(out=pk[:, h, :], in_=qi.ap[:, 4 + h, :], identity=k.identf.ap), reads=[qi, k.identf], writes=[bk[0]])
            for h in range(4):
                S.op('pe', lambda e, h=h, vi=vi, pv=pv: e.transpose(out=pv[:, h, :], in_=vi.ap[:, h, :], identity=k.identf.ap), reads=[vi, k.identf], writes=[bk[1]])
            pc = bank_view(bk[2], F32, [64, 8, 64])
            for h in range(4):
                S.op('pe', lambda e, h=h, qb=qb, pc=pc: e.matmul(pc[:, h, :], lhsT=qb.ap[:, 4 + h, :], rhs=qb.ap[:, 4 + h, :], start=True, stop=True), reads=[qb], writes=[bk[2]])
                S.op('pe', lambda e, h=h, qb=qb, pc=pc: e.matmul(pc[:, 4 + h, :], lhsT=qb.ap[:, 4 + h, :], rhs=qb.ap[:, h, :], start=True, stop=True), reads=[qb], writes=[bk[2]])
            le = lhsE[b]
            S.op('dve', lambda e, le=le, gd=gd, CM=CM: e.tensor_tensor(out=le.ap, in0=CM.unsqueeze(1).broadcast_to([128, 4, 64]), in1=gd.ap.unsqueeze(2).broadcast_to([128, 4, 64]), op=ALU.mult),
                 reads=[gc_, gd], writes=[le])
            pe_ = bank_view(bk[3], F32, [64, 4, 64])
            for h in range(4):
                S.op('pe', lambda e, h=h, le=le, RC=RC, pe_=pe_: e.matmul(pe_[:, h, :], lhsT=le.ap[:, h, :], rhs=RC, start=True, stop=True), reads=[le, gc_], writes=[bk[3]])
            pgc = bk[3].ap[0:64, 256:260]
            S.op('pe', lambda e, gd=gd, UC=UC, pgc=pgc: e.matmul(pgc, lhsT=UC, rhs=gd.ap[0:64, :], start=True, stop=True), reads=[gd, gc_], writes=[bk[3]])
            pgs = bk[3].ap[:, 264:268]
            S.op('pe', lambda e, gd=gd, pgs=pgs: e.matmul(pgs, lhsT=k.ones_f.ap[0:64, :], rhs=gd.ap[0:64, :], start=True, stop=True), reads=[gd, k.ones_f], writes=[bk[3]])
            em, dt_ = Em[b], DT[b]
            S.op('dve', lambda e, em=em, pe_=pe_, BM=BM: e.tensor_tensor(out=em.ap, in0=pe_, in1=bc4(BM), op=ALU.add), reads=[bk[3], gc_], writes=[em])
            S.op('act', lambda e, em=em, dt_=dt_: e.activation(out=dt_.ap, in_=em.ap, func=AF.Exp, scale=-1.0), reads=[em], writes=[dt_])
            S.op('dve', lambda e, b=b, pgc=pgc: e.tensor_copy(out=gcs[b].ap, in_=pgc), reads=[bk[3]], writes=[gcs[b]])
            S.op('act', lambda e, b=b, pgc=pgc: e.activation(out=egc[b].ap, in_=pgc, func=AF.Exp), reads=[bk[3]], writes=[egc[b]])
            S.op('dve', lambda e, b=b: e.tensor_scalar(out=negegc[b].ap, in0=egc[b].ap, scalar1=-1.0, scalar2=None, op0=ALU.mult), reads=[egc[b]], writes=[negegc[b]])
            S.op('dve', lambda e, b=b, pgs=pgs: e.tensor_tensor(out=ekd[b].ap, in0=pgs[0:64, :], in1=gcs[b].ap, op=ALU.subtract), reads=[bk[3], gcs[b]], writes=[ekd[b]])
            S.op('act', lambda e, b=b: e.activation(out=ekd[b].ap, in_=ekd[b].ap, func=AF.Exp), reads=[ekd[b]], writes=[ekd[b]])
            S.op('act', lambda e, b=b, pgs=pgs: e.activation(out=glast[b].ap, in_=pgs, func=AF.Exp), reads=[bk[3]], writes=[glast[b]])
            S.op('dve', lambda e, b=b, pc=pc, dt_=dt_: e.tensor_tensor(out=KKD[b].ap, in0=pc[:, 0:4, :], in1=dt_.ap, op=ALU.mult), reads=[bk[2], dt_], writes=[KKD[b]])
            S.op('dve', lambda e, b=b, pc=pc, dt_=dt_: e.tensor_tensor(out=QKD[b].ap, in0=pc[:, 4:8, :], in1=dt_.ap, op=ALU.mult), reads=[bk[2], dt_], writes=[QKD[b]])
            S.op('pool', lambda e, b=b, be=be: e.tensor_tensor(out=KKD[b].ap, in0=KKD[b].ap, in1=be.ap.unsqueeze(2).broadcast_to([64, 4, 64]), op=ALU.mult), reads=[KKD[b], be], writes=[KKD[b]])
            S.op('pool', lambda e, b=b, ST=ST: e.tensor_tensor(out=Mf[b].ap, in0=KKD[b].ap, in1=bc4(ST), op=ALU.mult), reads=[KKD[b], gc_], writes=[Mf[b]])
            S.op('pool', lambda e, b=b: e.tensor_copy(out=Mb[b].ap, in_=Mf[b].ap), reads=[Mf[b]], writes=[Mb[b]])
            S.op('dve', lambda e, b=b: e.tensor_tensor(out=Yf[b].ap, in0=bc4(k.identf.ap[0:64, 0:64]), in1=Mf[b].ap, op=ALU.subtract), reads=[k.identf, Mf[b]], writes=[Yf[b]])
            S.op('pool', lambda e, b=b: e.tensor_copy(out=Yb[b].ap, in_=Yf[b].ap), reads=[Yf[b]], writes=[Yb[b]])
            pn = bank_view(bk[4], F32, [64, 4, 64])
            for h in range(4):
                S.op('pe', lambda e, h=h, b=b, pn=pn: e.transpose(out=pn[:, h, :], in_=Mf[b].ap[:, h, :], identity=k.identf.ap[0:64, 0:64]), reads=[Mf[b], k.identf], writes=[bk[4]])
            _cp(S, 'act', Nb[b], Nb[b].ap, bk[4], pn)
            curN, curM = Nb[b], Mb[b]
            nm = NM[b]
            for lvl in range(5):
                pnm = bank_view(bk[4 + (lvl % 2)], F32, [64, 8, 64])
                lastl = lvl == 4
                cN, cM = curN, curM
                nview = (lambda cN=cN: cN.ap) if lvl == 0 else (lambda nm=nm: nm.ap[:, 0:4, :])
                mview = (lambda cM=cM: cM.ap) if lvl == 0 else (lambda nm=nm: nm.ap[:, 4:8, :])
                srcs = [curN, curM] if lvl == 0 else [nm]
                for h in range(4):
                    S.op('pe', lambda e, h=h, pnm=pnm, nview=nview, mview=mview: e.matmul(pnm[:, h, :], lhsT=mview()[:, h, :], rhs=nview()[:, h, :], start=True, stop=True),
                         reads=srcs, writes=[bk[4 + (lvl % 2)]])
                    if not lastl:
                        S.op('pe', lambda e, h=h, pnm=pnm, nview=nview, mview=mview: e.matmul(pnm[:, 4 + h, :], lhsT=nview()[:, h, :], rhs=mview()[:, h, :], start=True, stop=True),
                             reads=srcs, writes=[bk[4 + (lvl % 2)]])
                if lastl:
                    _cp(S, 'act', nm, nm.ap[:, 0:4, :], bk[4 + (lvl % 2)], pnm[:, 0:4, :])
                else:
                    _cp(S, 'act', nm, nm.ap, bk[4 + (lvl % 2)], pnm)
                py = bank_view(bk[6], F32, [64, 4, 64])
                for h in range(4):
                    S.op('pe', lambda e, h=h, py=py, nm=nm, b=b: e.matmul(py[:, h, :], lhsT=nm.ap[:, h, :], rhs=Yb[b].ap[:, h, :], start=True, stop=True),
                         reads=[nm, Yb[b]], writes=[bk[6]])
                S.op('dve', lambda e, b=b, py=py: e.tensor_tensor(out=Yf[b].ap, in0=Yf[b].ap, in1=py, op=ALU.add), reads=[Yf[b], bk[6]], writes=[Yf[b]])
                S.op('pool', lambda e, b=b: e.tensor_copy(out=Yb[b].ap, in_=Yf[b].ap), reads=[Yf[b]], writes=[Yb[b]])
            S.op('dve', lambda e, b=b, pk=pk: e.tensor_tensor(out=kdec[b].ap, in0=pk, in1=ekd[b].ap.unsqueeze(2).broadcast_to([64, 4, 128]), op=ALU.mult), reads=[bk[0], ekd[b]], writes=[kdec[b]])
            _cp(S, 'act', vtok[b], vtok[b].ap, bk[1], pv)
            pks = bank_view(bk[7], F32, [64, 4, 128]); pvn = bank_view(bk[6], F32, [64, 4, 128])
            p1 = bank_view(bk[0], F32, [64, 4, 128]); p2 = bank_view(bk[1], F32, [64, 4, 128]); pds = bank_view(bk[2], F32, [128, 4, 128])
            for h in range(4):
                S.op('pe', lambda e, h=h, qb=qb, pks=pks: e.matmul(pks[:, h, :], lhsT=qb.ap[:, 4 + h, :], rhs=Sb.ap[:, h, :], start=True, stop=True), reads=[qb, Sb], writes=[bk[7]])
                S.op('dve', lambda e, h=h, b=b, pks=pks: e.scalar_tensor_tensor(out=rp[b].ap[:, h, :], in0=pks[:, h, :], scalar=negegc[b].ap[:, h:h + 1], in1=vtok[b].ap[:, h, :],
                                                                               op0=ALU.mult, op1=ALU.add), reads=[bk[7], negegc[b], vtok[b]], writes=[rp[b]])
                S.op('pe', lambda e, h=h, b=b, pvn=pvn: e.matmul(pvn[:, h, :], lhsT=Yb[b].ap[:, h, :], rhs=rp[b].ap[:, h, :], start=True, stop=True), reads=[Yb[b], rp[b]], writes=[bk[6]])
                S.op('act', lambda e, h=h, b=b, be=be, pvn=pvn: e.activation(out=vnew[b].ap[:, h, :], in_=pvn[:, h, :], func=AF.Copy, scale=be.ap[:, h:h + 1]), reads=[bk[6], be], writes=[vnew[b]])
                if want_o:
                    S.op('pe', lambda e, h=h, qb=qb, p1=p1: e.matmul(p1[:, h, :], lhsT=qb.ap[:, h, :], rhs=Sb.ap[:, h, :], start=True, stop=True), reads=[qb, Sb], writes=[bk[0]])
                    S.op('pe', lambda e, h=h, b=b, p2=p2: e.matmul(p2[:, h, :], lhsT=QKD[b].ap[:, h, :], rhs=vnew[b].ap[:, h, :], start=True, stop=True), reads=[QKD[b], vnew[b]], writes=[bk[1]])
                S.op('pe', lambda e, h=h, b=b, pds=pds: e.matmul(pds[:, h, :], lhsT=kdec[b].ap[:, h, :], rhs=vnew[b].ap[:, h, :], start=True, stop=True), reads=[kdec[b], vnew[b]], writes=[bk[2]])
                S.op('dve', lambda e, h=h, b=b, pds=pds: e.scalar_tensor_tensor(out=Sf.ap[:, h, :], in0=Sf.ap[:, h, :], scalar=glast[b].ap[:, h:h + 1], in1=pds[:, h, :],
                                                                               op0=ALU.mult, op1=ALU.add), reads=[Sf, glast[b], bk[2]], writes=[Sf])
                S.op('pool', lambda e, h=h: e.tensor_copy(out=Sb.ap[:, h, :], in_=Sf.ap[:, h, :]), reads=[Sf], writes=[Sb])
            if not want_o:
                continue
            S.op('dve', lambda e, b=b, p1=p1: e.tensor_tensor(out=o1[b].ap, in0=p1, in1=egc[b].ap.unsqueeze(2).broadcast_to([64, 4, 128]), op=ALU.mult), reads=[bk[0], egc[b]], writes=[o1[b]])
            S.op('dve', lambda e, b=b, p2=p2: e.tensor_tensor(out=ost[b].ap, in0=o1[b].ap, in1=p2, op=ALU.add), reads=[o1[b], bk[1]], writes=[ost[b]])
            if d == 0:
                S.dma('sp', lambda e, b=b, tok0=tok0: e.dma_start(out=k.of_d.ap[tok0:tok0 + 64, :], in_=ost[b].ap.rearrange("p a b -> p (a b)")), reads=[ost[b]], writes=[k.of_d])
                continue
            S.dma('sp', lambda e, b=b, tok0=tok0: e.dma_start(out=ofw[b].ap.rearrange("p a b -> p (a b)"), in_=k.of_d.ap[tok0:tok0 + 64, :]), reads=[k.of_d], writes=[ofw[b]])
            S.dma('sp', lambda e, b=b, tok0=tok0: e.dma_start(out=zc[b].ap.rearrange("p a b -> p (a b)"), in_=k.z_d.ap[tok0:tok0 + 64, :]), reads=[k.z_d], writes=[zc[b]])
            S.op('pool', lambda e, b=b: e.tensor_tensor(out=ost[b].ap, in0=ost[b].ap, in1=ofw[b].ap, op=ALU.add), reads=[ost[b], ofw[b]], writes=[ost[b]])
            S.op('pool', lambda e, b=b: e.tensor_tensor(out=sqg[b].ap, in0=ost[b].ap, in1=ost[b].ap, op=ALU.mult), reads=[ost[b]], writes=[sqg[b]])
            S.op('dve', lambda e, b=b: e.tensor_reduce(out=ssq[b].ap, in_=sqg[b].ap, axis=AX.X, op=ALU.add), reads=[sqg[b]], writes=[ssq[b]])
            S.op('act', lambda e, b=b: e.activation(out=ssq[b].ap, in_=ssq[b].ap, func=AF.Sqrt, bias=k.epst.ap[0:64, :], scale=1.0 / 128), reads=[ssq[b], k.epst], writes=[ssq[b]])
            S.op('dve', lambda e, b=b: e.reciprocal(out=ssq[b].ap, in_=ssq[b].ap), reads=[ssq[b]], writes=[ssq[b]])
            S.op('dve', lambda e, b=b: e.tensor_tensor(out=ost[b].ap, in0=ost[b].ap, in1=ssq[b].ap.unsqueeze(2).broadcast_to([64, 4, 128]), op=ALU.mult), reads=[ost[b], ssq[b]], writes=[ost[b]])
            S.op('pool', lambda e, b=b: e.tensor_tensor(out=ost[b].ap, in0=ost[b].ap, in1=gnw.ap.unsqueeze(1).broadcast_to([64, 4, 128]), op=ALU.mult), reads=[ost[b], gnw], writes=[ost[b]])
            S.op('dve', lambda e, b=b: e.tensor_tensor(out=yg[b].ap, in0=ost[b].ap, in1=zc[b].ap, op=ALU.mult), reads=[ost[b], zc[b]], writes=[yg[b]])
            pyt = bank_view(bk[5], BF16, [128, 4, 64])
            for h in range(4):
                S.op('pe', lambda e, h=h, b=b, pyt=pyt: e.transpose(out=pyt[:, h, :], in_=yg[b].ap[:, h, :], identity=k.identb.ap[0:64, 0:64]), reads=[yg[b], k.identb], writes=[bk[5]])
            _cp(S, 'act', ygT[b], ygT[b].ap, bk[5], pyt)
            S.dma('sp', lambda e, b=b, tok0=tok0: e.dma_start(out=k.yT_d.ap[2, :, tok0:tok0 + 64].rearrange("(c p) t -> p c t", p=128), in_=ygT[b].ap), reads=[ygT[b]], writes=[k.yT_d])


def gdn_consts():
    g = np.zeros((2, 128, 5, 64), np.float32)
    kk = np.arange(64)[:, None]
    ii = np.arange(64)[None, :]
    for d in range(2):
        U = (kk <= ii) if d == 0 else (kk >= ii)
        U = U.astype(np.float32)
        valid = U
        g[d, 0:64, 0] = U
        g[d, 64:128, 0] = -1.0
        g[d, 0:64, 1] = 1.0
        g[d, 64:128, 1] = U
        g[d, 0:64, 2] = U
        g[d, 0:64, 3] = (1.0 - valid) * 30000.0
        g[d, 0:64, 4] = valid * (1.0 - np.eye(64, dtype=np.float32))
    return g


def _rope_tables(N):
    rows = N // 64
    row = np.repeat(np.arange(rows, dtype=np.float32), 64)
    col = np.tile(np.arange(64, dtype=np.float32), rows)
    inv_freq = (np.float32(10000.0) ** (-np.arange(0, 32, 2, dtype=np.float32) / np.float32(32))).astype(np.float32)
    ang = np.concatenate([row[:, None] * inv_freq, col[:, None] * inv_freq], axis=-1).astype(np.float32)
    cos, sin = np.cos(ang).astype(np.float32), np.sin(ang).astype(np.float32)
    C = np.zeros((N, 64), np.float32)
    Sg = np.zeros((N, 64), np.float32)
    for a in range(2):
        for pr in range(2):
            C[:, a * 32 + pr * 16:a * 32 + pr * 16 + 16] = cos[:, a * 16:(a + 1) * 16]
            Sg[:, a * 32 + pr * 16:a * 32 + pr * 16 + 16] = sin[:, a * 16:(a + 1) * 16] * (-1.0 if pr == 0 else 1.0)
    return C, Sg


_NC_CACHE = {}


def kernel(**inputs):
    B, N, _ = inputs['x'].shape
    CTX = inputs['ctx'].shape[1]
    key = (N, CTX)
    if key not in _NC_CACHE:
        _NC_CACHE[key] = build(N=N, CTX=CTX)
    nc = _NC_CACHE[key]
    f32 = lambda a: np.ascontiguousarray(np.asarray(a, dtype=np.float32))
    shared = {nm: f32(v) for nm, v in inputs.items() if nm not in ('x', 'c', 'ctx')}
    shared['gdn_a_log'] = shared['gdn_a_log'].reshape(DEPTH, 8)
    shared['gdn_dt_bias'] = shared['gdn_dt_bias'].reshape(DEPTH, 8)
    C, Sg = _rope_tables(N)
    shared['ident'] = np.eye(128, dtype=np.float32)
    shared['ropeC'] = C
    shared['ropeS'] = Sg
    shared['ustrict'] = np.triu(np.ones((128, 128), np.float32), 1)
    shared['gdnc'] = gdn_consts()
    x, c, ctx = f32(inputs['x']), f32(inputs['c']), f32(inputs['ctx'])
    in_maps = []
    for b in range(B):
        m = dict(shared)
        m['x'] = x[b]
        m['c'] = c[b]
        m['ctx'] = ctx[b]
        in_maps.append(m)
    res = run_bass_kernel_spmd(nc, in_maps, core_ids=list(range(B)))
    return np.stack([np.asarray(r['out'], dtype=np.float32) for r in res.results], axis=0)
```
